# Optimizing a Trainium2 kernel written in Bass

```python
import math
import jax, jax.numpy as jnp
from jax import lax
import numpy as np

D_MODEL = 1024
BATCH = 8
SEQ = 2048
DEPTH = 2

R_HEADS = 4
R_DK = 64
R_DV = 128
R_CHUNK = 128
ROPE_BASE = 10000.0
G_HEADS = 4
G_DK = 128
G_DV = 128
G_CHUNK = 64
CONV_K = 4
CONV_CH = 2 * G_HEADS * G_DK + G_HEADS * G_DV
IN_SIZES = (R_HEADS * R_DK, R_HEADS * R_DK, R_HEADS * R_DV, R_HEADS * R_DV,
            CONV_CH, G_HEADS * G_DV, G_HEADS, G_HEADS)
IN_COLS = 3592
MIX_WIDTH = R_HEADS * R_DV + G_HEADS * G_DV
D_FF = 2816
N_EXPERTS = 8
TOP_K = 2
D_FF_EXPERT = 3584
N_DENSE = (DEPTH + 1) // 2
N_MOE = DEPTH // 2
ALPHA = (2 * DEPTH) ** 0.25
BETA_INIT = (8 * DEPTH) ** -0.25
LN_EPS = 1e-5
NORM_EPS = 1e-6

kernel_name = 'hybrid_retnet_gdn_deepnorm_moe'


def layer_norm(x, g, b):
    xf = x.astype(jnp.float32)
    mu = jnp.mean(xf, axis=-1, keepdims=True)
    var = jnp.mean(jnp.square(xf - mu), axis=-1, keepdims=True)
    y = (xf - mu) * lax.rsqrt(var + LN_EPS) * g.astype(jnp.float32) + b.astype(jnp.float32)
    return y.astype(x.dtype)


def to_chunks(t, c):
    b, tl = t.shape[:2]
    t = t.reshape(b, tl // c, c, *t.shape[2:])
    return jnp.moveaxis(t, 2, 3)


def from_chunks(t):
    t = jnp.moveaxis(t, 3, 2)
    return t.reshape(t.shape[0], -1, *t.shape[3:])


def rope(t, pos):
    d = t.shape[-1]
    inv = ROPE_BASE ** (-jnp.arange(0, d, 2, dtype=jnp.float32) / d)
    ang = pos[:, None] * inv[None, :]
    cos = jnp.cos(ang)[:, None, :]
    sin = jnp.sin(ang)[:, None, :]
    t1, t2 = t[..., : d // 2], t[..., d // 2:]
    return jnp.concatenate([t1 * cos - t2 * sin, t1 * sin + t2 * cos], axis=-1)


def retention(q, k, v):
    b, tl, h, dk = q.shape
    dv = v.shape[-1]
    c = R_CHUNK
    log_g = jnp.log(1.0 - 2.0 ** (-5.0 - jnp.arange(h, dtype=jnp.float32)))
    idx = jnp.arange(c, dtype=jnp.float32)
    causal = jnp.tril(jnp.ones((c, c), dtype=bool))
    dmat = jnp.exp(jnp.where(causal, (idx[:, None] - idx[None, :])[None] * log_g[:, None, None], -jnp.inf))
    qc, kc, vc = to_chunks(q, c), to_chunks(k, c), to_chunks(v, c)
    scores = jnp.einsum('bnhid,bnhjd->bnhij', qc, kc) * dmat
    o_intra = jnp.einsum('bnhij,bnhje->bnhie', scores, vc)
    zeta = jnp.exp((c - 1.0 - idx)[None, :] * log_g[:, None])
    xi = jnp.exp((idx + 1.0)[None, :] * log_g[:, None])
    kv = jnp.einsum('bnhcd,bnhce->nbhde', kc * zeta[:, :, None], vc)
    chunk_decay = jnp.exp(c * log_g)[:, None, None]

    def step(s, kv_n):
        return s * chunk_decay + kv_n, s

    _, s_prev = lax.scan(step, jnp.zeros((b, h, dk, dv), jnp.float32), kv)
    o_inter = jnp.einsum('bnhcd,nbhde->bnhce', qc * xi[:, :, None], s_prev)
    return from_chunks(o_intra + o_inter)


def gated_delta_rule(q, k, v, g, beta):
    b, tl, h, dk = q.shape
    dv = v.shape[-1]
    c = G_CHUNK
    qc, kc, vc = to_chunks(q, c), to_chunks(k, c), to_chunks(v, c)
    gc = jnp.cumsum(to_chunks(g, c), axis=-1)
    bc = to_chunks(beta, c)[..., None]
    causal = jnp.tril(jnp.ones((c, c), dtype=bool))
    strict = jnp.tril(jnp.ones((c, c), dtype=bool), -1)
    decay = jnp.exp(jnp.where(causal, gc[..., :, None] - gc[..., None, :], -jnp.inf))
    kb = kc * bc
    a = jnp.where(strict, jnp.einsum('bnhid,bnhjd->bnhij', kb, kc) * decay, 0.0)
    rhs = jnp.concatenate([vc * bc, kb * jnp.exp(gc)[..., None]], axis=-1)
    sol = lax.linalg.triangular_solve(a, rhs, left_side=True, lower=True, unit_diagonal=True)
    u, w = sol[..., :dv], sol[..., dv:]
    qk = jnp.where(causal, jnp.einsum('bnhid,bnhjd->bnhij', qc, kc) * decay, 0.0)
    q_dec = qc * jnp.exp(gc)[..., None]
    k_dec = kc * jnp.exp(gc[..., -1:] - gc)[..., None]
    cd = jnp.exp(gc[..., -1])[..., None, None]
    xs = (jnp.moveaxis(u, 1, 0), jnp.moveaxis(w, 1, 0), jnp.moveaxis(qk, 1, 0),
          jnp.moveaxis(q_dec, 1, 0), jnp.moveaxis(k_dec, 1, 0), jnp.moveaxis(cd, 1, 0))

    def step(s, xs_n):
        u_n, w_n, qk_n, qd_n, kd_n, cd_n = xs_n
        v_new = u_n - jnp.einsum('bhcd,bhde->bhce', w_n, s)
        o_n = jnp.einsum('bhcd,bhde->bhce', qd_n, s) + jnp.einsum('bhij,bhje->bhie', qk_n, v_new)
        s = s * cd_n + jnp.einsum('bhcd,bhce->bhde', kd_n, v_new)
        return s, o_n

    _, o = lax.scan(step, jnp.zeros((b, h, dk, dv), jnp.float32), xs)
    return from_chunks(jnp.moveaxis(o, 0, 1))


def causal_conv(x, w):
    return lax.conv_general_dilated(x, w[:, None, :], window_strides=(1,), padding=((CONV_K - 1, 0),),
                                    dimension_numbers=('NWC', 'WIO', 'NWC'),
                                    feature_group_count=x.shape[-1])


def l2norm(t):
    return t * lax.rsqrt(jnp.sum(jnp.square(t), axis=-1, keepdims=True) + NORM_EPS)


def hybrid_mixer(x, w_in, conv_w, a_log, dt_bias, gdn_norm_w, w_o):
    b, tl, _ = x.shape
    f32 = jnp.float32
    hproj = x @ w_in
    splits = np.cumsum(IN_SIZES)[:-1].tolist()
    rq, rk, rv, rg, gqkv, gg, ga, gb = jnp.split(hproj, splits, axis=-1)
    pos = jnp.arange(tl, dtype=f32)
    rq = rope(rq.reshape(b, tl, R_HEADS, R_DK).astype(f32), pos)
    rk = rope(rk.reshape(b, tl, R_HEADS, R_DK).astype(f32), pos) * (R_DK ** -0.5)
    rv = rv.reshape(b, tl, R_HEADS, R_DV).astype(f32)
    ro = retention(rq, rk, rv)
    mu = jnp.mean(ro, axis=-1, keepdims=True)
    var = jnp.mean(jnp.square(ro - mu), axis=-1, keepdims=True)
    ro = (ro - mu) * lax.rsqrt(var + LN_EPS) * jax.nn.silu(rg.reshape(b, tl, R_HEADS, R_DV).astype(f32))
    gqkv = jax.nn.silu(causal_conv(gqkv, conv_w)).astype(f32)
    gq, gk, gv = jnp.split(gqkv, [G_HEADS * G_DK, 2 * G_HEADS * G_DK], axis=-1)
    gq = l2norm(gq.reshape(b, tl, G_HEADS, G_DK)) * (G_DK ** -0.5)
    gk = l2norm(gk.reshape(b, tl, G_HEADS, G_DK))
    gv = gv.reshape(b, tl, G_HEADS, G_DV)
    g = -jnp.exp(a_log.astype(f32)) * jax.nn.softplus(ga.astype(f32) + dt_bias.astype(f32))
    beta = jax.nn.sigmoid(gb.astype(f32))
    go = gated_delta_rule(gq, gk, gv, g, beta)
    go = go * lax.rsqrt(jnp.mean(jnp.square(go), axis=-1, keepdims=True) + NORM_EPS)
    go = go * gdn_norm_w.astype(f32) * jax.nn.silu(gg.reshape(b, tl, G_HEADS, G_DV).astype(f32))
    o = jnp.concatenate([ro.reshape(b, tl, -1), go.reshape(b, tl, -1)], axis=-1).astype(x.dtype)
    return o @ w_o


def swiglu(x, w_gate, w_up, w_down):
    return (jax.nn.silu(x @ w_gate) * (x @ w_up)) @ w_down


def moe_swiglu(x, router_w, w_gate, w_up, w_down):
    logits = (x @ router_w).astype(jnp.float32)
    top_vals, top_idx = lax.top_k(logits, TOP_K)
    gates = jax.nn.softmax(top_vals, axis=-1)
    combine = jnp.sum(jax.nn.one_hot(top_idx, N_EXPERTS, dtype=jnp.float32) * gates[..., None], axis=-2)
    combine = combine.astype(x.dtype)
    y = jnp.zeros_like(x)
    for e in range(N_EXPERTS):
        y = y + combine[..., e:e + 1] * swiglu(x, w_gate[e], w_up[e], w_down[e])
    return y


def setup_inputs(seed: int = 0) -> dict:
    key = jax.random.key(seed)
    ks = jax.random.split(key, 18)
    f32 = jnp.float32

    def nrm(k, shape, scale):
        return jax.random.normal(k, shape, f32) * scale

    x = nrm(ks[0], (BATCH, SEQ, D_MODEL), 1.0)
    w_in = nrm(ks[1], (DEPTH, D_MODEL, IN_COLS), D_MODEL ** -0.5)
    conv_w = nrm(ks[2], (DEPTH, CONV_K, CONV_CH), CONV_K ** -0.5)
    a_log = jnp.log(jax.random.uniform(ks[3], (DEPTH, G_HEADS), f32, 1.0, 16.0))
    dt = jnp.exp(jax.random.uniform(ks[4], (DEPTH, G_HEADS), f32, math.log(1e-3), math.log(1e-1)))
    dt_bias = dt + jnp.log(-jnp.expm1(-dt))
    gdn_norm_w = 1.0 + nrm(ks[5], (DEPTH, G_DV), 0.02)
    w_o = nrm(ks[6], (DEPTH, MIX_WIDTH, D_MODEL), BETA_INIT * MIX_WIDTH ** -0.5)
    ln1_g = 1.0 + nrm(ks[7], (DEPTH, D_MODEL), 0.02)
    ln1_b = nrm(ks[8], (DEPTH, D_MODEL), 0.02)
    ln2_g = 1.0 + nrm(ks[9], (DEPTH, D_MODEL), 0.02)
    ln2_b = nrm(ks[10], (DEPTH, D_MODEL), 0.02)
    ffn_w_gate = nrm(ks[11], (N_DENSE, D_MODEL, D_FF), D_MODEL ** -0.5)
    ffn_w_up = nrm(ks[12], (N_DENSE, D_MODEL, D_FF), D_MODEL ** -0.5)
    ffn_w_down = nrm(ks[13], (N_DENSE, D_FF, D_MODEL), BETA_INIT * D_FF ** -0.5)
    router_w = nrm(ks[14], (N_MOE, D_MODEL, N_EXPERTS), D_MODEL ** -0.5)
    moe_w_gate = nrm(ks[15], (N_MOE, N_EXPERTS, D_MODEL, D_FF_EXPERT), D_MODEL ** -0.5)
    moe_w_up = nrm(ks[16], (N_MOE, N_EXPERTS, D_MODEL, D_FF_EXPERT), D_MODEL ** -0.5)
    moe_w_down = nrm(ks[17], (N_MOE, N_EXPERTS, D_FF_EXPERT, D_MODEL), BETA_INIT * D_FF_EXPERT ** -0.5)
    return {'x': x, 'w_in': w_in, 'conv_w': conv_w, 'a_log': a_log, 'dt_bias': dt_bias,
            'gdn_norm_w': gdn_norm_w, 'w_o': w_o, 'ln1_g': ln1_g, 'ln1_b': ln1_b,
            'ln2_g': ln2_g, 'ln2_b': ln2_b, 'ffn_w_gate': ffn_w_gate, 'ffn_w_up': ffn_w_up,
            'ffn_w_down': ffn_w_down, 'router_w': router_w, 'moe_w_gate': moe_w_gate,
            'moe_w_up': moe_w_up, 'moe_w_down': moe_w_down}


def reference(x, w_in, conv_w, a_log, dt_bias, gdn_norm_w, w_o, ln1_g, ln1_b, ln2_g, ln2_b,
              ffn_w_gate, ffn_w_up, ffn_w_down, router_w, moe_w_gate, moe_w_up, moe_w_down):
    for l in range(DEPTH):
        mix = hybrid_mixer(x, w_in[l], conv_w[l], a_log[l], dt_bias[l], gdn_norm_w[l], w_o[l])
        x = layer_norm(ALPHA * x + mix, ln1_g[l], ln1_b[l])
        if l % 2 == 0:
            f = swiglu(x, ffn_w_gate[l // 2], ffn_w_up[l // 2], ffn_w_down[l // 2])
        else:
            f = moe_swiglu(x, router_w[l // 2], moe_w_gate[l // 2], moe_w_up[l // 2], moe_w_down[l // 2])
        x = layer_norm(ALPHA * x + f, ln2_g[l], ln2_b[l])
    return x
```

```python
import math
import numpy as np
import ml_dtypes
import concourse.bass as bass
import concourse.mybir as mybir
from concourse.bass_utils import run_bass_kernel_spmd
from contextlib import ExitStack

F32 = mybir.dt.float32
BF16 = mybir.dt.bfloat16
AF = mybir.ActivationFunctionType
ALU = mybir.AluOpType
AX = mybir.AxisListType

T = 2048
D = 1024
NT = 16
KC = 8
DEPTH = 2
ALPHA = (2 * DEPTH) ** 0.25
INVA = 1.0 / ALPHA
LN_EPS = 1e-5
LN_EPS_S = LN_EPS / (ALPHA * ALPHA)
NORM_EPS = 1e-6
D_FF = 2816
D_FFE = 3584
NE = 8
WEXT = 4104


class Prog:
    ENGS = ("pe", "act", "dve", "pool", "sp")

    def __init__(self, nc, same_engine_sync=True):
        self.nc = nc
        self.es = ExitStack()
        self.ops = []
        self.tok = {}
        self.same_engine_sync = same_engine_sync
        self.base_deps = set()
        self.last_eng = {}
        self.last_key = {}
        self.scopes = []

    def sb(self, name, shape, dt):
        es = self.scopes[-1] if self.scopes else self.es
        self.uid = getattr(self, "uid", 0) + 1
        return es.enter_context(self.nc.sbuf_tensor("%s_u%d" % (name, self.uid), list(shape), dt))

    def push_scope(self):
        self.barrier()
        self.scopes.append(ExitStack())

    def pop_scope(self):
        self.barrier()
        self.scopes.pop().close()

    def ps(self, name, shape, dt=F32):
        return self.es.enter_context(self.nc.psum_tensor(name, list(shape), dt))

    def op(self, eng, fn, reads=(), writes=(), dma_key=None):
        idx = len(self.ops)
        deps = set(self.base_deps)
        for t in reads:
            e = self.tok.get(t)
            if e is not None and e[0] is not None:
                deps.add(e[0])
        for t in writes:
            e = self.tok.get(t)
            if e is not None:
                if e[0] is not None:
                    deps.add(e[0])
                deps.update(e[1])
        for t in reads:
            e = self.tok.setdefault(t, [None, []])
            e[1].append(idx)
        for t in writes:
            self.tok[t] = [idx, []]
        deps.discard(idx)
        self.ops.append(dict(eng=eng, fn=fn, deps=deps, dma_key=dma_key))
        self.last_eng[eng] = idx
        if dma_key is not None:
            self.last_key[dma_key] = idx
        return idx

    def barrier(self):
        self.base_deps = set(self.last_eng.values()) | set(self.last_key.values())

    def pe(self, fn, reads=(), writes=()):
        return self.op("pe", fn, reads, writes)

    def act(self, fn, reads=(), writes=()):
        return self.op("act", fn, reads, writes)

    def dve(self, fn, reads=(), writes=()):
        return self.op("dve", fn, reads, writes)

    def pool(self, fn, reads=(), writes=()):
        return self.op("pool", fn, reads, writes)

    def dma(self, out, in_, reads=(), writes=(), key=None, eng="sp"):
        assert key is not None
        return self.op(eng, lambda e: e.dma_start(out=out, in_=in_), reads, writes, dma_key=key)

    def emit(self):
        nc = self.nc
        ops = self.ops
        n = len(ops)
        needed = [False] * n
        for i, o in enumerate(ops):
            keep = set()
            for d in o["deps"]:
                od = ops[d]
                if od["dma_key"] is None and od["eng"] == o["eng"]:
                    if o["eng"] == "pe" or not self.same_engine_sync:
                        continue
                keep.add(d)
            o["deps"] = keep
            for d in keep:
                needed[d] = True
        eng_sem = {}
        key_sem = {}
        eng_cnt = {e: 0 for e in self.ENGS}
        key_cnt = {}
        for i, o in enumerate(ops):
            if o["dma_key"] is not None:
                k = o["dma_key"]
                if k not in key_sem:
                    key_sem[k] = self.es.enter_context(nc.semaphore("d_" + str(k)))
                    key_cnt[k] = 0
                key_cnt[k] += 16
                o["sig"] = (key_sem[k], key_cnt[k])
                o["inc"] = (key_sem[k], 16)
            elif needed[i]:
                e = o["eng"]
                if e not in eng_sem:
                    eng_sem[e] = self.es.enter_context(nc.semaphore("e_" + e))
                eng_cnt[e] += 1
                o["sig"] = (eng_sem[e], eng_cnt[e])
                o["inc"] = (eng_sem[e], 1)
            else:
                o["sig"] = None
                o["inc"] = None
        per_eng = {e: [] for e in self.ENGS}
        for i, o in enumerate(ops):
            per_eng[o["eng"]].append(i)
        self.stats = {e: len(v) for e, v in per_eng.items()}
        self.stats["sems"] = len(key_sem) + len(eng_sem)

        def emit_engine(eng_name, engine):
            waited = {}
            for i in per_eng[eng_name]:
                o = ops[i]
                w = {}
                for d in o["deps"]:
                    sem, val = ops[d]["sig"]
                    key = id(sem)
                    if key not in w or w[key][1] < val:
                        w[key] = (sem, val)
                for key, (sem, val) in w.items():
                    if waited.get(key, 0) >= val:
                        continue
                    engine.wait_ge(sem, val)
                    waited[key] = val
                ins = o["fn"](engine)
                if o["inc"] is not None:
                    ins.then_inc(o["inc"][0], o["inc"][1])
            last = {}
            for i in per_eng[eng_name]:
                o = ops[i]
                if o["dma_key"] is not None:
                    sem, val = o["sig"]
                    last[id(sem)] = (sem, max(val, last.get(id(sem), (None, 0))[1]))
            for key, (sem, val) in last.items():
                if waited.get(key, 0) < val:
                    engine.wait_ge(sem, val)

        with nc.Block() as block:
            if per_eng["sp"]:
                @block.sync
                def _(e):
                    emit_engine("sp", e)
            if per_eng["pe"]:
                @block.tensor
                def _(e):
                    emit_engine("pe", e)
            if per_eng["act"]:
                @block.scalar
                def _(e):
                    emit_engine("act", e)
            if per_eng["dve"]:
                @block.vector
                def _(e):
                    emit_engine("dve", e)
            if per_eng["pool"]:
                @block.gpsimd
                def _(e):
                    emit_engine("pool", e)
        self.es.close()


def make_consts():
    c = {}
    c["ident32"] = np.eye(128, dtype=np.float32)
    c["ones32"] = np.ones((128, 128), np.float32)
    inv = (10000.0 ** (-np.arange(0, 64, 2, dtype=np.float32) / np.float32(64))).astype(np.float32)
    pos = np.arange(T, dtype=np.float32)
    ang = (pos[:, None] * inv[None, :]).astype(np.float32)
    cos = np.cos(ang.astype(np.float64)).astype(np.float32).T
    sin = np.sin(ang.astype(np.float64)).astype(np.float32).T
    C = np.zeros((128, T), np.float32)
    S = np.zeros((128, T), np.float32)
    for p in range(128):
        C[p] = cos[p % 32]
        S[p] = sin[p % 32] * (-1.0 if (p % 64) < 32 else 1.0)
    c["ropeC"] = C
    c["ropeS"] = S
    gam = 1.0 - 2.0 ** (-5.0 - np.arange(4, dtype=np.float64))
    idx = np.arange(128, dtype=np.float64)
    maskT = np.zeros((128, 4, 128), np.float32)
    for h in range(4):
        dlt = idx[None, :] - idx[:, None]
        m = np.where(dlt >= 0, gam[h] ** np.maximum(dlt, 0), 0.0) * 0.125
        maskT[:, h, :] = m
    c["r_maskT"] = maskT
    rcol = np.zeros((128, 12), np.float32)
    for h in range(4):
        rcol[:, h] = gam[h] ** (idx + 1.0)
        rcol[:, 4 + h] = gam[h] ** (127.0 - idx) * 0.125
    for j in range(2):
        rcol[:64, 8 + j] = gam[2 * j] ** 128.0
        rcol[64:, 8 + j] = gam[2 * j + 1] ** 128.0
    c["r_col"] = rcol
    t = np.arange(128)
    same = (t[:, None] // 64) == (t[None, :] // 64)
    tri = (same & (t[:, None] <= t[None, :])).astype(np.float32)
    su = (same & (t[:, None] > t[None, :])).astype(np.float32)
    c["g_tri"] = tri
    c["g_su"] = su
    c["g_blk"] = same.astype(np.float32)
    NEG = -30000.0
    c["g_negS"] = np.where(same & (t[None, :] < t[:, None]), 1.0, 0.0).astype(np.float32)
    c["g_negI"] = np.where(same & (t[None, :] >= t[:, None]), 1.0, 0.0).astype(np.float32)
    selA = np.zeros((128, 128), np.float32); selA[:64, :] = 1.0
    selB = np.zeros((128, 128), np.float32); selB[64:, :] = 1.0
    c["g_selA"] = selA
    c["g_selB"] = selB
    return c


CONST_NAMES = ["ident32", "ones32", "ropeC", "ropeS", "r_maskT", "r_col", "g_tri", "g_su",
               "g_blk", "g_negS", "g_negI", "g_selA", "g_selB"]


class Builder:
    def __init__(self, cfg):
        self.cfg = cfg
        nc = bass.Bass("TRN2", target_bir_lowering=False)
        self.nc = nc
        self.P = Prog(nc, same_engine_sync=cfg.get("ses", True))
        self.dram = {}

    def din(self, name, shape, dt=F32):
        t = self.nc.dram_tensor(name, list(shape), dt, kind="ExternalInput").ap()
        self.dram[name] = t
        return t

    def dout(self, name, shape, dt=F32):
        t = self.nc.dram_tensor(name, list(shape), dt, kind="ExternalOutput").ap()
        self.dram[name] = t
        return t

    def build(self):
        cfg = self.cfg
        P = self.P
        nc = self.nc
        layers = cfg.get("layers", [0, 1])
        x_d = self.din("x", [T, D])
        win_d = self.din("w_in", [DEPTH, D, WEXT])
        cw_d = self.din("conv_w", [DEPTH, 128, 12, 4])
        alog_d = self.din("a_log", [DEPTH, 128, 4])
        dtb_d = self.din("dt_bias", [DEPTH, 128, 4])
        gnw_d = self.din("gdn_norm_w", [DEPTH, 128, 128])
        wo_d = self.din("w_o", [DEPTH, D, D])
        lnp_d = {k: self.din(k, [DEPTH, 128, D]) for k in ("ln1_g", "ln1_b", "ln2_g", "ln2_b")}
        lnc_d = {k: self.din(k + "_c", [DEPTH, 128, KC]) for k in ("ln1_g", "ln1_b", "ln2_g", "ln2_b")}
        fwg_d = self.din("ffn_w_gate", [D, D_FF])
        fwu_d = self.din("ffn_w_up", [D, D_FF])
        fwd_d = self.din("ffn_w_down", [D_FF, D])
        rw_d = self.din("router_w", [D, NE])
        mwg_d = self.din("moe_w_gate", [NE, D, D_FFE])
        mwu_d = self.din("moe_w_up", [NE, D, D_FFE])
        mwd_d = self.din("moe_w_down", [NE, D_FFE, D])
        cst_d = {}
        cshapes = {"ident32": [128, 128], "ones32": [128, 128], "ropeC": [128, T], "ropeS": [128, T],
                   "r_maskT": [128, 4, 128], "r_col": [128, 12], "g_tri": [128, 128], "g_su": [128, 128],
                   "g_blk": [128, 128], "g_negS": [128, 128], "g_negI": [128, 128],
                   "g_selA": [128, 128], "g_selB": [128, 128]}
        for k in CONST_NAMES:
            cst_d[k] = self.din("c_" + k, cshapes[k])
        out_d = self.dout("out", [T, D])
        self.dbg_d = None
        if cfg.get("dbg"):
            self.dbg_d = self.dout("dbg", cfg["dbg_shape"])

        self.X = P.sb("X", [128, NT, D], F32)
        self.XT = P.sb("XT", [128, KC, T], BF16)
        self.ident32 = P.sb("ident32", [128, 128], F32)
        self.identb = P.sb("identb", [128, 128], BF16)
        self.ones32 = P.sb("ones32", [128, 128], F32)
        self.LNG = P.sb("LNG", [128, D], F32)
        self.LNB = P.sb("LNB", [128, D], F32)
        self.small = P.sb("small", [128, NT, 16], F32)
        self.CMB = P.sb("CMB", [128, NT, NE], F32)
        self.PS = [P.ps("ps%d" % k, [128, 512], F32) for k in range(7)]
        self.PSB = P.ps("psb", [128, 1024], BF16)

        P.dma(self.ident32[:], cst_d["ident32"], writes=["ident32"], key="c0")
        P.dma(self.ones32[:], cst_d["ones32"], writes=["ones32"], key="c1")
        P.dma(self.identb[:], cst_d["ident32"], writes=["identb"], key="c2", eng="pool")
        self.onesb = P.sb("onesb", [128, 128], BF16)
        P.dma(self.onesb[:], cst_d["ones32"], writes=["onesb"], key="c3", eng="pool")
        self.cst_d = cst_d
        xv = x_d.rearrange("(tt p) d -> p tt d", p=128)
        for q in range(4):
            P.dma(self.X[:, q * 4:(q + 1) * 4, :], xv[:, q * 4:(q + 1) * 4, :],
                  writes=[("X", tt) for tt in range(q * 4, q * 4 + 4)], key="xin%d" % q)
        for tt in range(NT):
            self.make_xt(tt)

        for l in layers:
            if cfg.get("mixer", True):
                self.mixer(l, win_d, cw_d, alog_d, dtb_d, gnw_d, wo_d)
            g = None
            if cfg.get("ffn", True):
                if l == 0:
                    g = self.ffn(1, D_FF, lambda e: fwg_d, lambda e: fwu_d, lambda e: fwd_d, None, "f0")
                else:
                    g = self.ffn(NE, D_FFE, lambda e: mwg_d[e], lambda e: mwu_d[e], lambda e: mwd_d[e], self.CMB, "f1")
                next(g)
            if cfg.get("mixer", True) or cfg.get("ln1"):
                self.layer_norm(l, "ln1", lnp_d, lnc_d, route=(l == 1 and cfg.get("ffn", True)), rw_d=rw_d)
            if cfg.get("ln_only"):
                self.layer_norm(l, "ln2", lnp_d, lnc_d)
            if g is not None:
                for _ in g:
                    pass
                if not cfg.get("no_ln2"):
                    self.layer_norm(l, "ln2", lnp_d, lnc_d, need_xt=(l != layers[-1]))

        ov = out_d.rearrange("(tt p) d -> p tt d", p=128)
        for q in range(4):
            P.dma(ov[:, q * 4:(q + 1) * 4, :], self.X[:, q * 4:(q + 1) * 4, :],
                  reads=[("X", tt) for tt in range(q * 4, q * 4 + 4)], key="xout%d" % q)
        P.emit()
        return nc

    def make_xt(self, tt, xt32=None):
        P = self.P
        X, XT = self.X, self.XT
        for half in range(2):
            bi = 5 + ((tt * 2 + half) % 2)
            bank = self.PS[bi]
            btok = ("ps", bi)
            for q in range(4):
                kc = half * 4 + q
                P.pe(lambda e, o=bank[:, q * 128:(q + 1) * 128], i=X[:, tt, kc * 128:(kc + 1) * 128]:
                     e.transpose(o, i, self.ident32[:]),
                     reads=[("X", tt), "ident32"], writes=[btok])
            outap = XT[:, half * 4:(half + 1) * 4, tt * 128:(tt + 1) * 128]
            inap = bank[:].rearrange("p (q c) -> p q c", q=4)
            xtok = ("XT", tt, half)
            if half == 0:
                P.act(lambda e, o=outap, i=inap: e.copy(o, i), reads=[btok], writes=[xtok])
            else:
                P.dve(lambda e, o=outap, i=inap: e.tensor_copy(o, i), reads=[btok], writes=[xtok])
            if xt32 is not None:
                o32 = xt32[:, half * 4:(half + 1) * 4, :]
                if half == 0:
                    P.act(lambda e, o=o32, i=inap: e.copy(o, i), reads=[btok], writes=[("xt32", id(xt32), half)])
                else:
                    P.dve(lambda e, o=o32, i=inap: e.tensor_copy(o, i), reads=[btok], writes=[("xt32", id(xt32), half)])

    def layer_norm(self, l, which, lnp_d, lnc_d, route=False, rw_d=None, need_xt=True):
        P = self.P
        X = self.X
        P.dma(self.LNG[:], lnp_d[which + "_g"][l], writes=["LNG"], key="lng")
        P.dma(self.LNB[:], lnp_d[which + "_b"][l], writes=["LNB"], key="lnb")
        sm = self.small
        if route:
            RW = P.sb("RW", [128, KC, NE], F32)
            XT32 = [P.sb("XT32_%d" % i, [128, KC, 128], F32) for i in range(2)]
            LG = P.sb("LG", [128, NT, NE], F32)
            P.dma(RW[:], rw_d.rearrange("(kc p) e -> p kc e", p=128), writes=["RW"], key="rw")
        for tt in range(NT):
            st = ("sm", tt)
            xt = ("X", tt)
            for hf in range(2):
                P.dve(lambda e, o=sm[:, tt, hf * 6:(hf + 1) * 6], i=X[:, tt, hf * 512:(hf + 1) * 512]: e.bn_stats(o, i),
                      reads=[xt, st], writes=[st])
            P.dve(lambda e, o=sm[:, tt, 12:14], i=sm[:, tt, 0:12].rearrange("p (a b) -> p a b", a=2): e.bn_aggr(o, i),
                  reads=[st], writes=[st])
        allst = [("sm", tt) for tt in range(NT)]
        P.act(lambda e: e.activation(sm[:, :, 14], sm[:, :, 13], AF.Sqrt, bias=LN_EPS_S), reads=allst, writes=["sm_r"])
        P.dve(lambda e: e.reciprocal(sm[:, :, 14], sm[:, :, 14]), reads=["sm_r"], writes=["sm_r"])
        P.dve(lambda e: e.scalar_tensor_tensor(sm[:, :, 15], sm[:, :, 12], -1.0, sm[:, :, 14], ALU.mult, ALU.mult), reads=allst + ["sm_r"], writes=["sm_r"])
        for tt in range(NT):
            xt = ("X", tt)
            P.act(lambda e, o=X[:, tt, :], s=sm[:, tt, 14:15], b=sm[:, tt, 15:16]:
                  e.activation(o, o, AF.Identity, bias=b, scale=s), reads=[xt, "sm_r"], writes=[xt])
        for tt in range(NT):
            xt = ("X", tt)
            P.pool(lambda e, o=X[:, tt, :]: e.tensor_tensor(o, o, self.LNG[:], ALU.mult), reads=[xt, "LNG"], writes=[xt])
            P.pool(lambda e, o=X[:, tt, :]: e.tensor_tensor(o, o, self.LNB[:], ALU.add), reads=[xt, "LNB"], writes=[xt])
        for tt in range(NT if need_xt else 0):
            xt32 = XT32[tt % 2] if route else None
            self.make_xt(tt, xt32=xt32)
            if route and self.cfg.get("route_mm", True):
                bank = self.PS[4]
                for kc in range(KC):
                    P.pe(lambda e, o=bank[:, 0:NE], a=xt32[:, kc, :], b=RW[:, kc, :], s=(kc == 0), t=(kc == KC - 1):
                         e.matmul(o, a, b, start=s, stop=t),
                         reads=[("xt32", id(xt32), kc // 4), "RW"], writes=[("ps", 4)])
                P.dve(lambda e, o=LG[:, tt, :], i=bank[:, 0:NE]: e.tensor_copy(o, i), reads=[("ps", 4)], writes=["LG"])
        if route:
            if self.cfg.get("route_stop", 9) >= 2:
                self.route(LG)
            else:
                P.dve(lambda e: e.memset(self.CMB[:], INVA), reads=["LG"], writes=["CMB"])

    def route(self, LG):
        P = self.P
        m1 = P.sb("rt_m1", [128, NT], F32)
        m2 = P.sb("rt_m2", [128, NT], F32)
        eq1 = P.sb("rt_eq1", [128, NT, NE], F32)
        eq2 = P.sb("rt_eq2", [128, NT, NE], F32)
        L2 = P.sb("rt_L2", [128, NT, NE], F32)
        g1 = P.sb("rt_g1", [128, NT], F32)
        g2 = P.sb("rt_g2", [128, NT], F32)
        R = ["LG", "rt"]
        bc = lambda a: a[:].unsqueeze(2).to_broadcast([128, NT, NE])
        P.dve(lambda e: e.tensor_reduce(m1[:], LG[:], AX.X, ALU.max), reads=R, writes=["rt"])
        P.dve(lambda e: e.tensor_tensor(eq1[:], LG[:], bc(m1), ALU.is_equal), reads=R, writes=["rt"])
        P.dve(lambda e: e.scalar_tensor_tensor(L2[:], eq1[:], -1.0e30, LG[:], ALU.mult, ALU.add), reads=R, writes=["rt"])
        P.dve(lambda e: e.tensor_reduce(m2[:], L2[:], AX.X, ALU.max), reads=R, writes=["rt"])
        P.dve(lambda e: e.tensor_tensor(eq2[:], L2[:], bc(m2), ALU.is_equal), reads=R, writes=["rt"])
        P.dve(lambda e: e.tensor_tensor(g2[:], m2[:], m1[:], ALU.subtract), reads=R, writes=["rt"])
        P.act(lambda e: e.activation(g2[:], g2[:], AF.Exp), reads=R, writes=["rt"])
        P.dve(lambda e: e.tensor_scalar(g1[:], g2[:], 1.0, None, ALU.add), reads=R, writes=["rt"])
        P.dve(lambda e: e.reciprocal(g1[:], g1[:]), reads=R, writes=["rt"])
        P.dve(lambda e: e.tensor_tensor(g2[:], g2[:], g1[:], ALU.mult), reads=R, writes=["rt"])
        P.dve(lambda e: e.tensor_tensor(eq1[:], eq1[:], bc(g1), ALU.mult), reads=R, writes=["rt"])
        P.dve(lambda e: e.tensor_tensor(eq2[:], eq2[:], bc(g2), ALU.mult), reads=R, writes=["rt"])
        P.dve(lambda e: e.tensor_tensor(eq1[:], eq1[:], eq2[:], ALU.add), reads=R, writes=["rt"])
        P.dve(lambda e: e.tensor_scalar(self.CMB[:], eq1[:], INVA, None, ALU.mult), reads=R, writes=["CMB"])

    def ffn(self, n_exp, F, wg_of, wu_of, wd_of, cmb, tag):
        P = self.P
        X, XT = self.X, self.XT
        P.push_scope()
        G = 4
        nft = F // 128
        groups = []
        for e in range(n_exp):
            f0 = 0
            while f0 < nft:
                g = min(G, nft - f0)
                groups.append((e, f0, g))
                f0 += g
        WG = [P.sb("%s_WG%d" % (tag, i), [128, KC, G * 128], BF16) for i in range(2)]
        WU = [P.sb("%s_WU%d" % (tag, i), [128, KC, G * 128], BF16) for i in range(2)]
        WD = [P.sb("%s_WD%d" % (tag, i), [128, G, D], BF16) for i in range(2)]
        HT = [P.sb("%s_HT%d" % (tag, i), [128, G, T], BF16) for i in range(2)]
        SG = [P.sb("%s_SG%d" % (tag, i), [128, 512], F32) for i in range(2)]
        PS = self.PS

        def load(gi):
            load_gu(gi)
            load_d(gi)

        def load_gu(gi):
            e, f0, g = groups[gi]
            s = gi % 2
            c0, c1 = f0 * 128, (f0 + g) * 128
            P.dma(WG[s][:, :, 0:g * 128], wg_of(e)[:, c0:c1].rearrange("(kc p) f -> p kc f", p=128),
                  writes=[("WG", s)], key="%swg%d" % (tag, s), eng="pool")
            P.dma(WU[s][:, :, 0:g * 128], wu_of(e)[:, c0:c1].rearrange("(kc p) f -> p kc f", p=128),
                  writes=[("WU", s)], key="%swu%d" % (tag, s), eng="pool")

        def load_d(gi):
            e, f0, g = groups[gi]
            s = gi % 2
            c0, c1 = f0 * 128, (f0 + g) * 128
            P.dma(WD[s][:, 0:g, :], wd_of(e)[c0:c1, :].rearrange("(j p) m -> p j m", p=128),
                  writes=[("WD", s)], key="%swd%d" % (tag, s), eng="pool")

        def gu(gi):
            e, f0, g = groups[gi]
            s = gi % 2
            cnt = 0
            for j in range(g):
                for tb in range(4):
                    b = cnt % 2
                    cnt += 1
                    pg, pu = PS[b], PS[2 + b]
                    for kc in range(KC):
                        P.pe(lambda en, o=pg[:], a=WG[s][:, kc, j * 128:(j + 1) * 128], r=XT[:, kc, tb * 512:(tb + 1) * 512],
                             st=(kc == 0), sp=(kc == KC - 1): en.matmul(o, a, r, start=st, stop=sp),
                             reads=[("WG", s)] + [("XT", tt, hh) for tt in range(tb * 4, tb * 4 + 4) for hh in range(2)], writes=[("ps", b)])
                    for kc in range(KC):
                        P.pe(lambda en, o=pu[:], a=WU[s][:, kc, j * 128:(j + 1) * 128], r=XT[:, kc, tb * 512:(tb + 1) * 512],
                             st=(kc == 0), sp=(kc == KC - 1): en.matmul(o, a, r, start=st, stop=sp),
                             reads=[("WU", s)] + [("XT", tt, hh) for tt in range(tb * 4, tb * 4 + 4) for hh in range(2)], writes=[("ps", 2 + b)])
                    P.act(lambda en, o=SG[b][:], i=pg[:]: en.activation(o, i, AF.Silu),
                          reads=[("ps", b)], writes=[("SG", b)])
                    P.dve(lambda en, o=HT[s][:, j, tb * 512:(tb + 1) * 512], a=SG[b][:], c=pu[:]:
                          en.tensor_tensor(o, a, c, ALU.mult),
                          reads=[("SG", b), ("ps", 2 + b)], writes=[("HT", s, j, tb)])

        def down(gi):
            e, f0, g = groups[gi]
            s = gi % 2
            for tt in range(NT):
                for hf in range(2):
                    b = 4 + ((tt * 2 + hf) % 2)
                    pd = PS[b]
                    for j in range(g):
                        P.pe(lambda en, o=pd[:], a=HT[s][:, j, tt * 128:(tt + 1) * 128], r=WD[s][:, j, hf * 512:(hf + 1) * 512],
                             st=(j == 0), sp=(j == g - 1): en.matmul(o, a, r, start=st, stop=sp),
                             reads=[("HT", s, j, tt // 4), ("WD", s)], writes=[("ps", b)])
                    sc = INVA if cmb is None else cmb[:, tt, e:e + 1]
                    P.dve(lambda en, o=X[:, tt, hf * 512:(hf + 1) * 512], i=pd[:], sc=sc:
                          en.scalar_tensor_tensor(o, i, sc, o, ALU.mult, ALU.add),
                          reads=[("ps", b), ("X", tt), "CMB"], writes=[("X", tt)])

        ng = len(groups)
        ng = min(ng, self.cfg.get("ffn_ng", ng))
        mode = self.cfg.get("ffn_mode", 3)
        load(0)
        yield
        for gi in range(ng):
            if gi + 1 < ng:
                load_gu(gi + 1)
            if mode >= 1:
                gu(gi)
            if gi >= 1 and mode >= 3:
                down(gi - 1)
            if gi + 1 < ng:
                load_d(gi + 1)
        if mode >= 3:
            down(ng - 1)
        if self.cfg.get("dbg") == "ht":
            tmp = P.sb("dbg_tmp", [128, T], F32)
            sl = (ng - 1) % 2
            P.dve(lambda en: en.tensor_copy(tmp[:], HT[sl][:, self.cfg.get("dbg_j", 0), :]), reads=[("HT", sl, j, tb) for j in range(G) for tb in range(4)], writes=["dbgtmp"])
            P.dma(self.dbg_d, tmp[:], reads=["dbgtmp"], key="dbg")
        P.pop_scope()

    def mixer(self, l, win_d, cw_d, alog_d, dtb_d, gnw_d, wo_d):
        P = self.P
        cfg = self.cfg
        X = self.X
        P.push_scope()
        OT = P.sb("OT", [128, KC, T], BF16)
        self.OT = OT
        if cfg.get("retnet", True):
            self.retnet(l, win_d, OT)
        if cfg.get("gdn", True):
            self.gdn(l, win_d, cw_d, alog_d, dtb_d, gnw_d, OT)
        if cfg.get("dbg") == "ot":
            tmp = P.sb("dbg_tmp", [128, T], F32)
            for kc in cfg.get("dbg_kcs", range(KC)):
                P.dve(lambda en, kc=kc: en.tensor_copy(tmp[:], OT[:, kc, :]), reads=[("OT", n) for n in range(NT)] + ["dbgtmp"], writes=["dbgtmp"])
                P.dma(self.dbg_d[:, kc, :], tmp[:], reads=["dbgtmp"], key="dbg")
        if cfg.get("wo", True):
            P.push_scope()
            WO = P.sb("WO", [128, KC, D], BF16)
            P.dma(WO[:], wo_d[l].rearrange("(kc p) m -> p kc m", p=128), writes=["WO"], key="wo", eng="pool")
            for tt in range(NT):
                for hf in range(2):
                    b = (tt * 2 + hf) % 2
                    for kc in range(KC):
                        P.pe(lambda en, o=self.PS[b][:], a=OT[:, kc, tt * 128:(tt + 1) * 128], r=WO[:, kc, hf * 512:(hf + 1) * 512],
                             st=(kc == 0), sp=(kc == KC - 1): en.matmul(o, a, r, start=st, stop=sp),
                             reads=[("OT", tt), "WO"], writes=[("ps", b)])
                    P.dve(lambda en, o=X[:, tt, hf * 512:(hf + 1) * 512], i=self.PS[b][:]:
                          en.scalar_tensor_tensor(o, i, INVA, o, ALU.mult, ALU.add),
                          reads=[("ps", b), ("X", tt)], writes=[("X", tt)])
            P.pop_scope()
        P.pop_scope()

    def retnet(self, l, win_d, OT):
        P = self.P
        PS, PSB, XT = self.PS, self.PSB, self.XT
        cst = self.cst_d
        P.push_scope()
        QTz = [P.sb("r_QTz%d" % h, [128, T], BF16) for h in range(4)]
        for h in range(4):
            ob = 64 * (1 - h % 2)
            P.pool(lambda en, o=QTz[h][ob:ob + 64, :]: en.memset(o, 0.0), writes=[("rqz", h)])
        KT = P.sb("r_KT", [128, 2, T], BF16)
        KZ = P.sb("r_KZ", [128, NT, 256], BF16)
        MASKT = P.sb("r_maskT", [128, 4, 128], F32)
        RCOL = P.sb("r_col", [128, 12], F32)
        P.dma(MASKT[:], cst["r_maskT"], writes=["r_maskT"], key="rc_m")
        P.dma(RCOL[:], cst["r_col"], writes=["r_col"], key="rc_c")
        P.push_scope()
        WQK = P.sb("r_WQK", [128, KC, 1024], BF16)
        RC = [P.sb("r_RC%d" % i, [128, 512], F32) for i in range(2)]
        RS = [P.sb("r_RS%d" % i, [128, 512], F32) for i in range(2)]
        T1 = [P.sb("r_T1%d" % i, [128, 512], F32) for i in range(2)]
        T2 = [P.sb("r_T2%d" % i, [128, 512], F32) for i in range(2)]
        P.dma(WQK[:], win_d[l][:, 0:1024].rearrange("(kc p) c -> p kc c", p=128), writes=["WQK"], key="rwqk", eng="pool")
        cnt = 0
        for tb in range(4):
            s = tb % 2
            P.dma(RC[s][:], cst["ropeC"][:, tb * 512:(tb + 1) * 512], writes=[("RC", s)], key="rc%d" % s)
            P.dma(RS[s][:], cst["ropeS"][:, tb * 512:(tb + 1) * 512], writes=[("RS", s)], key="rs%d" % s)
            xr = [("XT", tt, hh) for tt in range(tb * 4, tb * 4 + 4) for hh in range(2)]
            for which in range(2):
                for j in range(2):
                    c = cnt % 2
                    cnt += 1
                    colA = which * 512 + j * 128
                    colB = colA + 256
                    for (col, bi) in ((colA, c), (colB, 2 + c)):
                        for kc in range(KC):
                            P.pe(lambda en, o=PS[bi][:], a=WQK[:, kc, col:col + 128], r=XT[:, kc, tb * 512:(tb + 1) * 512],
                                 st=(kc == 0), sp=(kc == KC - 1): en.matmul(o, a, r, start=st, stop=sp),
                                 reads=["WQK"] + xr, writes=[("ps", bi)])
                    P.dve(lambda en, o=T1[c][:], a=PS[c][:], b=RC[s][:]: en.tensor_tensor(o, a, b, ALU.mult),
                          reads=[("ps", c), ("RC", s)], writes=[("T1", c)])
                    P.dve(lambda en, o=T2[c][:], a=PS[2 + c][:], b=RS[s][:]: en.tensor_tensor(o, a, b, ALU.mult),
                          reads=[("ps", 2 + c), ("RS", s)], writes=[("T2", c)])
                    if which == 1:
                        P.pool(lambda en, o=KT[:, j, tb * 512:(tb + 1) * 512], a=T1[c][:], b=T2[c][:]: en.tensor_tensor(o, a, b, ALU.add),
                               reads=[("T1", c), ("T2", c)], writes=[("rqk", which, j, tb)])
                    else:
                        for hh in range(2):
                            pr = slice(64 * hh, 64 * hh + 64)
                            P.pool(lambda en, o=QTz[2 * j + hh][pr, tb * 512:(tb + 1) * 512], a=T1[c][pr, :], b=T2[c][pr, :]: en.tensor_tensor(o, a, b, ALU.add),
                                   reads=[("T1", c), ("T2", c)], writes=[("rqk", 0, 2 * j + hh, tb)])
        rstop = self.cfg.get("ret_stop", 99)
        for tt in range(NT if rstop >= 2 else 0):
            for j in range(2):
                P.pe(lambda en, o=PSB[:, j * 128:(j + 1) * 128], i=KT[:, j, tt * 128:(tt + 1) * 128]: en.transpose(o, i, self.identb[:]),
                     reads=[("rqk", 1, j, tt // 4), "identb"], writes=["psb"])
            for j in range(2):
                for hh in range(2):
                    h = 2 * j + hh
                    P.act(lambda en, o=KZ[:, tt, j * 128 + hh * 64: j * 128 + hh * 64 + 64], i=PSB[:, j * 128 + hh * 64: j * 128 + hh * 64 + 64],
                          sc=RCOL[:, 4 + h:5 + h]: en.activation(o, i, AF.Copy, scale=sc),
                          reads=["psb", "r_col"], writes=[("KZ", tt, j, hh)])
        P.pop_scope()
        P.push_scope()
        WV = P.sb("r_WV", [128, KC, 1024], BF16)
        P.dma(WV[:], win_d[l][:, 1024:2048].rearrange("(kc p) c -> p kc c", p=128), writes=["WV"], key="rwv", eng="pool")
        Vt = [P.sb("r_Vt%d" % i, [128, 512], BF16) for i in range(2)]
        RG = [P.sb("r_RG%d" % i, [128, 512], F32) for i in range(2)]
        S32 = P.sb("r_S32", [128, 2, 128], F32)
        Sb = P.sb("r_Sb", [128, 2, 128], BF16)
        PT = [P.sb("r_PT%d" % i, [128, 128], BF16) for i in range(4)]
        TMP = [P.sb("r_TMP%d" % i, [128, 128], F32) for i in range(4)]
        OR = [P.sb("r_OR%d" % i, [128, 4, 128], F32) for i in range(2)]
        OF = [P.sb("r_OF0", [128, 512], F32)] * 2
        ST = P.sb("r_ST", [128, 2, 4, 8], F32)
        R2 = P.sb("r_R2", [128, 2, 4, 2], F32)
        P.dve(lambda en: en.memset(S32[:], 0.0), writes=["rS32"])
        P.dve(lambda en: en.memset(Sb[:], 0.0), writes=[("rSb", 0), ("rSb", 1)])
        KVB = (1, 2)
        INB = (4, 6)

        def front(n):
            s = n % 2
            tk = slice(n * 128, (n + 1) * 128)
            xr = [("XT", n, hh) for hh in range(2)]
            kvb, inb = KVB[s], INB[s]
            for part in range(2):
                for kc in range(KC):
                    P.pe(lambda en, a=XT[:, kc, tk], r=WV[:, kc, part * 512:(part + 1) * 512],
                         st=(kc == 0), sp=(kc == KC - 1): en.matmul(PS[0][:], a, r, start=st, stop=sp),
                         reads=["WV"] + xr, writes=[("ps", 0)])
                if part == 0:
                    P.act(lambda en, o=Vt[s][:]: en.copy(o, PS[0][:]), reads=[("ps", 0)], writes=[("rVt", s)])
                else:
                    P.act(lambda en, o=RG[s][:]: en.activation(o, PS[0][:], AF.Silu), reads=[("ps", 0)], writes=[("rRG", s)])
            for j in range(2):
                P.pe(lambda en, o=PS[kvb][:, j * 256:(j + 1) * 256], a=KZ[:, n, j * 128:(j + 1) * 128], r=Vt[s][:, j * 256:(j + 1) * 256]:
                     en.matmul(o, a, r, start=True, stop=True),
                     reads=[("KZ", n, j, 0), ("KZ", n, j, 1), ("rVt", s)], writes=[("ps", kvb)])
            for h in range(4):
                j = h // 2
                hs = slice(h * 128, (h + 1) * 128)
                P.pe(lambda en, o=PS[3][:, hs], a=KT[:, j, tk], r=QTz[h][:, tk]: en.matmul(o, a, r, start=True, stop=True),
                     reads=[("rqk", 0, h, n // 4), ("rqk", 1, j, n // 4), ("rqz", h)], writes=[("ps", 3)])
            for h in range(4):
                hs = slice(h * 128, (h + 1) * 128)
                P.dve(lambda en, o=PT[h][:], a=PS[3][:, hs], b=MASKT[:, h, :]: en.tensor_tensor(o, a, b, ALU.mult),
                      reads=[("ps", 3), "r_maskT"], writes=[("rPT", h)])
            for h in range(4):
                hs = slice(h * 128, (h + 1) * 128)
                P.pe(lambda en, o=PS[inb][:, hs], a=PT[h][:], r=Vt[s][:, hs]: en.matmul(o, a, r, start=True, stop=True),
                     reads=[("rPT", h), ("rVt", s)], writes=[("ps", inb)])

        def back(n):
            s = n % 2
            tk = slice(n * 128, (n + 1) * 128)
            kvb, inb = KVB[s], INB[s]
            for h in range(4):
                j = h // 2
                hs = slice(h * 128, (h + 1) * 128)
                P.pe(lambda en, o=PS[5][:, hs], a=QTz[h][:, tk], r=Sb[:, j, :]: en.matmul(o, a, r, start=True, stop=True),
                     reads=[("rqk", 0, h, n // 4), ("rqz", h), ("rSb", j)], writes=[("ps", 5)])
            for h in range(4):
                hs = slice(h * 128, (h + 1) * 128)
                P.act(lambda en, o=TMP[h][:], i=PS[5][:, hs], sc=RCOL[:, h:h + 1]: en.activation(o, i, AF.Copy, scale=sc),
                      reads=[("ps", 5), "r_col"], writes=[("rTMP", h)])
                P.dve(lambda en, o=OR[s][:, h, :], a=PS[inb][:, hs], b=TMP[h][:]: en.tensor_tensor(o, a, b, ALU.add),
                      reads=[("ps", inb), ("rTMP", h)], writes=[("rOR", s, h)])
            for h in range(4):
                j, pb = h // 2, 64 * (h % 2)
                P.dve(lambda en, o=S32[pb:pb + 64, j, :], i=PS[kvb][pb:pb + 64, j * 256 + (h % 2) * 128: j * 256 + (h % 2) * 128 + 128],
                      sc=RCOL[pb:pb + 64, 8 + j:9 + j]: en.scalar_tensor_tensor(o, o, sc, i, ALU.mult, ALU.add),
                      reads=[("ps", kvb), "rS32", "r_col"], writes=["rS32"])
            for j in range(2):
                P.act(lambda en, o=Sb[:, j, :], i=S32[:, j, :]: en.copy(o, i), reads=["rS32"], writes=[("rSb", j)])
            for h in range(4):
                P.dve(lambda en, o=ST[:, n % 2, h, 0:6], i=OR[s][:, h, :]: en.bn_stats(o, i), reads=[("rOR", s, h)], writes=[("rST", n % 2, h)])
                P.dve(lambda en, o=ST[:, n % 2, h, 6:8], i=ST[:, n % 2, h, 0:6]: en.bn_aggr(o, i), reads=[("rST", n % 2, h)], writes=[("rST", n % 2, h)])
            st_all = [("rST", n % 2, h) for h in range(4)]
            P.act(lambda en, o=R2[:, n % 2, :, 0], i=ST[:, n % 2, :, 7]: en.activation(o, i, AF.Sqrt, bias=LN_EPS), reads=st_all, writes=[("rR2", n % 2)])
            P.dve(lambda en, o=R2[:, n % 2, :, 0]: en.reciprocal(o, o), reads=[("rR2", n % 2)], writes=[("rR2", n % 2)])
            P.dve(lambda en, o=R2[:, n % 2, :, 1], a=ST[:, n % 2, :, 6], b=R2[:, n % 2, :, 0]: en.scalar_tensor_tensor(o, a, -1.0, b, ALU.mult, ALU.mult),
                  reads=st_all + [("rR2", n % 2)], writes=[("rR2", n % 2)])
            for h in range(4):
                P.act(lambda en, o=OR[s][:, h, :], sc=R2[:, n % 2, h, 0:1], bi=R2[:, n % 2, h, 1:2]: en.activation(o, o, AF.Identity, bias=bi, scale=sc),
                      reads=[("rOR", s, h), ("rR2", n % 2)], writes=[("rOR", s, h)])
            P.dve(lambda en, o=OF[s][:], a=OR[s][:].rearrange("p h e -> p (h e)"), b=RG[s][:]: en.tensor_tensor(o, a, b, ALU.mult),
                  reads=[("rOR", s, h) for h in range(4)] + [("rRG", s)], writes=["rOF"])
            for h in range(4):
                hs = slice(h * 128, (h + 1) * 128)
                P.pe(lambda en, o=PS[5][:, hs], i=OF[s][:, hs]: en.transpose(o, i, self.ident32[:]),
                     reads=["rOF", "ident32"], writes=[("ps", 5)])
            P.act(lambda en, o=OT[:, 0:4, tk], i=PS[5][:].rearrange("p (q c) -> p q c", q=4): en.copy(o, i),
                  reads=[("ps", 5)], writes=[("OT", n)])

        nt_r = NT if rstop >= 3 else 0
        if nt_r:
            front(0)
        for n in range(nt_r):
            if n + 1 < nt_r:
                front(n + 1)
            back(n)
        P.pop_scope()
        P.pop_scope()

    def gdn(self, l, win_d, cw_d, alog_d, dtb_d, gnw_d, OT):
        P = self.P
        cfg = self.cfg
        PS, PSB, XT = self.PS, self.PSB, self.XT
        cst = self.cst_d
        I32 = self.ident32
        P.push_scope()
        CN = {}
        for k in ("g_tri", "g_su", "g_blk", "g_negS", "g_negI", "g_selA", "g_selB"):
            CN[k] = P.sb(k, [128, 128], F32)
            P.dma(CN[k][:], cst[k], writes=[k], key="gc_" + k)
        TRI, SU, BLK, NEGS, NEGI, SELA, SELB = (CN[k] for k in ("g_tri", "g_su", "g_blk", "g_negS", "g_negI", "g_selA", "g_selB"))
        CW = P.sb("g_CW", [128, 12, 4], F32)
        ALOG = P.sb("g_ALOG", [128, 4], F32)
        DTB = P.sb("g_DTB", [128, 4], F32)
        GNW = P.sb("g_GNW", [128, 128], F32)
        P.dma(CW[:], cw_d[l], writes=["g_CW"], key="gc_cw")
        P.dma(ALOG[:], alog_d[l], writes=["g_sc"], key="gc_al")
        P.dma(DTB[:], dtb_d[l], writes=["g_sc2"], key="gc_dt")
        P.dma(GNW[:], gnw_d[l], writes=["g_GNW"], key="gc_gn")
        WAB = P.sb("g_WAB", [128, KC, 8], BF16)
        P.dma(WAB[:], win_d[l][:, 4096:4104].rearrange("(kc p) c -> p kc c", p=128), writes=["g_WAB"], key="gwab", eng="pool")
        names = ["AB8", "Z", "Gt", "BETA", "NBETA", "GC", "GL", "EG", "EKD", "BEG"]
        SC = {}
        SC["AB8"] = P.sb("g_AB8", [128, NT, 8], F32)
        for k in names[1:]:
            SC[k] = P.sb("g_" + k, [128, NT, 4], F32)
        CD = P.sb("g_CD", [128, 2, NT, 4], F32)
        NEGA = P.sb("g_NEGA", [128, 4], F32)
        S_ = ["g_scal"]
        for tt in range(NT):
            for kc in range(KC):
                P.pe(lambda en, o=PS[0][:, tt * 8:(tt + 1) * 8], a=XT[:, kc, tt * 128:(tt + 1) * 128], r=WAB[:, kc, :], st=(kc == 0), sp=(kc == KC - 1):
                     en.matmul(o, a, r, start=st, stop=sp), reads=["g_WAB", ("XT", tt, 0), ("XT", tt, 1)], writes=[("ps", 0)])
        P.dve(lambda en: en.tensor_copy(SC["AB8"][:], PS[0][:, 0:128].rearrange("p (t c) -> p t c", c=8)), reads=[("ps", 0)], writes=S_)
        bc4 = lambda a: a[:].unsqueeze(1).to_broadcast([128, NT, 4])
        P.dve(lambda en: en.tensor_tensor(SC["Z"][:], SC["AB8"][:, :, 0:4], bc4(DTB), ALU.add), reads=S_ + ["g_sc2"], writes=S_)
        P.act(lambda en: en.activation(SC["Z"][:], SC["Z"][:], AF.Exp), reads=S_, writes=S_)
        P.act(lambda en: en.activation(SC["Z"][:], SC["Z"][:], AF.Ln, bias=1.0), reads=S_, writes=S_)
        P.act(lambda en: en.activation(NEGA[:], ALOG[:], AF.Exp), reads=["g_sc"], writes=S_)
        P.dve(lambda en: en.scalar_tensor_tensor(SC["Gt"][:], SC["Z"][:], -1.0, bc4(NEGA), ALU.mult, ALU.mult), reads=S_, writes=S_)
        P.act(lambda en: en.activation(SC["BETA"][:], SC["AB8"][:, :, 4:8], AF.Sigmoid), reads=S_, writes=S_)
        P.dve(lambda en: en.tensor_scalar(SC["NBETA"][:], SC["BETA"][:], -1.0, None, ALU.mult), reads=S_, writes=S_)
        gflat = SC["Gt"][:].rearrange("p t c -> p (t c)")
        for (lhs, name, off) in ((TRI, "g_tri", 0), (BLK, "g_blk", 64), (SELA, "g_selA", 128), (SELB, "g_selB", 192)):
            P.pe(lambda en, o=PS[1][:, off:off + 64], a=lhs[:]: en.matmul(o, a, gflat, start=True, stop=True), reads=S_ + [name], writes=[("ps", 1)])
        v64 = lambda off: PS[1][:, off:off + 64].rearrange("p (t c) -> p t c", c=4)
        P.dve(lambda en: en.tensor_copy(SC["GC"][:], v64(0)), reads=[("ps", 1)], writes=S_)
        P.dve(lambda en: en.tensor_copy(SC["GL"][:], v64(64)), reads=[("ps", 1)], writes=S_)
        P.dve(lambda en: en.tensor_copy(CD[:, 0, :, :], v64(128)), reads=[("ps", 1)], writes=S_)
        P.dve(lambda en: en.tensor_copy(CD[:, 1, :, :], v64(192)), reads=[("ps", 1)], writes=S_)
        P.act(lambda en: en.activation(CD[:], CD[:], AF.Exp), reads=S_, writes=S_)
        P.act(lambda en: en.activation(SC["EG"][:], SC["GC"][:], AF.Exp), reads=S_, writes=S_)
        P.dve(lambda en: en.tensor_tensor(SC["EKD"][:], SC["GL"][:], SC["GC"][:], ALU.subtract), reads=S_, writes=S_)
        P.act(lambda en: en.activation(SC["EKD"][:], SC["EKD"][:], AF.Exp), reads=S_, writes=S_)
        P.dve(lambda en: en.tensor_tensor(SC["BEG"][:], SC["BETA"][:], SC["EG"][:], ALU.mult), reads=S_, writes=S_)
        P.barrier()
        gstop = cfg.get("gdn_stop", 99)

        def one_pass(hp):
            P.push_scope()
            QN = P.sb("g_QN", [128, 2, T], BF16)
            KN = P.sb("g_KN", [128, 2, T], BF16)
            KTOK = P.sb("g_KTOK", [128, NT, 256], BF16)
            VTOK = P.sb("g_VTOK", [128, NT, 256], BF16)
            P.push_scope()
            PRE = P.sb("g_PRE", [128, T + 3], F32)
            CV = P.sb("g_CV", [128, T], F32)
            VT = P.sb("g_VT", [128, T], BF16)
            RN = [P.sb("g_RN%d" % i, [128, 512], F32) for i in range(2)]
            WC = [P.sb("g_WC%d" % i, [128, KC, 128], BF16) for i in range(2)]
            P.dve(lambda en: en.memset(PRE[:, 0:3], 0.0), writes=["g_PRE0"])
            SSQB = (4, 5, 6, 0)
            items = []
            cnt = 0
            for kind in range(3):
                for hh in range(2):
                    items.append((kind, hh, kind * 4 + 2 * hp + hh, cnt % 2))
                    cnt += 1

            def stageA(it):
                kind, hh, f, s = it
                c0 = 2048 + f * 128
                P.dma(WC[s][:], win_d[l][:, c0:c0 + 128].rearrange("(kc p) c -> p kc c", p=128), writes=[("g_WC", s)], key="gwc%d" % s, eng="pool")
                for tb in range(4):
                    b = 2 + tb % 2
                    blk = slice(tb * 512, (tb + 1) * 512)
                    for kc in range(KC):
                        P.pe(lambda en, o=PS[b][:], a=WC[s][:, kc, :], r=XT[:, kc, blk], st=(kc == 0), sp=(kc == KC - 1):
                             en.matmul(o, a, r, start=st, stop=sp),
                             reads=[("g_WC", s)] + [("XT", tt, q) for tt in range(tb * 4, tb * 4 + 4) for q in range(2)], writes=[("ps", b)])
                    P.act(lambda en, o=PRE[:, 3 + tb * 512: 3 + (tb + 1) * 512], i=PS[b][:]: en.copy(o, i), reads=[("ps", b)], writes=[("g_PRE", tb)])

            def stageB(it):
                kind, hh, f, s = it
                for tb in range(4):
                    blk = slice(tb * 512, (tb + 1) * 512)
                    pre_r = [("g_PRE", tb), "g_PRE0", "g_CW"] + ([("g_PRE", tb - 1)] if tb > 0 else [])
                    P.dve(lambda en, sc=CW[:, f, 3:4], o=CV[:, blk], i=PRE[:, 3 + tb * 512: 3 + (tb + 1) * 512]: en.tensor_scalar(o, i, sc, None, ALU.mult),
                          reads=pre_r, writes=[("g_CV", tb)])
                    for k in (2, 1, 0):
                        P.dve(lambda en, sc=CW[:, f, k:k + 1], o=CV[:, blk], i=PRE[:, k + tb * 512: k + (tb + 1) * 512]: en.scalar_tensor_tensor(o, i, sc, o, ALU.mult, ALU.add),
                              reads=pre_r + [("g_CV", tb)], writes=[("g_CV", tb)])
                    if kind == 2:
                        P.act(lambda en, o=VT[:, blk], i=CV[:, blk]: en.activation(o, i, AF.Silu), reads=[("g_CV", tb)], writes=[("g_VT", tb)])
                    else:
                        P.act(lambda en, o=CV[:, blk]: en.activation(o, o, AF.Silu), reads=[("g_CV", tb)], writes=[("g_CV", tb)])
                        P.act(lambda en, o=VT[:, blk], i=CV[:, blk]: en.activation(o, i, AF.Square), reads=[("g_CV", tb)], writes=[("g_VT", tb)])
                        b2_ = SSQB[tb]
                        P.pe(lambda en, o=PS[b2_][:], r=VT[:, blk]: en.matmul(o, self.onesb[:], r, start=True, stop=True),
                             reads=[("g_VT", tb), "onesb"], writes=[("ps", b2_)])
                if kind == 2:
                    tposes(VTOK, lambda tt: VT[:, tt * 128:(tt + 1) * 128], lambda tt: [("g_VT", tt // 4)], kind, hh)

            def stageC(it):
                kind, hh, f, s = it
                if kind == 2:
                    return
                dst = QN if kind == 0 else KN
                scale = (128.0 ** -0.5) if kind == 0 else 1.0
                for tb in range(4):
                    blk = slice(tb * 512, (tb + 1) * 512)
                    b2_ = SSQB[tb]
                    P.act(lambda en, o=RN[tb % 2][:], i=PS[b2_][:]: en.activation(o, i, AF.Ln, bias=NORM_EPS), reads=[("ps", b2_)], writes=[("g_RN", tb % 2)])
                    P.act(lambda en, o=RN[tb % 2][:]: en.activation(o, o, AF.Exp, scale=-0.5), reads=[("g_RN", tb % 2)], writes=[("g_RN", tb % 2)])
                    P.dve(lambda en, o=dst[:, hh, blk], a=CV[:, blk], r=RN[tb % 2][:], sc=scale:
                          en.scalar_tensor_tensor(o, a, sc, r, ALU.mult, ALU.mult),
                          reads=[("g_CV", tb), ("g_RN", tb % 2)], writes=[("g_N", kind, hh, tb)])
                if kind == 1:
                    tposes(KTOK, lambda tt: KN[:, hh, tt * 128:(tt + 1) * 128], lambda tt: [("g_N", 1, hh, tt // 4)], kind, hh)

            def tposes(dstT, srcview, rtok_of, kind, hh):
                for rnd in range(2):
                    for q in range(8):
                        tt = rnd * 8 + q
                        P.pe(lambda en, o=PSB[:, q * 128:(q + 1) * 128], i=srcview(tt): en.transpose(o, i, self.identb[:]),
                             reads=rtok_of(tt) + ["identb"], writes=["psb"])
                    P.act(lambda en, o=dstT[:, rnd * 8:(rnd + 1) * 8, hh * 128:(hh + 1) * 128], i=PSB[:].rearrange("p (q c) -> p q c", q=8): en.copy(o, i),
                          reads=["psb"], writes=[("g_TOK", kind, hh, rnd)])

            stageA(items[0])
            for i, it in enumerate(items):
                stageB(it)
                if i + 1 < len(items):
                    stageA(items[i + 1])
                stageC(it)
            P.pop_scope()
            P.push_scope()
            WGG = P.sb("g_WGG", [128, KC, 256], BF16)
            cg = 3584 + 2 * hp * 128
            P.dma(WGG[:], win_d[l][:, cg:cg + 256].rearrange("(kc p) c -> p kc c", p=128), writes=["g_WGG"], key="gwgg", eng="pool")
            f32t = lambda nm: P.sb(nm, [128, 128], F32)
            b16t = lambda nm: P.sb(nm, [128, 128], BF16)
            TT = []
            for hh in range(2):
                d = {}
                for nm in ("R", "R2", "Dm", "DTm", "Nm", "Mm", "Ma", "Mb", "Pa", "Pb", "VNf", "TMPo"):
                    d[nm] = f32t("g_%s%d" % (nm, hh))
                d["Na"], d["Nb"] = d["R"], d["R2"]
                for nm in ("Pu", "Pw"):
                    d[nm] = b16t("g_%s%d" % (nm, hh))
                for nm in ("VN", "VN2"):
                    d[nm] = [b16t("g_%s%d_%d" % (nm, hh, i)) for i in range(2)]
                d["QKm"] = [b16t("g_QKm%d_%d" % (hh, i)) for i in range(2)]
                d["WT"] = [b16t("g_WT%d_%d" % (hh, i)) for i in range(2)]
                d["U"] = [f32t("g_U%d_%d" % (hh, i)) for i in range(2)]
                d["S32"] = f32t("g_S32_%d" % hh)
                d["SB"] = b16t("g_SB_%d" % hh)
                TT.append(d)
            GO = [P.sb("g_GO%d" % i, [128, 256], F32) for i in range(2)]
            GG = P.sb("g_GG", [128, 256], F32)
            OFg = P.sb("g_OFg", [128, 256], F32)
            SS = P.sb("g_SS", [128, NT, 2], F32)
            RS = P.sb("g_RS", [128, NT, 2], F32)
            P.dve(lambda en: en.memset(SS[:], 0.0), writes=["g_SS"])
            for hh in range(2):
                P.dve(lambda en, t=TT[hh]["S32"]: en.memset(t[:], 0.0), writes=[("gS32", hh)])
                P.dve(lambda en, t=TT[hh]["SB"]: en.memset(t[:], 0.0), writes=[("gSB", hh)])
                for nm in ("VN", "VN2"):
                    for i in range(2):
                        P.pool(lambda en, t=TT[hh][nm][i]: en.memset(t[:], 0.0), writes=[("g2", nm, hh, i)])

            def prep(n, hh):
                h = 2 * hp + hh
                d = TT[hh]
                tk = slice(n * 128, (n + 1) * 128)
                bk = PS[hh]
                bt = ("ps", hh)
                bB = PS[2 + hh]
                btB = ("ps", 2 + hh)
                tg = lambda nm: ("g2", nm, hh)
                rq = [("g_N", 0, hh, n // 4)]
                rk = [("g_N", 1, hh, n // 4)]
                gcol = SC["Gt"][:, n, h:h + 1]
                P.dve(lambda en: en.tensor_scalar(d["R"][:], SU[:], gcol, None, ALU.mult), reads=S_ + ["g_su"], writes=[tg("R")])
                P.dve(lambda en: en.tensor_scalar(d["R2"][:], TRI[:], gcol, None, ALU.mult), reads=S_ + ["g_tri"], writes=[tg("R2")])
                P.pe(lambda en: en.matmul(bB[:, 256:384], KN[:, hh, tk], KN[:, hh, tk], start=True, stop=True), reads=rk, writes=[btB])
                P.pe(lambda en: en.matmul(bB[:, 384:512], KN[:, hh, tk], QN[:, hh, tk], start=True, stop=True), reads=rk + rq, writes=[btB])
                yield
                P.pe(lambda en: en.matmul(bk[:, 256:384], TRI[:], d["R"][:], start=True, stop=True), reads=[tg("R"), "g_tri"], writes=[bt])
                P.pe(lambda en: en.matmul(bk[:, 384:512], SU[:], d["R2"][:], start=True, stop=True), reads=[tg("R2"), "g_su"], writes=[bt])
                yield
                P.act(lambda en: en.activation(d["Dm"][:], bk[:, 256:384], AF.Exp), reads=[bt], writes=[tg("Dm")])
                P.act(lambda en: en.activation(d["DTm"][:], bk[:, 384:512], AF.Exp), reads=[bt], writes=[tg("DTm")])
                yield
                P.pool(lambda en: en.tensor_tensor(d["Dm"][:], d["Dm"][:], NEGS[:], ALU.mult), reads=[tg("Dm"), "g_negS"], writes=[tg("Dm")])
                P.pool(lambda en: en.tensor_tensor(d["DTm"][:], d["DTm"][:], NEGI[:], ALU.mult), reads=[tg("DTm"), "g_negI"], writes=[tg("DTm")])
                yield
                P.dve(lambda en: en.scalar_tensor_tensor(d["Nm"][:], bB[:, 256:384], SC["NBETA"][:, n, h:h + 1], d["Dm"][:], ALU.mult, ALU.mult),
                      reads=[btB, tg("Dm")] + S_, writes=[tg("Nm")])
                qkm = d["QKm"][n % 2]
                P.dve(lambda en: en.tensor_tensor(qkm[:], bB[:, 384:512], d["DTm"][:], ALU.mult), reads=[btB, tg("DTm")], writes=[("g2", "QKm", hh, n % 2)])
                yield
                P.pe(lambda en: en.transpose(bk[:, 0:128], d["Nm"][:], I32[:]), reads=[tg("Nm"), "ident32"], writes=[bt])
                yield
                P.act(lambda en: en.copy(d["Mm"][:], bk[:, 0:128]), reads=[bt], writes=[tg("Mm")])
                yield
                P.pool(lambda en: en.tensor_tensor(d["Pa"][:], d["Mm"][:], I32[:], ALU.add), reads=[tg("Mm"), "ident32"], writes=[tg("Pa")])
                Nk, Mk, Pk = d["Nm"], d["Mm"], d["Pa"]
                nN, nM, nP = ("Nm", "Mm", "Pa")
                for k in range(5):
                    Nn, nNn = (d["Na"], "R") if k % 2 == 0 else (d["Nb"], "R2")
                    Mn, nMn = (d["Ma"], "Ma") if k % 2 == 0 else (d["Mb"], "Mb")
                    Pn, nPn = (d["Pb"], "Pb") if k % 2 == 0 else (d["Pa"], "Pa")
                    P.pe(lambda en, a=Mk, r=Nk: en.matmul(bk[:, 0:128], a[:], r[:], start=True, stop=True), reads=[tg(nN), tg(nM)], writes=[bt])
                    if k < 4:
                        P.pe(lambda en, a=Nk, r=Mk: en.matmul(bB[:, 0:128], a[:], r[:], start=True, stop=True), reads=[tg(nN), tg(nM)], writes=[btB])
                    yield
                    P.act(lambda en, o=Nn: en.copy(o[:], bk[:, 0:128]), reads=[bt], writes=[tg(nNn)])
                    if k < 4:
                        P.dve(lambda en, o=Mn: en.tensor_copy(o[:], bB[:, 0:128]), reads=[btB], writes=[tg(nMn)])
                    yield
                    P.pe(lambda en, a=Nn, r=Pk: en.matmul(bB[:, 128:256], a[:], r[:], start=True, stop=True), reads=[tg(nP), tg(nNn)], writes=[btB])
                    yield
                    P.dve(lambda en, o=Pn, r=Pk: en.tensor_tensor(o[:], bB[:, 128:256], r[:], ALU.add), reads=[btB, tg(nP)], writes=[tg(nPn)])
                    yield
                    if k == 4:
                        P.act(lambda en, r=Pn: en.activation(d["Pu"][:], r[:], AF.Copy, scale=SC["BETA"][:, n, h:h + 1]), reads=[tg(nPn)] + S_, writes=[tg("Pu")])
                        P.act(lambda en, r=Pn: en.activation(d["Pw"][:], r[:], AF.Copy, scale=SC["BEG"][:, n, h:h + 1]), reads=[tg(nPn)] + S_, writes=[tg("Pw")])
                        yield
                    Nk, Mk, Pk = Nn, Mn, Pn
                    nN, nM, nP = nNn, nMn, nPn
                P.pe(lambda en: en.matmul(bB[:, 0:128], d["Pu"][:], VTOK[:, n, hh * 128:(hh + 1) * 128], start=True, stop=True),
                     reads=[tg("Pu"), ("g_TOK", 2, hh, n // 8)], writes=[btB])
                P.pe(lambda en: en.matmul(bk[:, 384:512], KTOK[:, n, hh * 128:(hh + 1) * 128], d["Pw"][:], start=True, stop=True),
                     reads=[tg("Pw"), ("g_TOK", 1, hh, n // 8)], writes=[bt])
                yield
                P.dve(lambda en: en.tensor_copy(d["U"][n % 2][:], bB[:, 0:128]), reads=[btB], writes=[("g2", "U", hh, n % 2)])
                P.act(lambda en: en.copy(d["WT"][n % 2][:], bk[:, 384:512]), reads=[bt], writes=[("g2", "WT", hh, n % 2)])
                yield

            def scan(n, hh):
                tk = slice(n * 128, (n + 1) * 128)
                h = 2 * hp + hh
                d = TT[hh]
                b4 = PS[4 + hh]
                bt4 = ("ps", 4 + hh)
                tg = lambda nm: ("g2", nm, hh)
                go = GO[n % 2]
                for cpar in range(2):
                    rows = slice(64 * cpar, 64 * cpar + 64)
                    P.pe(lambda en: en.matmul(b4[:, 0:128], d["WT"][n % 2][:], d["SB"][:], start=True, stop=True),
                         reads=[("g2", "WT", hh, n % 2), ("gSB", hh)], writes=[bt4])
                    P.pe(lambda en: en.matmul(b4[:, 128:256], QN[:, hh, tk], d["SB"][:], start=True, stop=True),
                         reads=[("g_N", 0, hh, n // 4), ("gSB", hh)], writes=[bt4])
                    yield
                    P.dve(lambda en, rows=rows: en.scalar_tensor_tensor(d["VNf"][rows, :], b4[rows, 0:128], -1.0, d["U"][n % 2][rows, :], ALU.mult, ALU.add),
                          reads=[bt4, ("g2", "U", hh, n % 2)], writes=[tg("VNf")])
                    P.dve(lambda en, rows=rows: en.tensor_scalar(d["TMPo"][rows, :], b4[rows, 128:256], SC["EG"][rows, n, h:h + 1], None, ALU.mult),
                          reads=[bt4] + S_, writes=[tg("TMPo")])
                    yield
                    P.act(lambda en, rows=rows, cpar=cpar: en.copy(d["VN"][cpar][rows, :], d["VNf"][rows, :]),
                          reads=[tg("VNf"), ("g2", "VN", hh, cpar)], writes=[("g2", "VN", hh, cpar)])
                    P.act(lambda en, rows=rows, cpar=cpar: en.activation(d["VN2"][cpar][rows, :], d["VNf"][rows, :], AF.Copy, scale=SC["EKD"][rows, n, h:h + 1]),
                          reads=[tg("VNf"), ("g2", "VN2", hh, cpar)] + S_, writes=[("g2", "VN2", hh, cpar)])
                    yield
                    P.pe(lambda en, cpar=cpar: en.matmul(b4[:, 256:384], d["QKm"][n % 2][:], d["VN"][cpar][:], start=True, stop=True),
                         reads=[("g2", "QKm", hh, n % 2), ("g2", "VN", hh, cpar)], writes=[bt4])
                    P.pe(lambda en, cpar=cpar: en.matmul(b4[:, 384:512], KTOK[:, n, hh * 128:(hh + 1) * 128], d["VN2"][cpar][:], start=True, stop=True),
                         reads=[("g_TOK", 1, hh, n // 8), ("g2", "VN2", hh, cpar)], writes=[bt4])
                    yield
                    P.dve(lambda en, cpar=cpar: en.scalar_tensor_tensor(d["S32"][:], d["S32"][:], CD[:, cpar, n, h:h + 1], b4[:, 384:512], ALU.mult, ALU.add),
                          reads=[bt4, ("gS32", hh)] + S_, writes=[("gS32", hh)])
                    P.dve(lambda en, rows=rows: en.tensor_tensor(go[rows, hh * 128:(hh + 1) * 128], d["TMPo"][rows, :], b4[rows, 256:384], ALU.add),
                          reads=[bt4, tg("TMPo")], writes=[("gGO", n % 2, hh)])
                    yield
                    P.act(lambda en: en.copy(d["SB"][:], d["S32"][:]), reads=[("gS32", hh)], writes=[("gSB", hh)])
                    yield

            def post(n):
                tk = slice(n * 128, (n + 1) * 128)
                b6 = PS[6]
                bt6 = ("ps", 6)
                go = GO[n % 2]
                for kc in range(KC):
                    P.pe(lambda en, kc=kc: en.matmul(b6[:, 0:256], XT[:, kc, tk], WGG[:, kc, :], start=(kc == 0), stop=(kc == KC - 1)),
                         reads=["g_WGG", ("XT", n, 0), ("XT", n, 1)], writes=[bt6])
                for hh in range(2):
                    P.act(lambda en, hh=hh: en.activation(OFg[:, hh * 128:(hh + 1) * 128], go[:, hh * 128:(hh + 1) * 128], AF.Square, accum_out=SS[:, n, hh:hh + 1]),
                          reads=[("gGO", n % 2, hh), "g_SS", ("gOF", hh)], writes=["g_SS", ("gOF", hh)])
                yield
                P.act(lambda en: en.activation(GG[:], b6[:, 0:256], AF.Silu), reads=[bt6], writes=["gGG"])
                P.act(lambda en: en.activation(RS[:, n, :], SS[:, n, :], AF.Sqrt, bias=NORM_EPS, scale=1.0 / 128.0), reads=["g_SS"], writes=["g_RS"])
                yield
                P.dve(lambda en: en.reciprocal(RS[:, n, :], RS[:, n, :]), reads=["g_RS"], writes=["g_RS"])
                yield
                for hh in range(2):
                    hs = slice(hh * 128, (hh + 1) * 128)
                    P.dve(lambda en, hh=hh, hs=hs: en.scalar_tensor_tensor(OFg[:, hs], go[:, hs], RS[:, n, hh:hh + 1], GNW[:], ALU.mult, ALU.mult),
                          reads=[("gGO", n % 2, hh), "g_RS", "g_GNW", ("gOF", hh)], writes=[("gOF", hh)])
                yield
                for hh in range(2):
                    hs = slice(hh * 128, (hh + 1) * 128)
                    P.pool(lambda en, hs=hs: en.tensor_tensor(OFg[:, hs], OFg[:, hs], GG[:, hs], ALU.mult), reads=[("gOF", hh), "gGG"], writes=[("gOF", hh)])
                yield
                for hh in range(2):
                    hs = slice(hh * 128, (hh + 1) * 128)
                    P.pe(lambda en, hh=hh, hs=hs: en.transpose(b6[:, 256 + hh * 128: 256 + (hh + 1) * 128], OFg[:, hs], I32[:]),
                         reads=[("gOF", hh), "ident32"], writes=[bt6])
                yield
                P.act(lambda en: en.copy(OT[:, 4 + 2 * hp: 6 + 2 * hp, tk], b6[:, 256:512].rearrange("p (q c) -> p q c", q=2)),
                      reads=[bt6], writes=[("OT", n)])
                yield

            nt_ = cfg.get("gdn_nt", NT) if gstop >= 3 else 0
            for it in range(nt_ + 2 if nt_ else 0):
                gens = []
                if it < nt_:
                    gens += [prep(it, 0), prep(it, 1)]
                if 1 <= it <= nt_ and gstop >= 4:
                    gens += [scan(it - 1, 0), scan(it - 1, 1)]
                if 2 <= it <= nt_ + 1 and gstop >= 5:
                    gens += [post(it - 2)]
                while gens:
                    alive = []
                    for g in gens:
                        try:
                            next(g)
                            alive.append(g)
                        except StopIteration:
                            pass
                    gens = alive
            P.pop_scope()
            P.pop_scope()

        for hp in (cfg.get("gdn_passes", range(2)) if gstop >= 2 else []):
            one_pass(hp)
        P.pop_scope()


def ext_w_in(w_in):
    L = w_in.shape[0]
    q = w_in[:, :, 0:256]
    k = w_in[:, :, 256:512]

    def sw(a):
        a4 = a.reshape(L, D, 4, 2, 32)
        return a4[:, :, :, ::-1, :].reshape(L, D, 256)
    parts = [q, sw(q), k, sw(k), w_in[:, :, 512:]]
    return np.ascontiguousarray(np.concatenate(parts, axis=2))


def prep_inputs(inp):
    f = lambda a: np.ascontiguousarray(np.asarray(a, dtype=np.float32))
    shared = {}
    shared["w_in"] = ext_w_in(f(inp["w_in"]))
    cw = f(inp["conv_w"])
    shared["conv_w"] = np.ascontiguousarray(cw.reshape(DEPTH, 4, 12, 128).transpose(0, 3, 2, 1))
    shared["a_log"] = np.ascontiguousarray(np.broadcast_to(f(inp["a_log"])[:, None, :], (DEPTH, 128, 4)))
    shared["dt_bias"] = np.ascontiguousarray(np.broadcast_to(f(inp["dt_bias"])[:, None, :], (DEPTH, 128, 4)))
    shared["gdn_norm_w"] = np.ascontiguousarray(np.broadcast_to(f(inp["gdn_norm_w"])[:, None, :], (DEPTH, 128, 128)))
    shared["w_o"] = f(inp["w_o"])
    for k in ("ln1_g", "ln1_b", "ln2_g", "ln2_b"):
        a = f(inp[k])
        shared[k] = np.ascontiguousarray(np.broadcast_to(a[:, None, :], (DEPTH, 128, D)))
        shared[k + "_c"] = np.ascontiguousarray(a.reshape(DEPTH, KC, 128).transpose(0, 2, 1))
    shared["ffn_w_gate"] = f(inp["ffn_w_gate"])[0]
    shared["ffn_w_up"] = f(inp["ffn_w_up"])[0]
    shared["ffn_w_down"] = f(inp["ffn_w_down"])[0]
    shared["router_w"] = f(inp["router_w"])[0]
    shared["moe_w_gate"] = f(inp["moe_w_gate"])[0]
    shared["moe_w_up"] = f(inp["moe_w_up"])[0]
    shared["moe_w_down"] = f(inp["moe_w_down"])[0]
    for k, v in make_consts().items():
        shared["c_" + k] = v
    return shared


_CACHE = {}


def run(inp, cfg, cores=8):
    shared = prep_inputs(inp)
    x = np.ascontiguousarray(np.asarray(inp["x"], dtype=np.float32))
    b = Builder(cfg)
    nc = b.build()
    in_maps = []
    for c in range(cores):
        m = dict(shared)
        m["x"] = x[c]
        in_maps.append(m)
    res = run_bass_kernel_spmd(nc, in_maps, core_ids=list(range(cores)))
    return res, b


def kernel(**inputs):
    res, _ = run(inputs, {}, cores=8)
    out = np.stack([r["out"] for r in res.results], axis=0)
    return out.astype(np.float32)
```

```python
import math
import numpy as np
import ml_dtypes
import concourse.bass as bass
import concourse.mybir as mybir
from concourse.bass_utils import run_bass_kernel_spmd
from contextlib import ExitStack

F32 = mybir.dt.float32
BF16 = mybir.dt.bfloat16
AF = mybir.ActivationFunctionType
ALU = mybir.AluOpType
AX = mybir.AxisListType

T = 2048
D = 1024
NT = 16
KC = 8
DEPTH = 2
ALPHA = (2 * DEPTH) ** 0.25
INVA = 1.0 / ALPHA
LN_EPS = 1e-5
LN_EPS_S = LN_EPS / (ALPHA * ALPHA)
NORM_EPS = 1e-6
D_FF = 2816
D_FFE = 3584
NE = 8
WEXT = 4104


class Prog:
    ENGS = ("pe", "act", "dve", "pool", "sp")

    def __init__(self, nc, same_engine_sync=True):
        self.nc = nc
        self.es = ExitStack()
        self.ops = []
        self.tok = {}
        self.same_engine_sync = same_engine_sync
        self.base_deps = set()
        self.last_eng = {}
        self.last_key = {}
        self.scopes = []

    def sb(self, name, shape, dt):
        es = self.scopes[-1] if self.scopes else self.es
        self.uid = getattr(self, "uid", 0) + 1
        return es.enter_context(self.nc.sbuf_tensor("%s_u%d" % (name, self.uid), list(shape), dt))

    def push_scope(self):
        self.barrier()
        self.scopes.append(ExitStack())

    def pop_scope(self):
        self.barrier()
        self.scopes.pop().close()

    def ps(self, name, shape, dt=F32):
        return self.es.enter_context(self.nc.psum_tensor(name, list(shape), dt))

    def op(self, eng, fn, reads=(), writes=(), dma_key=None):
        idx = len(self.ops)
        deps = set(self.base_deps)
        for t in reads:
            e = self.tok.get(t)
            if e is not None and e[0] is not None:
                deps.add(e[0])
        for t in writes:
            e = self.tok.get(t)
            if e is not None:
                if e[0] is not None:
                    deps.add(e[0])
                deps.update(e[1])
        for t in reads:
            e = self.tok.setdefault(t, [None, []])
            e[1].append(idx)
        for t in writes:
            self.tok[t] = [idx, []]
        deps.discard(idx)
        self.ops.append(dict(eng=eng, fn=fn, deps=deps, dma_key=dma_key))
        self.last_eng[eng] = idx
        if dma_key is not None:
            self.last_key[dma_key] = idx
        return idx

    def barrier(self):
        self.base_deps = set(self.last_eng.values()) | set(self.last_key.values())

    def pe(self, fn, reads=(), writes=()):
        return self.op("pe", fn, reads, writes)

    def act(self, fn, reads=(), writes=()):
        return self.op("act", fn, reads, writes)

    def dve(self, fn, reads=(), writes=()):
        return self.op("dve", fn, reads, writes)

    def pool(self, fn, reads=(), writes=()):
        return self.op("pool", fn, reads, writes)

    def dma(self, out, in_, reads=(), writes=(), key=None, eng="sp"):
        assert key is not None
        return self.op(eng, lambda e: e.dma_start(out=out, in_=in_), reads, writes, dma_key=key)

    def emit(self):
        nc = self.nc
        ops = self.ops
        n = len(ops)
        needed = [False] * n
        for i, o in enumerate(ops):
            keep = set()
            for d in o["deps"]:
                od = ops[d]
                if od["dma_key"] is None and od["eng"] == o["eng"]:
                    if o["eng"] == "pe" or not self.same_engine_sync:
                        continue
                keep.add(d)
            o["deps"] = keep
            for d in keep:
                needed[d] = True
        eng_sem = {}
        key_sem = {}
        eng_cnt = {e: 0 for e in self.ENGS}
        key_cnt = {}
        for i, o in enumerate(ops):
            if o["dma_key"] is not None:
                k = o["dma_key"]
                if k not in key_sem:
                    key_sem[k] = self.es.enter_context(nc.semaphore("d_" + str(k)))
                    key_cnt[k] = 0
                key_cnt[k] += 16
                o["sig"] = (key_sem[k], key_cnt[k])
                o["inc"] = (key_sem[k], 16)
            elif needed[i]:
                e = o["eng"]
                if e not in eng_sem:
                    eng_sem[e] = self.es.enter_context(nc.semaphore("e_" + e))
                eng_cnt[e] += 1
                o["sig"] = (eng_sem[e], eng_cnt[e])
                o["inc"] = (eng_sem[e], 1)
            else:
                o["sig"] = None
                o["inc"] = None
        per_eng = {e: [] for e in self.ENGS}
        for i, o in enumerate(ops):
            per_eng[o["eng"]].append(i)
        self.stats = {e: len(v) for e, v in per_eng.items()}
        self.stats["sems"] = len(key_sem) + len(eng_sem)

        def emit_engine(eng_name, engine):
            waited = {}
            for i in per_eng[eng_name]:
                o = ops[i]
                w = {}
                for d in o["deps"]:
                    sem, val = ops[d]["sig"]
                    key = id(sem)
                    if key not in w or w[key][1] < val:
                        w[key] = (sem, val)
                for key, (sem, val) in w.items():
                    if waited.get(key, 0) >= val:
                        continue
                    engine.wait_ge(sem, val)
                    waited[key] = val
                ins = o["fn"](engine)
                if o["inc"] is not None:
                    ins.then_inc(o["inc"][0], o["inc"][1])
            last = {}
            for i in per_eng[eng_name]:
                o = ops[i]
                if o["dma_key"] is not None:
                    sem, val = o["sig"]
                    last[id(sem)] = (sem, max(val, last.get(id(sem), (None, 0))[1]))
            for key, (sem, val) in last.items():
                if waited.get(key, 0) < val:
                    engine.wait_ge(sem, val)

        with nc.Block() as block:
            if per_eng["sp"]:
                @block.sync
                def _(e):
                    emit_engine("sp", e)
            if per_eng["pe"]:
                @block.tensor
                def _(e):
                    emit_engine("pe", e)
            if per_eng["act"]:
                @block.scalar
                def _(e):
                    emit_engine("act", e)
            if per_eng["dve"]:
                @block.vector
                def _(e):
                    emit_engine("dve", e)
            if per_eng["pool"]:
                @block.gpsimd
                def _(e):
                    emit_engine("pool", e)
        self.es.close()


def make_consts():
    c = {}
    c["ident32"] = np.eye(128, dtype=np.float32)
    c["ones32"] = np.ones((128, 128), np.float32)
    inv = (10000.0 ** (-np.arange(0, 64, 2, dtype=np.float32) / np.float32(64))).astype(np.float32)
    pos = np.arange(T, dtype=np.float32)
    ang = (pos[:, None] * inv[None, :]).astype(np.float32)
    cos = np.cos(ang.astype(np.float64)).astype(np.float32).T
    sin = np.sin(ang.astype(np.float64)).astype(np.float32).T
    C = np.zeros((128, T), np.float32)
    S = np.zeros((128, T), np.float32)
    for p in range(128):
        C[p] = cos[p % 32]
        S[p] = sin[p % 32] * (-1.0 if (p % 64) < 32 else 1.0)
    c["ropeC"] = C
    c["ropeS"] = S
    gam = 1.0 - 2.0 ** (-5.0 - np.arange(4, dtype=np.float64))
    idx = np.arange(128, dtype=np.float64)
    maskT = np.zeros((128, 4, 128), np.float32)
    for h in range(4):
        dlt = idx[None, :] - idx[:, None]
        m = np.where(dlt >= 0, gam[h] ** np.maximum(dlt, 0), 0.0) * 0.125
        maskT[:, h, :] = m
    c["r_maskT"] = maskT
    rcol = np.zeros((128, 12), np.float32)
    for h in range(4):
        rcol[:, h] = gam[h] ** (idx + 1.0)
        rcol[:, 4 + h] = gam[h] ** (127.0 - idx) * 0.125
    for j in range(2):
        rcol[:64, 8 + j] = gam[2 * j] ** 128.0
        rcol[64:, 8 + j] = gam[2 * j + 1] ** 128.0
    c["r_col"] = rcol
    t = np.arange(128)
    same = (t[:, None] // 64) == (t[None, :] // 64)
    tri = (same & (t[:, None] <= t[None, :])).astype(np.float32)
    su = (same & (t[:, None] > t[None, :])).astype(np.float32)
    c["g_tri"] = tri
    c["g_su"] = su
    c["g_blk"] = same.astype(np.float32)
    NEG = -30000.0
    c["g_negS"] = np.where(same & (t[None, :] < t[:, None]), 1.0, 0.0).astype(np.float32)
    c["g_negI"] = np.where(same & (t[None, :] >= t[:, None]), 1.0, 0.0).astype(np.float32)
    selA = np.zeros((128, 128), np.float32); selA[:64, :] = 1.0
    selB = np.zeros((128, 128), np.float32); selB[64:, :] = 1.0
    c["g_selA"] = selA
    c["g_selB"] = selB
    return c


CONST_NAMES = ["ident32", "ones32", "ropeC", "ropeS", "r_maskT", "r_col", "g_tri", "g_su",
               "g_blk", "g_negS", "g_negI", "g_selA", "g_selB"]


class Builder:
    def __init__(self, cfg):
        self.cfg = cfg
        nc = bass.Bass("TRN2", target_bir_lowering=False)
        self.nc = nc
        self.P = Prog(nc, same_engine_sync=cfg.get("ses", True))
        self.dram = {}

    def din(self, name, shape, dt=F32):
        t = self.nc.dram_tensor(name, list(shape), dt, kind="ExternalInput").ap()
        self.dram[name] = t
        return t

    def dout(self, name, shape, dt=F32):
        t = self.nc.dram_tensor(name, list(shape), dt, kind="ExternalOutput").ap()
        self.dram[name] = t
        return t

    def build(self):
        cfg = self.cfg
        P = self.P
        nc = self.nc
        layers = cfg.get("layers", [0, 1])
        x_d = self.din("x", [T, D])
        win_d = self.din("w_in", [DEPTH, D, WEXT])
        cw_d = self.din("conv_w", [DEPTH, 128, 12, 4])
        alog_d = self.din("a_log", [DEPTH, 128, 4])
        dtb_d = self.din("dt_bias", [DEPTH, 128, 4])
        gnw_d = self.din("gdn_norm_w", [DEPTH, 128, 128])
        wo_d = self.din("w_o", [DEPTH, D, D])
        lnp_d = {k: self.din(k, [DEPTH, 128, D]) for k in ("ln1_g", "ln1_b", "ln2_g", "ln2_b")}
        lnc_d = {k: self.din(k + "_c", [DEPTH, 128, KC]) for k in ("ln1_g", "ln1_b", "ln2_g", "ln2_b")}
        fwg_d = self.din("ffn_w_gate", [D, D_FF])
        fwu_d = self.din("ffn_w_up", [D, D_FF])
        fwd_d = self.din("ffn_w_down", [D_FF, D])
        rw_d = self.din("router_w", [D, NE])
        mwg_d = self.din("moe_w_gate", [NE, D, D_FFE])
        mwu_d = self.din("moe_w_up", [NE, D, D_FFE])
        mwd_d = self.din("moe_w_down", [NE, D_FFE, D])
        cst_d = {}
        cshapes = {"ident32": [128, 128], "ones32": [128, 128], "ropeC": [128, T], "ropeS": [128, T],
                   "r_maskT": [128, 4, 128], "r_col": [128, 12], "g_tri": [128, 128], "g_su": [128, 128],
                   "g_blk": [128, 128], "g_negS": [128, 128], "g_negI": [128, 128],
                   "g_selA": [128, 128], "g_selB": [128, 128]}
        for k in CONST_NAMES:
            cst_d[k] = self.din("c_" + k, cshapes[k])
        out_d = self.dout("out", [T, D])
        self.dbg_d = None
        if cfg.get("dbg"):
            self.dbg_d = self.dout("dbg", cfg["dbg_shape"])

        self.X = P.sb("X", [128, NT, D], F32)
        self.XT = P.sb("XT", [128, KC, T], BF16)
        self.ident32 = P.sb("ident32", [128, 128], F32)
        self.identb = P.sb("identb", [128, 128], BF16)
        self.ones32 = P.sb("ones32", [128, 128], F32)
        self.small = P.sb("small", [128, NT, 16], F32)
        self.CMB = P.sb("CMB", [128, NT, NE], F32)
        self.PS = [P.ps("ps%d" % k, [128, 512], F32) for k in range(7)]
        self.PSB = P.ps("psb", [128, 1024], BF16)

        P.dma(self.ident32[:], cst_d["ident32"], writes=["ident32"], key="c0")
        P.dma(self.ones32[:], cst_d["ones32"], writes=["ones32"], key="c1")
        P.dma(self.identb[:], cst_d["ident32"], writes=["identb"], key="c2", eng="pool")
        self.onesb = P.sb("onesb", [128, 128], BF16)
        P.dma(self.onesb[:], cst_d["ones32"], writes=["onesb"], key="c3", eng="pool")
        self.cst_d = cst_d
        xv = x_d.rearrange("(tt p) d -> p tt d", p=128)
        for q in range(4):
            P.dma(self.X[:, q * 4:(q + 1) * 4, :], xv[:, q * 4:(q + 1) * 4, :],
                  writes=[("X", tt) for tt in range(q * 4, q * 4 + 4)], key="xin%d" % q)
        for tt in range(NT):
            self.make_xt(tt)

        for l in layers:
            if cfg.get("mixer", True):
                self.mixer(l, win_d, cw_d, alog_d, dtb_d, gnw_d, wo_d)
            if cfg.get("mixer", True) or cfg.get("ln1"):
                self.layer_norm(l, "ln1", lnp_d, lnc_d, route=(l == 1 and cfg.get("ffn", True)), rw_d=rw_d)
            if cfg.get("ln_only"):
                self.layer_norm(l, "ln2", lnp_d, lnc_d)
            if cfg.get("ffn", True):
                if l == 0:
                    self.ffn(1, D_FF, lambda e: fwg_d, lambda e: fwu_d, lambda e: fwd_d, None, "f0")
                else:
                    self.ffn(NE, D_FFE, lambda e: mwg_d[e], lambda e: mwu_d[e], lambda e: mwd_d[e], self.CMB, "f1")
                if not cfg.get("no_ln2"):
                    self.layer_norm(l, "ln2", lnp_d, lnc_d, need_xt=(l != layers[-1]))

        ov = out_d.rearrange("(tt p) d -> p tt d", p=128)
        for q in range(4):
            P.dma(ov[:, q * 4:(q + 1) * 4, :], self.X[:, q * 4:(q + 1) * 4, :],
                  reads=[("X", tt) for tt in range(q * 4, q * 4 + 4)], key="xout%d" % q)
        P.emit()
        return nc

    def make_xt(self, tt, xt32=None):
        P = self.P
        X, XT = self.X, self.XT
        for half in range(2):
            bi = 5 + ((tt * 2 + half) % 2)
            bank = self.PS[bi]
            btok = ("ps", bi)
            for q in range(4):
                kc = half * 4 + q
                P.pe(lambda e, o=bank[:, q * 128:(q + 1) * 128], i=X[:, tt, kc * 128:(kc + 1) * 128]:
                     e.transpose(o, i, self.ident32[:]),
                     reads=[("X", tt), "ident32"], writes=[btok])
            outap = XT[:, half * 4:(half + 1) * 4, tt * 128:(tt + 1) * 128]
            inap = bank[:].rearrange("p (q c) -> p q c", q=4)
            xtok = ("XT", tt, half)
            if half == 0:
                P.act(lambda e, o=outap, i=inap: e.copy(o, i), reads=[btok], writes=[xtok])
            else:
                P.dve(lambda e, o=outap, i=inap: e.tensor_copy(o, i), reads=[btok], writes=[xtok])
            if xt32 is not None:
                o32 = xt32[:, half * 4:(half + 1) * 4, :]
                if half == 0:
                    P.act(lambda e, o=o32, i=inap: e.copy(o, i), reads=[btok], writes=[("xt32", id(xt32), half)])
                else:
                    P.dve(lambda e, o=o32, i=inap: e.tensor_copy(o, i), reads=[btok], writes=[("xt32", id(xt32), half)])

    def layer_norm(self, l, which, lnp_d, lnc_d, route=False, rw_d=None, need_xt=True):
        P = self.P
        X = self.X
        P.push_scope()
        LNG = P.sb("LNG", [128, D], F32)
        LNB = P.sb("LNB", [128, D], F32)
        P.dma(LNG[:], lnp_d[which + "_g"][l], writes=["LNG"], key="lng")
        P.dma(LNB[:], lnp_d[which + "_b"][l], writes=["LNB"], key="lnb")
        sm = self.small
        if route:
            RW = P.sb("RW", [128, KC, NE], F32)
            XT32 = [P.sb("XT32_%d" % i, [128, KC, 128], F32) for i in range(2)]
            LG = P.sb("LG", [128, NT, NE], F32)
            P.dma(RW[:], rw_d.rearrange("(kc p) e -> p kc e", p=128), writes=["RW"], key="rw")
        for tt in range(NT):
            st = ("sm", tt)
            xt = ("X", tt)
            for hf in range(2):
                P.dve(lambda e, o=sm[:, tt, hf * 6:(hf + 1) * 6], i=X[:, tt, hf * 512:(hf + 1) * 512]: e.bn_stats(o, i),
                      reads=[xt, st], writes=[st])
            P.dve(lambda e, o=sm[:, tt, 12:14], i=sm[:, tt, 0:12].rearrange("p (a b) -> p a b", a=2): e.bn_aggr(o, i),
                  reads=[st], writes=[st])
        allst = [("sm", tt) for tt in range(NT)]
        P.act(lambda e: e.activation(sm[:, :, 14], sm[:, :, 13], AF.Sqrt, bias=LN_EPS_S), reads=allst, writes=["sm_r"])
        P.dve(lambda e: e.reciprocal(sm[:, :, 14], sm[:, :, 14]), reads=["sm_r"], writes=["sm_r"])
        P.dve(lambda e: e.scalar_tensor_tensor(sm[:, :, 15], sm[:, :, 12], -1.0, sm[:, :, 14], ALU.mult, ALU.mult), reads=allst + ["sm_r"], writes=["sm_r"])
        for tt in range(NT):
            xt = ("X", tt)
            P.act(lambda e, o=X[:, tt, :], s=sm[:, tt, 14:15], b=sm[:, tt, 15:16]:
                  e.activation(o, o, AF.Identity, bias=b, scale=s), reads=[xt, "sm_r"], writes=[xt])
        for tt in range(NT):
            xt = ("X", tt)
            P.pool(lambda e, o=X[:, tt, :]: e.tensor_tensor(o, o, LNG[:], ALU.mult), reads=[xt, "LNG"], writes=[xt])
            P.pool(lambda e, o=X[:, tt, :]: e.tensor_tensor(o, o, LNB[:], ALU.add), reads=[xt, "LNB"], writes=[xt])
        for tt in range(NT if need_xt else 0):
            xt32 = XT32[tt % 2] if route else None
            self.make_xt(tt, xt32=xt32)
            if route and self.cfg.get("route_mm", True):
                bank = self.PS[4]
                for kc in range(KC):
                    P.pe(lambda e, o=bank[:, 0:NE], a=xt32[:, kc, :], b=RW[:, kc, :], s=(kc == 0), t=(kc == KC - 1):
                         e.matmul(o, a, b, start=s, stop=t),
                         reads=[("xt32", id(xt32), kc // 4), "RW"], writes=[("ps", 4)])
                P.dve(lambda e, o=LG[:, tt, :], i=bank[:, 0:NE]: e.tensor_copy(o, i), reads=[("ps", 4)], writes=["LG"])
        if route:
            if self.cfg.get("route_stop", 9) >= 2:
                self.route(LG)
            else:
                P.dve(lambda e: e.memset(self.CMB[:], INVA), reads=["LG"], writes=["CMB"])
        P.pop_scope()

    def route(self, LG):
        P = self.P
        m1 = P.sb("rt_m1", [128, NT], F32)
        m2 = P.sb("rt_m2", [128, NT], F32)
        eq1 = P.sb("rt_eq1", [128, NT, NE], F32)
        eq2 = P.sb("rt_eq2", [128, NT, NE], F32)
        L2 = P.sb("rt_L2", [128, NT, NE], F32)
        g1 = P.sb("rt_g1", [128, NT], F32)
        g2 = P.sb("rt_g2", [128, NT], F32)
        R = ["LG", "rt"]
        bc = lambda a: a[:].unsqueeze(2).to_broadcast([128, NT, NE])
        P.dve(lambda e: e.tensor_reduce(m1[:], LG[:], AX.X, ALU.max), reads=R, writes=["rt"])
        P.dve(lambda e: e.tensor_tensor(eq1[:], LG[:], bc(m1), ALU.is_equal), reads=R, writes=["rt"])
        P.dve(lambda e: e.scalar_tensor_tensor(L2[:], eq1[:], -1.0e30, LG[:], ALU.mult, ALU.add), reads=R, writes=["rt"])
        P.dve(lambda e: e.tensor_reduce(m2[:], L2[:], AX.X, ALU.max), reads=R, writes=["rt"])
        P.dve(lambda e: e.tensor_tensor(eq2[:], L2[:], bc(m2), ALU.is_equal), reads=R, writes=["rt"])
        P.dve(lambda e: e.tensor_tensor(g2[:], m2[:], m1[:], ALU.subtract), reads=R, writes=["rt"])
        P.act(lambda e: e.activation(g2[:], g2[:], AF.Exp), reads=R, writes=["rt"])
        P.dve(lambda e: e.tensor_scalar(g1[:], g2[:], 1.0, None, ALU.add), reads=R, writes=["rt"])
        P.dve(lambda e: e.reciprocal(g1[:], g1[:]), reads=R, writes=["rt"])
        P.dve(lambda e: e.tensor_tensor(g2[:], g2[:], g1[:], ALU.mult), reads=R, writes=["rt"])
        P.dve(lambda e: e.tensor_tensor(eq1[:], eq1[:], bc(g1), ALU.mult), reads=R, writes=["rt"])
        P.dve(lambda e: e.tensor_tensor(eq2[:], eq2[:], bc(g2), ALU.mult), reads=R, writes=["rt"])
        P.dve(lambda e: e.tensor_tensor(eq1[:], eq1[:], eq2[:], ALU.add), reads=R, writes=["rt"])
        P.dve(lambda e: e.tensor_scalar(self.CMB[:], eq1[:], INVA, None, ALU.mult), reads=R, writes=["CMB"])

    def ffn(self, n_exp, F, wg_of, wu_of, wd_of, cmb, tag):
        P = self.P
        X, XT = self.X, self.XT
        P.push_scope()
        G = 4
        nft = F // 128
        groups = []
        for e in range(n_exp):
            f0 = 0
            while f0 < nft:
                g = min(G, nft - f0)
                groups.append((e, f0, g))
                f0 += g
        WG = [P.sb("%s_WG%d" % (tag, i), [128, KC, G * 128], BF16) for i in range(2)]
        WU = [P.sb("%s_WU%d" % (tag, i), [128, KC, G * 128], BF16) for i in range(2)]
        WD = [P.sb("%s_WD%d" % (tag, i), [128, G, D], BF16) for i in range(2)]
        HT = [P.sb("%s_HT%d" % (tag, i), [128, G, T], BF16) for i in range(2)]
        SG = [P.sb("%s_SG%d" % (tag, i), [128, 512], F32) for i in range(2)]
        PS = self.PS

        def load(gi):
            load_gu(gi)
            load_d(gi)

        def load_gu(gi):
            e, f0, g = groups[gi]
            s = gi % 2
            c0, c1 = f0 * 128, (f0 + g) * 128
            P.dma(WG[s][:, :, 0:g * 128], wg_of(e)[:, c0:c1].rearrange("(kc p) f -> p kc f", p=128),
                  writes=[("WG", s)], key="%swg%d" % (tag, s), eng="pool")
            P.dma(WU[s][:, :, 0:g * 128], wu_of(e)[:, c0:c1].rearrange("(kc p) f -> p kc f", p=128),
                  writes=[("WU", s)], key="%swu%d" % (tag, s), eng="pool")

        def load_d(gi):
            e, f0, g = groups[gi]
            s = gi % 2
            c0, c1 = f0 * 128, (f0 + g) * 128
            P.dma(WD[s][:, 0:g, :], wd_of(e)[c0:c1, :].rearrange("(j p) m -> p j m", p=128),
                  writes=[("WD", s)], key="%swd%d" % (tag, s), eng="pool")

        def gu(gi):
            e, f0, g = groups[gi]
            s = gi % 2
            cnt = 0
            for j in range(g):
                for tb in range(4):
                    b = cnt % 2
                    cnt += 1
                    pg, pu = PS[b], PS[2 + b]
                    for kc in range(KC):
                        P.pe(lambda en, o=pg[:], a=WG[s][:, kc, j * 128:(j + 1) * 128], r=XT[:, kc, tb * 512:(tb + 1) * 512],
                             st=(kc == 0), sp=(kc == KC - 1): en.matmul(o, a, r, start=st, stop=sp),
                             reads=[("WG", s)] + [("XT", tt, hh) for tt in range(tb * 4, tb * 4 + 4) for hh in range(2)], writes=[("ps", b)])
                    for kc in range(KC):
                        P.pe(lambda en, o=pu[:], a=WU[s][:, kc, j * 128:(j + 1) * 128], r=XT[:, kc, tb * 512:(tb + 1) * 512],
                             st=(kc == 0), sp=(kc == KC - 1): en.matmul(o, a, r, start=st, stop=sp),
                             reads=[("WU", s)] + [("XT", tt, hh) for tt in range(tb * 4, tb * 4 + 4) for hh in range(2)], writes=[("ps", 2 + b)])
                    P.act(lambda en, o=SG[b][:], i=pg[:]: en.activation(o, i, AF.Silu),
                          reads=[("ps", b)], writes=[("SG", b)])
                    P.dve(lambda en, o=HT[s][:, j, tb * 512:(tb + 1) * 512], a=SG[b][:], c=pu[:]:
                          en.tensor_tensor(o, a, c, ALU.mult),
                          reads=[("SG", b), ("ps", 2 + b)], writes=[("HT", s, j, tb)])

        def down(gi):
            e, f0, g = groups[gi]
            s = gi % 2
            for tt in range(NT):
                for hf in range(2):
                    b = 4 + ((tt * 2 + hf) % 2)
                    pd = PS[b]
                    for j in range(g):
                        P.pe(lambda en, o=pd[:], a=HT[s][:, j, tt * 128:(tt + 1) * 128], r=WD[s][:, j, hf * 512:(hf + 1) * 512],
                             st=(j == 0), sp=(j == g - 1): en.matmul(o, a, r, start=st, stop=sp),
                             reads=[("HT", s, j, tt // 4), ("WD", s)], writes=[("ps", b)])
                    sc = INVA if cmb is None else cmb[:, tt, e:e + 1]
                    P.dve(lambda en, o=X[:, tt, hf * 512:(hf + 1) * 512], i=pd[:], sc=sc:
                          en.scalar_tensor_tensor(o, i, sc, o, ALU.mult, ALU.add),
                          reads=[("ps", b), ("X", tt), "CMB"], writes=[("X", tt)])

        ng = len(groups)
        ng = min(ng, self.cfg.get("ffn_ng", ng))
        mode = self.cfg.get("ffn_mode", 3)
        load(0)
        for gi in range(ng):
            if gi + 1 < ng:
                load_gu(gi + 1)
            if mode >= 1:
                gu(gi)
            if gi >= 1 and mode >= 3:
                down(gi - 1)
            if gi + 1 < ng:
                load_d(gi + 1)
        if mode >= 3:
            down(ng - 1)
        if self.cfg.get("dbg") == "ht":
            tmp = P.sb("dbg_tmp", [128, T], F32)
            sl = (ng - 1) % 2
            P.dve(lambda en: en.tensor_copy(tmp[:], HT[sl][:, self.cfg.get("dbg_j", 0), :]), reads=[("HT", sl, j, tb) for j in range(G) for tb in range(4)], writes=["dbgtmp"])
            P.dma(self.dbg_d, tmp[:], reads=["dbgtmp"], key="dbg")
        P.pop_scope()

    def mixer(self, l, win_d, cw_d, alog_d, dtb_d, gnw_d, wo_d):
        P = self.P
        cfg = self.cfg
        X = self.X
        P.push_scope()
        OT = P.sb("OT", [128, KC, T], BF16)
        self.OT = OT
        if cfg.get("retnet", True):
            self.retnet(l, win_d, OT)
        if cfg.get("gdn", True):
            self.gdn(l, win_d, cw_d, alog_d, dtb_d, gnw_d, OT)
        if cfg.get("dbg") == "ot":
            tmp = P.sb("dbg_tmp", [128, T], F32)
            for kc in cfg.get("dbg_kcs", range(KC)):
                P.dve(lambda en, kc=kc: en.tensor_copy(tmp[:], OT[:, kc, :]), reads=[("OT", n) for n in range(NT)] + ["dbgtmp"], writes=["dbgtmp"])
                P.dma(self.dbg_d[:, kc, :], tmp[:], reads=["dbgtmp"], key="dbg")
        if cfg.get("wo", True):
            P.push_scope()
            WO = P.sb("WO", [128, KC, D], BF16)
            P.dma(WO[:], wo_d[l].rearrange("(kc p) m -> p kc m", p=128), writes=["WO"], key="wo", eng="pool")
            for tt in range(NT):
                for hf in range(2):
                    b = (tt * 2 + hf) % 2
                    for kc in range(KC):
                        P.pe(lambda en, o=self.PS[b][:], a=OT[:, kc, tt * 128:(tt + 1) * 128], r=WO[:, kc, hf * 512:(hf + 1) * 512],
                             st=(kc == 0), sp=(kc == KC - 1): en.matmul(o, a, r, start=st, stop=sp),
                             reads=[("OT", tt), "WO"], writes=[("ps", b)])
                    P.dve(lambda en, o=X[:, tt, hf * 512:(hf + 1) * 512], i=self.PS[b][:]:
                          en.scalar_tensor_tensor(o, i, INVA, o, ALU.mult, ALU.add),
                          reads=[("ps", b), ("X", tt)], writes=[("X", tt)])
            P.pop_scope()
        P.pop_scope()

    def retnet(self, l, win_d, OT):
        P = self.P
        PS, PSB, XT = self.PS, self.PSB, self.XT
        cst = self.cst_d
        P.push_scope()
        QTz = [P.sb("r_QTz%d" % h, [128, T], BF16) for h in range(4)]
        for h in range(4):
            ob = 64 * (1 - h % 2)
            P.pool(lambda en, o=QTz[h][ob:ob + 64, :]: en.memset(o, 0.0), writes=[("rqz", h)])
        KT = P.sb("r_KT", [128, 2, T], BF16)
        KZ = P.sb("r_KZ", [128, NT, 256], BF16)
        MASKT = P.sb("r_maskT", [128, 4, 128], F32)
        RCOL = P.sb("r_col", [128, 12], F32)
        P.dma(MASKT[:], cst["r_maskT"], writes=["r_maskT"], key="rc_m")
        P.dma(RCOL[:], cst["r_col"], writes=["r_col"], key="rc_c")
        P.push_scope()
        WQK = P.sb("r_WQK", [128, KC, 1024], BF16)
        RC = [P.sb("r_RC%d" % i, [128, 512], F32) for i in range(2)]
        RS = [P.sb("r_RS%d" % i, [128, 512], F32) for i in range(2)]
        T1 = [P.sb("r_T1%d" % i, [128, 512], F32) for i in range(2)]
        T2 = [P.sb("r_T2%d" % i, [128, 512], F32) for i in range(2)]
        P.dma(WQK[:], win_d[l][:, 0:1024].rearrange("(kc p) c -> p kc c", p=128), writes=["WQK"], key="rwqk", eng="pool")
        cnt = 0
        for tb in range(4):
            s = tb % 2
            P.dma(RC[s][:], cst["ropeC"][:, tb * 512:(tb + 1) * 512], writes=[("RC", s)], key="rc%d" % s)
            P.dma(RS[s][:], cst["ropeS"][:, tb * 512:(tb + 1) * 512], writes=[("RS", s)], key="rs%d" % s)
            xr = [("XT", tt, hh) for tt in range(tb * 4, tb * 4 + 4) for hh in range(2)]
            for which in range(2):
                for j in range(2):
                    c = cnt % 2
                    cnt += 1
                    colA = which * 512 + j * 128
                    colB = colA + 256
                    for (col, bi) in ((colA, c), (colB, 2 + c)):
                        for kc in range(KC):
                            P.pe(lambda en, o=PS[bi][:], a=WQK[:, kc, col:col + 128], r=XT[:, kc, tb * 512:(tb + 1) * 512],
                                 st=(kc == 0), sp=(kc == KC - 1): en.matmul(o, a, r, start=st, stop=sp),
                                 reads=["WQK"] + xr, writes=[("ps", bi)])
                    P.dve(lambda en, o=T1[c][:], a=PS[c][:], b=RC[s][:]: en.tensor_tensor(o, a, b, ALU.mult),
                          reads=[("ps", c), ("RC", s)], writes=[("T1", c)])
                    P.dve(lambda en, o=T2[c][:], a=PS[2 + c][:], b=RS[s][:]: en.tensor_tensor(o, a, b, ALU.mult),
                          reads=[("ps", 2 + c), ("RS", s)], writes=[("T2", c)])
                    if which == 1:
                        P.pool(lambda en, o=KT[:, j, tb * 512:(tb + 1) * 512], a=T1[c][:], b=T2[c][:]: en.tensor_tensor(o, a, b, ALU.add),
                               reads=[("T1", c), ("T2", c)], writes=[("rqk", which, j, tb)])
                    else:
                        for hh in range(2):
                            pr = slice(64 * hh, 64 * hh + 64)
                            P.pool(lambda en, o=QTz[2 * j + hh][pr, tb * 512:(tb + 1) * 512], a=T1[c][pr, :], b=T2[c][pr, :]: en.tensor_tensor(o, a, b, ALU.add),
                                   reads=[("T1", c), ("T2", c)], writes=[("rqk", 0, 2 * j + hh, tb)])
        rstop = self.cfg.get("ret_stop", 99)
        for tt in range(NT if rstop >= 2 else 0):
            for j in range(2):
                P.pe(lambda en, o=PSB[:, j * 128:(j + 1) * 128], i=KT[:, j, tt * 128:(tt + 1) * 128]: en.transpose(o, i, self.identb[:]),
                     reads=[("rqk", 1, j, tt // 4), "identb"], writes=["psb"])
            for j in range(2):
                for hh in range(2):
                    h = 2 * j + hh
                    P.act(lambda en, o=KZ[:, tt, j * 128 + hh * 64: j * 128 + hh * 64 + 64], i=PSB[:, j * 128 + hh * 64: j * 128 + hh * 64 + 64],
                          sc=RCOL[:, 4 + h:5 + h]: en.activation(o, i, AF.Copy, scale=sc),
                          reads=["psb", "r_col"], writes=[("KZ", tt, j, hh)])
        P.pop_scope()
        P.push_scope()
        WV = P.sb("r_WV", [128, KC, 1024], BF16)
        P.dma(WV[:], win_d[l][:, 1024:2048].rearrange("(kc p) c -> p kc c", p=128), writes=["WV"], key="rwv", eng="pool")
        Vt = [P.sb("r_Vt%d" % i, [128, 512], BF16) for i in range(2)]
        RG = [P.sb("r_RG%d" % i, [128, 512], F32) for i in range(2)]
        S32 = P.sb("r_S32", [128, 2, 128], F32)
        Sb = P.sb("r_Sb", [128, 2, 128], BF16)
        PT = [P.sb("r_PT%d" % i, [128, 128], BF16) for i in range(4)]
        TMP = [P.sb("r_TMP%d" % i, [128, 128], F32) for i in range(4)]
        OR = [P.sb("r_OR%d" % i, [128, 4, 128], F32) for i in range(2)]
        OF = [P.sb("r_OF0", [128, 512], F32)] * 2
        ST = P.sb("r_ST", [128, 2, 4, 8], F32)
        R2 = P.sb("r_R2", [128, 2, 4, 2], F32)
        P.dve(lambda en: en.memset(S32[:], 0.0), writes=["rS32"])
        P.dve(lambda en: en.memset(Sb[:], 0.0), writes=[("rSb", 0), ("rSb", 1)])
        for n in range(NT if rstop >= 3 else 0):
            s = n % 2
            tk = slice(n * 128, (n + 1) * 128)
            xr = [("XT", n, hh) for hh in range(2)]
            for part in range(2):
                for kc in range(KC):
                    P.pe(lambda en, o=PS[part][:], a=XT[:, kc, tk], r=WV[:, kc, part * 512:(part + 1) * 512],
                         st=(kc == 0), sp=(kc == KC - 1): en.matmul(o, a, r, start=st, stop=sp),
                         reads=["WV"] + xr, writes=[("ps", part)])
            P.act(lambda en, o=Vt[s][:], i=PS[0][:]: en.copy(o, i), reads=[("ps", 0)], writes=[("rVt", s)])
            P.act(lambda en, o=RG[s][:], i=PS[1][:]: en.activation(o, i, AF.Silu), reads=[("ps", 1)], writes=[("rRG", s)])
            if rstop < 4:
                continue
            for j in range(0 if self.cfg.get("ret_skip_kv") else 2):
                P.pe(lambda en, o=PS[2][:, j * 256:(j + 1) * 256], a=KZ[:, n, j * 128:(j + 1) * 128], r=Vt[s][:, j * 256:(j + 1) * 256]:
                     en.matmul(o, a, r, start=True, stop=True),
                     reads=[("KZ", n, j, 0), ("KZ", n, j, 1), ("rVt", s)], writes=[("ps", 2)])
            for h in range(4):
                j, pb = h // 2, 64 * (h % 2)
                hs = slice(h * 128, (h + 1) * 128)
                P.pe(lambda en, o=PS[3][:, hs], a=KT[:, j, tk], r=QTz[h][:, tk]: en.matmul(o, a, r, start=True, stop=True),
                     reads=[("rqk", 0, h, n // 4), ("rqk", 1, j, n // 4), ("rqz", h)], writes=[("ps", 3)])
            for h in range(4):
                hs = slice(h * 128, (h + 1) * 128)
                P.dve(lambda en, o=PT[h][:], a=PS[3][:, hs], b=MASKT[:, h, :]: en.tensor_tensor(o, a, b, ALU.mult),
                      reads=[("ps", 3), "r_maskT"], writes=[("rPT", h)])
            if rstop < 5:
                continue
            for h in range(4):
                j, pb = h // 2, 64 * (h % 2)
                hs = slice(h * 128, (h + 1) * 128)
                P.pe(lambda en, o=PS[4][:, hs], a=PT[h][:], r=Vt[s][:, hs]: en.matmul(o, a, r, start=True, stop=True),
                     reads=[("rPT", h), ("rVt", s)], writes=[("ps", 4)])
                P.pe(lambda en, o=PS[5][:, hs], a=QTz[h][:, tk], r=Sb[:, j, :]: en.matmul(o, a, r, start=True, stop=True),
                     reads=[("rqk", 0, h, n // 4), ("rqz", h), ("rSb", j)], writes=[("ps", 5)])
            if rstop < 6:
                continue
            for h in range(4):
                hs = slice(h * 128, (h + 1) * 128)
                P.act(lambda en, o=TMP[h][:], i=PS[5][:, hs], sc=RCOL[:, h:h + 1]: en.activation(o, i, AF.Copy, scale=sc),
                      reads=[("ps", 5), "r_col"], writes=[("rTMP", h)])
                P.dve(lambda en, o=OR[s][:, h, :], a=PS[4][:, hs], b=TMP[h][:]: en.tensor_tensor(o, a, b, ALU.add),
                      reads=[("ps", 4), ("rTMP", h)], writes=[("rOR", s, h)])
            for h in range(4):
                j, pb = h // 2, 64 * (h % 2)
                P.dve(lambda en, o=S32[pb:pb + 64, j, :], i=PS[2][pb:pb + 64, j * 256 + (h % 2) * 128: j * 256 + (h % 2) * 128 + 128],
                      sc=RCOL[pb:pb + 64, 8 + j:9 + j]: en.scalar_tensor_tensor(o, o, sc, i, ALU.mult, ALU.add),
                      reads=[("ps", 2), "rS32", "r_col"], writes=["rS32"])
            for j in range(2):
                P.act(lambda en, o=Sb[:, j, :], i=S32[:, j, :]: en.copy(o, i), reads=["rS32"], writes=[("rSb", j)])
            if rstop < 7:
                continue
            for h in range(4):
                P.dve(lambda en, o=ST[:, n % 2, h, 0:6], i=OR[s][:, h, :]: en.bn_stats(o, i), reads=[("rOR", s, h)], writes=[("rST", n % 2, h)])
                P.dve(lambda en, o=ST[:, n % 2, h, 6:8], i=ST[:, n % 2, h, 0:6]: en.bn_aggr(o, i), reads=[("rST", n % 2, h)], writes=[("rST", n % 2, h)])
            st_all = [("rST", n % 2, h) for h in range(4)]
            P.act(lambda en, o=R2[:, n % 2, :, 0], i=ST[:, n % 2, :, 7]: en.activation(o, i, AF.Sqrt, bias=LN_EPS), reads=st_all, writes=[("rR2", n % 2)])
            P.dve(lambda en, o=R2[:, n % 2, :, 0]: en.reciprocal(o, o), reads=[("rR2", n % 2)], writes=[("rR2", n % 2)])
            P.dve(lambda en, o=R2[:, n % 2, :, 1], a=ST[:, n % 2, :, 6], b=R2[:, n % 2, :, 0]: en.scalar_tensor_tensor(o, a, -1.0, b, ALU.mult, ALU.mult),
                  reads=st_all + [("rR2", n % 2)], writes=[("rR2", n % 2)])
            for h in range(4):
                P.act(lambda en, o=OR[s][:, h, :], sc=R2[:, n % 2, h, 0:1], bi=R2[:, n % 2, h, 1:2]: en.activation(o, o, AF.Identity, bias=bi, scale=sc),
                      reads=[("rOR", s, h), ("rR2", n % 2)], writes=[("rOR", s, h)])
            P.dve(lambda en, o=OF[s][:], a=OR[s][:].rearrange("p h e -> p (h e)"), b=RG[s][:]: en.tensor_tensor(o, a, b, ALU.mult),
                  reads=[("rOR", s, h) for h in range(4)] + [("rRG", s)], writes=["rOF"])
            for h in range(4):
                hs = slice(h * 128, (h + 1) * 128)
                P.pe(lambda en, o=PS[6][:, hs], i=OF[s][:, hs]: en.transpose(o, i, self.ident32[:]),
                     reads=["rOF", "ident32"], writes=[("ps", 6)])
            P.act(lambda en, o=OT[:, 0:4, tk], i=PS[6][:].rearrange("p (q c) -> p q c", q=4): en.copy(o, i),
                  reads=[("ps", 6)], writes=[("OT", n)])
        P.pop_scope()
        P.pop_scope()

    def gdn(self, l, win_d, cw_d, alog_d, dtb_d, gnw_d, OT):
        P = self.P
        cfg = self.cfg
        PS, PSB, XT = self.PS, self.PSB, self.XT
        cst = self.cst_d
        I32 = self.ident32
        P.push_scope()
        CN = {}
        for k in ("g_tri", "g_su", "g_blk", "g_negS", "g_negI", "g_selA", "g_selB"):
            CN[k] = P.sb(k, [128, 128], F32)
            P.dma(CN[k][:], cst[k], writes=[k], key="gc_" + k)
        TRI, SU, BLK, NEGS, NEGI, SELA, SELB = (CN[k] for k in ("g_tri", "g_su", "g_blk", "g_negS", "g_negI", "g_selA", "g_selB"))
        CW = P.sb("g_CW", [128, 12, 4], F32)
        ALOG = P.sb("g_ALOG", [128, 4], F32)
        DTB = P.sb("g_DTB", [128, 4], F32)
        GNW = P.sb("g_GNW", [128, 128], F32)
        P.dma(CW[:], cw_d[l], writes=["g_CW"], key="gc_cw")
        P.dma(ALOG[:], alog_d[l], writes=["g_sc"], key="gc_al")
        P.dma(DTB[:], dtb_d[l], writes=["g_sc2"], key="gc_dt")
        P.dma(GNW[:], gnw_d[l], writes=["g_GNW"], key="gc_gn")
        WAB = P.sb("g_WAB", [128, KC, 8], BF16)
        P.dma(WAB[:], win_d[l][:, 4096:4104].rearrange("(kc p) c -> p kc c", p=128), writes=["g_WAB"], key="gwab", eng="pool")
        names = ["AB8", "Z", "Gt", "BETA", "NBETA", "GC", "GL", "EG", "EKD", "BEG"]
        SC = {}
        SC["AB8"] = P.sb("g_AB8", [128, NT, 8], F32)
        for k in names[1:]:
            SC[k] = P.sb("g_" + k, [128, NT, 4], F32)
        CD = P.sb("g_CD", [128, 2, NT, 4], F32)
        NEGA = P.sb("g_NEGA", [128, 4], F32)
        S_ = ["g_scal"]
        for tt in range(NT):
            for kc in range(KC):
                P.pe(lambda en, o=PS[0][:, tt * 8:(tt + 1) * 8], a=XT[:, kc, tt * 128:(tt + 1) * 128], r=WAB[:, kc, :], st=(kc == 0), sp=(kc == KC - 1):
                     en.matmul(o, a, r, start=st, stop=sp), reads=["g_WAB", ("XT", tt, 0), ("XT", tt, 1)], writes=[("ps", 0)])
        P.dve(lambda en: en.tensor_copy(SC["AB8"][:], PS[0][:, 0:128].rearrange("p (t c) -> p t c", c=8)), reads=[("ps", 0)], writes=S_)
        bc4 = lambda a: a[:].unsqueeze(1).to_broadcast([128, NT, 4])
        P.dve(lambda en: en.tensor_tensor(SC["Z"][:], SC["AB8"][:, :, 0:4], bc4(DTB), ALU.add), reads=S_ + ["g_sc2"], writes=S_)
        P.act(lambda en: en.activation(SC["Z"][:], SC["Z"][:], AF.Exp), reads=S_, writes=S_)
        P.act(lambda en: en.activation(SC["Z"][:], SC["Z"][:], AF.Ln, bias=1.0), reads=S_, writes=S_)
        P.act(lambda en: en.activation(NEGA[:], ALOG[:], AF.Exp), reads=["g_sc"], writes=S_)
        P.dve(lambda en: en.scalar_tensor_tensor(SC["Gt"][:], SC["Z"][:], -1.0, bc4(NEGA), ALU.mult, ALU.mult), reads=S_, writes=S_)
        P.act(lambda en: en.activation(SC["BETA"][:], SC["AB8"][:, :, 4:8], AF.Sigmoid), reads=S_, writes=S_)
        P.dve(lambda en: en.tensor_scalar(SC["NBETA"][:], SC["BETA"][:], -1.0, None, ALU.mult), reads=S_, writes=S_)
        gflat = SC["Gt"][:].rearrange("p t c -> p (t c)")
        for (lhs, name, off) in ((TRI, "g_tri", 0), (BLK, "g_blk", 64), (SELA, "g_selA", 128), (SELB, "g_selB", 192)):
            P.pe(lambda en, o=PS[1][:, off:off + 64], a=lhs[:]: en.matmul(o, a, gflat, start=True, stop=True), reads=S_ + [name], writes=[("ps", 1)])
        v64 = lambda off: PS[1][:, off:off + 64].rearrange("p (t c) -> p t c", c=4)
        P.dve(lambda en: en.tensor_copy(SC["GC"][:], v64(0)), reads=[("ps", 1)], writes=S_)
        P.dve(lambda en: en.tensor_copy(SC["GL"][:], v64(64)), reads=[("ps", 1)], writes=S_)
        P.dve(lambda en: en.tensor_copy(CD[:, 0, :, :], v64(128)), reads=[("ps", 1)], writes=S_)
        P.dve(lambda en: en.tensor_copy(CD[:, 1, :, :], v64(192)), reads=[("ps", 1)], writes=S_)
        P.act(lambda en: en.activation(CD[:], CD[:], AF.Exp), reads=S_, writes=S_)
        P.act(lambda en: en.activation(SC["EG"][:], SC["GC"][:], AF.Exp), reads=S_, writes=S_)
        P.dve(lambda en: en.tensor_tensor(SC["EKD"][:], SC["GL"][:], SC["GC"][:], ALU.subtract), reads=S_, writes=S_)
        P.act(lambda en: en.activation(SC["EKD"][:], SC["EKD"][:], AF.Exp), reads=S_, writes=S_)
        P.dve(lambda en: en.tensor_tensor(SC["BEG"][:], SC["BETA"][:], SC["EG"][:], ALU.mult), reads=S_, writes=S_)
        P.barrier()
        gstop = cfg.get("gdn_stop", 99)

        def one_pass(hp):
            P.push_scope()
            QN = P.sb("g_QN", [128, 2, T], BF16)
            KN = P.sb("g_KN", [128, 2, T], BF16)
            KTOK = P.sb("g_KTOK", [128, NT, 256], BF16)
            VTOK = P.sb("g_VTOK", [128, NT, 256], BF16)
            P.push_scope()
            PRE = P.sb("g_PRE", [128, T + 3], F32)
            CV = P.sb("g_CV", [128, T], F32)
            VT = P.sb("g_VT", [128, T], BF16)
            RN = [P.sb("g_RN%d" % i, [128, 512], F32) for i in range(2)]
            WC = [P.sb("g_WC%d" % i, [128, KC, 128], BF16) for i in range(2)]
            P.dve(lambda en: en.memset(PRE[:, 0:3], 0.0), writes=["g_PRE0"])
            SSQB = (4, 5, 6, 0)
            items = []
            cnt = 0
            for kind in range(3):
                for hh in range(2):
                    items.append((kind, hh, kind * 4 + 2 * hp + hh, cnt % 2))
                    cnt += 1

            def stageA(it):
                kind, hh, f, s = it
                c0 = 2048 + f * 128
                P.dma(WC[s][:], win_d[l][:, c0:c0 + 128].rearrange("(kc p) c -> p kc c", p=128), writes=[("g_WC", s)], key="gwc%d" % s, eng="pool")
                for tb in range(4):
                    b = 2 + tb % 2
                    blk = slice(tb * 512, (tb + 1) * 512)
                    for kc in range(KC):
                        P.pe(lambda en, o=PS[b][:], a=WC[s][:, kc, :], r=XT[:, kc, blk], st=(kc == 0), sp=(kc == KC - 1):
                             en.matmul(o, a, r, start=st, stop=sp),
                             reads=[("g_WC", s)] + [("XT", tt, q) for tt in range(tb * 4, tb * 4 + 4) for q in range(2)], writes=[("ps", b)])
                    P.act(lambda en, o=PRE[:, 3 + tb * 512: 3 + (tb + 1) * 512], i=PS[b][:]: en.copy(o, i), reads=[("ps", b)], writes=[("g_PRE", tb)])

            def stageB(it):
                kind, hh, f, s = it
                for tb in range(4):
                    blk = slice(tb * 512, (tb + 1) * 512)
                    pre_r = [("g_PRE", tb), "g_PRE0", "g_CW"] + ([("g_PRE", tb - 1)] if tb > 0 else [])
                    P.dve(lambda en, sc=CW[:, f, 3:4], o=CV[:, blk], i=PRE[:, 3 + tb * 512: 3 + (tb + 1) * 512]: en.tensor_scalar(o, i, sc, None, ALU.mult),
                          reads=pre_r, writes=[("g_CV", tb)])
                    for k in (2, 1, 0):
                        P.dve(lambda en, sc=CW[:, f, k:k + 1], o=CV[:, blk], i=PRE[:, k + tb * 512: k + (tb + 1) * 512]: en.scalar_tensor_tensor(o, i, sc, o, ALU.mult, ALU.add),
                              reads=pre_r + [("g_CV", tb)], writes=[("g_CV", tb)])
                    if kind == 2:
                        P.act(lambda en, o=VT[:, blk], i=CV[:, blk]: en.activation(o, i, AF.Silu), reads=[("g_CV", tb)], writes=[("g_VT", tb)])
                    else:
                        P.act(lambda en, o=CV[:, blk]: en.activation(o, o, AF.Silu), reads=[("g_CV", tb)], writes=[("g_CV", tb)])
                        P.act(lambda en, o=VT[:, blk], i=CV[:, blk]: en.activation(o, i, AF.Square), reads=[("g_CV", tb)], writes=[("g_VT", tb)])
                        b2_ = SSQB[tb]
                        P.pe(lambda en, o=PS[b2_][:], r=VT[:, blk]: en.matmul(o, self.onesb[:], r, start=True, stop=True),
                             reads=[("g_VT", tb), "onesb"], writes=[("ps", b2_)])
                if kind == 2:
                    tposes(VTOK, lambda tt: VT[:, tt * 128:(tt + 1) * 128], lambda tt: [("g_VT", tt // 4)], kind, hh)

            def stageC(it):
                kind, hh, f, s = it
                if kind == 2:
                    return
                dst = QN if kind == 0 else KN
                scale = (128.0 ** -0.5) if kind == 0 else 1.0
                for tb in range(4):
                    blk = slice(tb * 512, (tb + 1) * 512)
                    b2_ = SSQB[tb]
                    P.act(lambda en, o=RN[tb % 2][:], i=PS[b2_][:]: en.activation(o, i, AF.Ln, bias=NORM_EPS), reads=[("ps", b2_)], writes=[("g_RN", tb % 2)])
                    P.act(lambda en, o=RN[tb % 2][:]: en.activation(o, o, AF.Exp, scale=-0.5), reads=[("g_RN", tb % 2)], writes=[("g_RN", tb % 2)])
                    P.dve(lambda en, o=dst[:, hh, blk], a=CV[:, blk], r=RN[tb % 2][:], sc=scale:
                          en.scalar_tensor_tensor(o, a, sc, r, ALU.mult, ALU.mult),
                          reads=[("g_CV", tb), ("g_RN", tb % 2)], writes=[("g_N", kind, hh, tb)])
                if kind == 1:
                    tposes(KTOK, lambda tt: KN[:, hh, tt * 128:(tt + 1) * 128], lambda tt: [("g_N", 1, hh, tt // 4)], kind, hh)

            def tposes(dstT, srcview, rtok_of, kind, hh):
                for rnd in range(2):
                    for q in range(8):
                        tt = rnd * 8 + q
                        P.pe(lambda en, o=PSB[:, q * 128:(q + 1) * 128], i=srcview(tt): en.transpose(o, i, self.identb[:]),
                             reads=rtok_of(tt) + ["identb"], writes=["psb"])
                    P.act(lambda en, o=dstT[:, rnd * 8:(rnd + 1) * 8, hh * 128:(hh + 1) * 128], i=PSB[:].rearrange("p (q c) -> p q c", q=8): en.copy(o, i),
                          reads=["psb"], writes=[("g_TOK", kind, hh, rnd)])

            stageA(items[0])
            for i, it in enumerate(items):
                stageB(it)
                if i + 1 < len(items):
                    stageA(items[i + 1])
                stageC(it)
            P.pop_scope()
            P.push_scope()
            WGG = P.sb("g_WGG", [128, KC, 256], BF16)
            cg = 3584 + 2 * hp * 128
            P.dma(WGG[:], win_d[l][:, cg:cg + 256].rearrange("(kc p) c -> p kc c", p=128), writes=["g_WGG"], key="gwgg", eng="pool")
            f32t = lambda nm: P.sb(nm, [128, 128], F32)
            b16t = lambda nm: P.sb(nm, [128, 128], BF16)
            TT = []
            for hh in range(2):
                d = {}
                for nm in ("R", "R2", "Dm", "DTm", "Na", "Nb", "Mm", "Ma", "Mb", "Pa", "Pb", "VNf", "TMPo"):
                    d[nm] = f32t("g_%s%d" % (nm, hh))
                d["Nm"] = [f32t("g_Nm%d_%d" % (hh, i)) for i in range(2)]
                for nm in ("Pu", "Pw"):
                    d[nm] = b16t("g_%s%d" % (nm, hh))
                for nm in ("VN", "VN2"):
                    d[nm] = [b16t("g_%s%d_%d" % (nm, hh, i)) for i in range(2)]
                d["QKm"] = [b16t("g_QKm%d_%d" % (hh, i)) for i in range(3)]
                d["WT"] = [b16t("g_WT%d_%d" % (hh, i)) for i in range(2)]
                d["U"] = [f32t("g_U%d_%d" % (hh, i)) for i in range(2)]
                d["S32"] = f32t("g_S32_%d" % hh)
                d["SB"] = b16t("g_SB_%d" % hh)
                TT.append(d)
            GO = [P.sb("g_GO%d" % i, [128, 256], F32) for i in range(2)]
            GG = P.sb("g_GG", [128, 256], F32)
            OFg = P.sb("g_OFg", [128, 256], F32)
            SS = P.sb("g_SS", [128, NT, 2], F32)
            RS = P.sb("g_RS", [128, NT, 2], F32)
            P.dve(lambda en: en.memset(SS[:], 0.0), writes=["g_SS"])
            for hh in range(2):
                P.dve(lambda en, t=TT[hh]["S32"]: en.memset(t[:], 0.0), writes=[("gS32", hh)])
                P.dve(lambda en, t=TT[hh]["SB"]: en.memset(t[:], 0.0), writes=[("gSB", hh)])
                for nm in ("VN", "VN2"):
                    for i in range(2):
                        P.pool(lambda en, t=TT[hh][nm][i]: en.memset(t[:], 0.0), writes=[("g2", nm, hh, i)])

            def early(n, hh):
                h = 2 * hp + hh
                d = TT[hh]
                tk = slice(n * 128, (n + 1) * 128)
                bk = PS[hh]
                bt = ("ps", hh)
                bB = PS[2 + hh]
                btB = ("ps", 2 + hh)
                tg = lambda nm: ("g2", nm, hh)
                rq = [("g_N", 0, hh, n // 4)]
                rk = [("g_N", 1, hh, n // 4)]
                gcol = SC["Gt"][:, n, h:h + 1]
                P.dve(lambda en: en.tensor_scalar(d["R"][:], SU[:], gcol, None, ALU.mult), reads=S_ + ["g_su"], writes=[tg("R")])
                P.dve(lambda en: en.tensor_scalar(d["R2"][:], TRI[:], gcol, None, ALU.mult), reads=S_ + ["g_tri"], writes=[tg("R2")])
                P.pe(lambda en: en.matmul(bB[:, 256:384], KN[:, hh, tk], KN[:, hh, tk], start=True, stop=True), reads=rk, writes=[btB])
                P.pe(lambda en: en.matmul(bB[:, 384:512], KN[:, hh, tk], QN[:, hh, tk], start=True, stop=True), reads=rk + rq, writes=[btB])
                yield
                P.pe(lambda en: en.matmul(bk[:, 256:384], TRI[:], d["R"][:], start=True, stop=True), reads=[tg("R"), "g_tri"], writes=[bt])
                P.pe(lambda en: en.matmul(bk[:, 384:512], SU[:], d["R2"][:], start=True, stop=True), reads=[tg("R2"), "g_su"], writes=[bt])
                yield
                P.act(lambda en: en.activation(d["Dm"][:], bk[:, 256:384], AF.Exp), reads=[bt], writes=[tg("Dm")])
                P.act(lambda en: en.activation(d["DTm"][:], bk[:, 384:512], AF.Exp), reads=[bt], writes=[tg("DTm")])
                yield
                P.pool(lambda en: en.tensor_tensor(d["Dm"][:], d["Dm"][:], NEGS[:], ALU.mult), reads=[tg("Dm"), "g_negS"], writes=[tg("Dm")])
                P.pool(lambda en: en.tensor_tensor(d["DTm"][:], d["DTm"][:], NEGI[:], ALU.mult), reads=[tg("DTm"), "g_negI"], writes=[tg("DTm")])
                yield
                nm_ = d["Nm"][n % 2]
                P.dve(lambda en: en.scalar_tensor_tensor(nm_[:], bB[:, 256:384], SC["NBETA"][:, n, h:h + 1], d["Dm"][:], ALU.mult, ALU.mult),
                      reads=[btB, tg("Dm")] + S_, writes=[("g2", "Nm", hh, n % 2)])
                qkm = d["QKm"][n % 3]
                P.dve(lambda en: en.tensor_tensor(qkm[:], bB[:, 384:512], d["DTm"][:], ALU.mult), reads=[btB, tg("DTm")], writes=[("g2", "QKm", hh, n % 3)])
                yield

            def late(n, hh):
                h = 2 * hp + hh
                d = TT[hh]
                bk = PS[hh]
                bt = ("ps", hh)
                bB = PS[2 + hh]
                btB = ("ps", 2 + hh)
                tg0 = lambda nm: ("g2", nm, hh)
                nmtok = ("g2", "Nm", hh, n % 2)
                tg = lambda nm: nmtok if nm == "Nm" else tg0(nm)
                nm_ = d["Nm"][n % 2]
                P.pe(lambda en: en.transpose(bk[:, 0:128], nm_[:], I32[:]), reads=[nmtok, "ident32"], writes=[bt])
                yield
                P.act(lambda en: en.copy(d["Mm"][:], bk[:, 0:128]), reads=[bt], writes=[tg("Mm")])
                yield
                P.pool(lambda en: en.tensor_tensor(d["Pa"][:], d["Mm"][:], I32[:], ALU.add), reads=[tg("Mm"), "ident32"], writes=[tg("Pa")])
                Nk, Mk, Pk = nm_, d["Mm"], d["Pa"]
                nN, nM, nP = ("Nm", "Mm", "Pa")
                for k in range(5):
                    Nn, nNn = (d["Na"], "Na") if k % 2 == 0 else (d["Nb"], "Nb")
                    Mn, nMn = (d["Ma"], "Ma") if k % 2 == 0 else (d["Mb"], "Mb")
                    Pn, nPn = (d["Pb"], "Pb") if k % 2 == 0 else (d["Pa"], "Pa")
                    P.pe(lambda en, a=Mk, r=Nk: en.matmul(bk[:, 0:128], a[:], r[:], start=True, stop=True), reads=[tg(nN), tg(nM)], writes=[bt])
                    if k < 4:
                        P.pe(lambda en, a=Nk, r=Mk: en.matmul(bB[:, 0:128], a[:], r[:], start=True, stop=True), reads=[tg(nN), tg(nM)], writes=[btB])
                    yield
                    P.act(lambda en, o=Nn: en.copy(o[:], bk[:, 0:128]), reads=[bt], writes=[tg(nNn)])
                    if k < 4:
                        P.dve(lambda en, o=Mn: en.tensor_copy(o[:], bB[:, 0:128]), reads=[btB], writes=[tg(nMn)])
                    yield
                    P.pe(lambda en, a=Nn, r=Pk: en.matmul(bB[:, 128:256], a[:], r[:], start=True, stop=True), reads=[tg(nP), tg(nNn)], writes=[btB])
                    yield
                    P.dve(lambda en, o=Pn, r=Pk: en.tensor_tensor(o[:], bB[:, 128:256], r[:], ALU.add), reads=[btB, tg(nP)], writes=[tg(nPn)])
                    yield
                    if k == 4:
                        P.act(lambda en, r=Pn: en.activation(d["Pu"][:], r[:], AF.Copy, scale=SC["BETA"][:, n, h:h + 1]), reads=[tg(nPn)] + S_, writes=[tg("Pu")])
                        P.act(lambda en, r=Pn: en.activation(d["Pw"][:], r[:], AF.Copy, scale=SC["BEG"][:, n, h:h + 1]), reads=[tg(nPn)] + S_, writes=[tg("Pw")])
                        yield
                    Nk, Mk, Pk = Nn, Mn, Pn
                    nN, nM, nP = nNn, nMn, nPn
                P.pe(lambda en: en.matmul(bB[:, 0:128], d["Pu"][:], VTOK[:, n, hh * 128:(hh + 1) * 128], start=True, stop=True),
                     reads=[tg("Pu"), ("g_TOK", 2, hh, n // 8)], writes=[btB])
                P.pe(lambda en: en.matmul(bk[:, 384:512], KTOK[:, n, hh * 128:(hh + 1) * 128], d["Pw"][:], start=True, stop=True),
                     reads=[tg("Pw"), ("g_TOK", 1, hh, n // 8)], writes=[bt])
                yield
                P.dve(lambda en: en.tensor_copy(d["U"][n % 2][:], bB[:, 0:128]), reads=[btB], writes=[("g2", "U", hh, n % 2)])
                P.act(lambda en: en.copy(d["WT"][n % 2][:], bk[:, 384:512]), reads=[bt], writes=[("g2", "WT", hh, n % 2)])
                yield

            def scan(n, hh):
                tk = slice(n * 128, (n + 1) * 128)
                h = 2 * hp + hh
                d = TT[hh]
                b4 = PS[4 + hh]
                bt4 = ("ps", 4 + hh)
                tg = lambda nm: ("g2", nm, hh)
                go = GO[n % 2]
                for cpar in range(2):
                    rows = slice(64 * cpar, 64 * cpar + 64)
                    P.pe(lambda en: en.matmul(b4[:, 0:128], d["WT"][n % 2][:], d["SB"][:], start=True, stop=True),
                         reads=[("g2", "WT", hh, n % 2), ("gSB", hh)], writes=[bt4])
                    P.pe(lambda en: en.matmul(b4[:, 128:256], QN[:, hh, tk], d["SB"][:], start=True, stop=True),
                         reads=[("g_N", 0, hh, n // 4), ("gSB", hh)], writes=[bt4])
                    yield
                    P.dve(lambda en, rows=rows: en.scalar_tensor_tensor(d["VNf"][rows, :], b4[rows, 0:128], -1.0, d["U"][n % 2][rows, :], ALU.mult, ALU.add),
                          reads=[bt4, ("g2", "U", hh, n % 2)], writes=[tg("VNf")])
                    P.dve(lambda en, rows=rows: en.tensor_scalar(d["TMPo"][rows, :], b4[rows, 128:256], SC["EG"][rows, n, h:h + 1], None, ALU.mult),
                          reads=[bt4] + S_, writes=[tg("TMPo")])
                    yield
                    P.act(lambda en, rows=rows, cpar=cpar: en.copy(d["VN"][cpar][rows, :], d["VNf"][rows, :]),
                          reads=[tg("VNf"), ("g2", "VN", hh, cpar)], writes=[("g2", "VN", hh, cpar)])
                    P.act(lambda en, rows=rows, cpar=cpar: en.activation(d["VN2"][cpar][rows, :], d["VNf"][rows, :], AF.Copy, scale=SC["EKD"][rows, n, h:h + 1]),
                          reads=[tg("VNf"), ("g2", "VN2", hh, cpar)] + S_, writes=[("g2", "VN2", hh, cpar)])
                    yield
                    P.pe(lambda en, cpar=cpar: en.matmul(b4[:, 256:384], d["QKm"][n % 3][:], d["VN"][cpar][:], start=True, stop=True),
                         reads=[("g2", "QKm", hh, n % 3), ("g2", "VN", hh, cpar)], writes=[bt4])
                    P.pe(lambda en, cpar=cpar: en.matmul(b4[:, 384:512], KTOK[:, n, hh * 128:(hh + 1) * 128], d["VN2"][cpar][:], start=True, stop=True),
                         reads=[("g_TOK", 1, hh, n // 8), ("g2", "VN2", hh, cpar)], writes=[bt4])
                    yield
                    P.dve(lambda en, cpar=cpar: en.scalar_tensor_tensor(d["S32"][:], d["S32"][:], CD[:, cpar, n, h:h + 1], b4[:, 384:512], ALU.mult, ALU.add),
                          reads=[bt4, ("gS32", hh)] + S_, writes=[("gS32", hh)])
                    P.dve(lambda en, rows=rows: en.tensor_tensor(go[rows, hh * 128:(hh + 1) * 128], d["TMPo"][rows, :], b4[rows, 256:384], ALU.add),
                          reads=[bt4, tg("TMPo")], writes=[("gGO", n % 2, hh)])
                    yield
                    P.act(lambda en: en.copy(d["SB"][:], d["S32"][:]), reads=[("gS32", hh)], writes=[("gSB", hh)])
                    yield

            def post(n):
                tk = slice(n * 128, (n + 1) * 128)
                b6 = PS[6]
                bt6 = ("ps", 6)
                go = GO[n % 2]
                for kc in range(KC):
                    P.pe(lambda en, kc=kc: en.matmul(b6[:, 0:256], XT[:, kc, tk], WGG[:, kc, :], start=(kc == 0), stop=(kc == KC - 1)),
                         reads=["g_WGG", ("XT", n, 0), ("XT", n, 1)], writes=[bt6])
                for hh in range(2):
                    P.act(lambda en, hh=hh: en.activation(OFg[:, hh * 128:(hh + 1) * 128], go[:, hh * 128:(hh + 1) * 128], AF.Square, accum_out=SS[:, n, hh:hh + 1]),
                          reads=[("gGO", n % 2, hh), "g_SS", ("gOF", hh)], writes=["g_SS", ("gOF", hh)])
                yield
                P.act(lambda en: en.activation(GG[:], b6[:, 0:256], AF.Silu), reads=[bt6], writes=["gGG"])
                P.act(lambda en: en.activation(RS[:, n, :], SS[:, n, :], AF.Sqrt, bias=NORM_EPS, scale=1.0 / 128.0), reads=["g_SS"], writes=["g_RS"])
                yield
                P.dve(lambda en: en.reciprocal(RS[:, n, :], RS[:, n, :]), reads=["g_RS"], writes=["g_RS"])
                yield
                for hh in range(2):
                    hs = slice(hh * 128, (hh + 1) * 128)
                    P.dve(lambda en, hh=hh, hs=hs: en.scalar_tensor_tensor(OFg[:, hs], go[:, hs], RS[:, n, hh:hh + 1], GNW[:], ALU.mult, ALU.mult),
                          reads=[("gGO", n % 2, hh), "g_RS", "g_GNW", ("gOF", hh)], writes=[("gOF", hh)])
                yield
                for hh in range(2):
                    hs = slice(hh * 128, (hh + 1) * 128)
                    P.pool(lambda en, hs=hs: en.tensor_tensor(OFg[:, hs], OFg[:, hs], GG[:, hs], ALU.mult), reads=[("gOF", hh), "gGG"], writes=[("gOF", hh)])
                yield
                for hh in range(2):
                    hs = slice(hh * 128, (hh + 1) * 128)
                    P.pe(lambda en, hh=hh, hs=hs: en.transpose(b6[:, 256 + hh * 128: 256 + (hh + 1) * 128], OFg[:, hs], I32[:]),
                         reads=[("gOF", hh), "ident32"], writes=[bt6])
                yield
                P.act(lambda en: en.copy(OT[:, 4 + 2 * hp: 6 + 2 * hp, tk], b6[:, 256:512].rearrange("p (q c) -> p q c", q=2)),
                      reads=[bt6], writes=[("OT", n)])
                yield

            nt_ = cfg.get("gdn_nt", NT) if gstop >= 3 else 0

            def rr(gens):
                while gens:
                    alive = []
                    for g in gens:
                        try:
                            next(g)
                            alive.append(g)
                        except StopIteration:
                            pass
                    gens = alive

            if nt_:
                rr([early(0, 0), early(0, 1)])
            for it in range(nt_ + 2 if nt_ else 0):
                gens = []
                if it < nt_:
                    gens += [late(it, 0), late(it, 1)]
                if it + 1 < nt_:
                    gens += [early(it + 1, 0), early(it + 1, 1)]
                if 1 <= it <= nt_ and gstop >= 4:
                    gens += [scan(it - 1, 0), scan(it - 1, 1)]
                if 2 <= it <= nt_ + 1 and gstop >= 5:
                    gens += [post(it - 2)]
                rr(gens)
            P.pop_scope()
            P.pop_scope()

        for hp in (cfg.get("gdn_passes", range(2)) if gstop >= 2 else []):
            one_pass(hp)
        P.pop_scope()


def ext_w_in(w_in):
    L = w_in.shape[0]
    q = w_in[:, :, 0:256]
    k = w_in[:, :, 256:512]

    def sw(a):
        a4 = a.reshape(L, D, 4, 2, 32)
        return a4[:, :, :, ::-1, :].reshape(L, D, 256)
    parts = [q, sw(q), k, sw(k), w_in[:, :, 512:]]
    return np.ascontiguousarray(np.concatenate(parts, axis=2))


def prep_inputs(inp):
    f = lambda a: np.ascontiguousarray(np.asarray(a, dtype=np.float32))
    shared = {}
    shared["w_in"] = ext_w_in(f(inp["w_in"]))
    cw = f(inp["conv_w"])
    shared["conv_w"] = np.ascontiguousarray(cw.reshape(DEPTH, 4, 12, 128).transpose(0, 3, 2, 1))
    shared["a_log"] = np.ascontiguousarray(np.broadcast_to(f(inp["a_log"])[:, None, :], (DEPTH, 128, 4)))
    shared["dt_bias"] = np.ascontiguousarray(np.broadcast_to(f(inp["dt_bias"])[:, None, :], (DEPTH, 128, 4)))
    shared["gdn_norm_w"] = np.ascontiguousarray(np.broadcast_to(f(inp["gdn_norm_w"])[:, None, :], (DEPTH, 128, 128)))
    shared["w_o"] = f(inp["w_o"])
    for k in ("ln1_g", "ln1_b", "ln2_g", "ln2_b"):
        a = f(inp[k])
        shared[k] = np.ascontiguousarray(np.broadcast_to(a[:, None, :], (DEPTH, 128, D)))
        shared[k + "_c"] = np.ascontiguousarray(a.reshape(DEPTH, KC, 128).transpose(0, 2, 1))
    shared["ffn_w_gate"] = f(inp["ffn_w_gate"])[0]
    shared["ffn_w_up"] = f(inp["ffn_w_up"])[0]
    shared["ffn_w_down"] = f(inp["ffn_w_down"])[0]
    shared["router_w"] = f(inp["router_w"])[0]
    shared["moe_w_gate"] = f(inp["moe_w_gate"])[0]
    shared["moe_w_up"] = f(inp["moe_w_up"])[0]
    shared["moe_w_down"] = f(inp["moe_w_down"])[0]
    for k, v in make_consts().items():
        shared["c_" + k] = v
    return shared


_CACHE = {}


def run(inp, cfg, cores=8):
    shared = prep_inputs(inp)
    x = np.ascontiguousarray(np.asarray(inp["x"], dtype=np.float32))
    b = Builder(cfg)
    nc = b.build()
    in_maps = []
    for c in range(cores):
        m = dict(shared)
        m["x"] = x[c]
        in_maps.append(m)
    res = run_bass_kernel_spmd(nc, in_maps, core_ids=list(range(cores)))
    return res, b


def kernel(**inputs):
    res, _ = run(inputs, {}, cores=8)
    out = np.stack([r["out"] for r in res.results], axis=0)
    return out.astype(np.float32)
```

```python
import math
import numpy as np
import ml_dtypes
import concourse.bass as bass
import concourse.mybir as mybir
from concourse.bass_utils import run_bass_kernel_spmd
from contextlib import ExitStack

F32 = mybir.dt.float32
BF16 = mybir.dt.bfloat16
AF = mybir.ActivationFunctionType
ALU = mybir.AluOpType
AX = mybir.AxisListType

T = 2048
D = 1024
NT = 16
KC = 8
DEPTH = 2
ALPHA = (2 * DEPTH) ** 0.25
INVA = 1.0 / ALPHA
LN_EPS = 1e-5
LN_EPS_S = LN_EPS / (ALPHA * ALPHA)
NORM_EPS = 1e-6
D_FF = 2816
D_FFE = 3584
NE = 8
WEXT = 4104


class Prog:
    ENGS = ("pe", "act", "dve", "pool", "sp")

    def __init__(self, nc, same_engine_sync=True):
        self.nc = nc
        self.es = ExitStack()
        self.ops = []
        self.tok = {}
        self.same_engine_sync = same_engine_sync
        self.base_deps = set()
        self.last_eng = {}
        self.last_key = {}
        self.scopes = []

    def sb(self, name, shape, dt):
        es = self.scopes[-1] if self.scopes else self.es
        self.uid = getattr(self, "uid", 0) + 1
        return es.enter_context(self.nc.sbuf_tensor("%s_u%d" % (name, self.uid), list(shape), dt))

    def push_scope(self):
        self.barrier()
        self.scopes.append(ExitStack())

    def pop_scope(self):
        self.barrier()
        self.scopes.pop().close()

    def ps(self, name, shape, dt=F32):
        return self.es.enter_context(self.nc.psum_tensor(name, list(shape), dt))

    def op(self, eng, fn, reads=(), writes=(), dma_key=None):
        idx = len(self.ops)
        deps = set(self.base_deps)
        for t in reads:
            e = self.tok.get(t)
            if e is not None and e[0] is not None:
                deps.add(e[0])
        for t in writes:
            e = self.tok.get(t)
            if e is not None:
                if e[0] is not None:
                    deps.add(e[0])
                deps.update(e[1])
        for t in reads:
            e = self.tok.setdefault(t, [None, []])
            e[1].append(idx)
        for t in writes:
            self.tok[t] = [idx, []]
        deps.discard(idx)
        self.ops.append(dict(eng=eng, fn=fn, deps=deps, dma_key=dma_key))
        self.last_eng[eng] = idx
        if dma_key is not None:
            self.last_key[dma_key] = idx
        return idx

    def barrier(self):
        self.base_deps = set(self.last_eng.values()) | set(self.last_key.values())

    def pe(self, fn, reads=(), writes=()):
        return self.op("pe", fn, reads, writes)

    def act(self, fn, reads=(), writes=()):
        return self.op("act", fn, reads, writes)

    def dve(self, fn, reads=(), writes=()):
        return self.op("dve", fn, reads, writes)

    def pool(self, fn, reads=(), writes=()):
        return self.op("pool", fn, reads, writes)

    def dma(self, out, in_, reads=(), writes=(), key=None, eng="sp"):
        assert key is not None
        return self.op(eng, lambda e: e.dma_start(out=out, in_=in_), reads, writes, dma_key=key)

    def emit(self):
        nc = self.nc
        ops = self.ops
        n = len(ops)
        needed = [False] * n
        for i, o in enumerate(ops):
            keep = set()
            for d in o["deps"]:
                od = ops[d]
                if od["dma_key"] is None and od["eng"] == o["eng"]:
                    if o["eng"] == "pe" or not self.same_engine_sync:
                        continue
                keep.add(d)
            o["deps"] = keep
            for d in keep:
                needed[d] = True
        eng_sem = {}
        key_sem = {}
        eng_cnt = {e: 0 for e in self.ENGS}
        key_cnt = {}
        for i, o in enumerate(ops):
            if o["dma_key"] is not None:
                k = o["dma_key"]
                if k not in key_sem:
                    key_sem[k] = self.es.enter_context(nc.semaphore("d_" + str(k)))
                    key_cnt[k] = 0
                key_cnt[k] += 16
                o["sig"] = (key_sem[k], key_cnt[k])
                o["inc"] = (key_sem[k], 16)
            elif needed[i]:
                e = o["eng"]
                if e not in eng_sem:
                    eng_sem[e] = self.es.enter_context(nc.semaphore("e_" + e))
                eng_cnt[e] += 1
                o["sig"] = (eng_sem[e], eng_cnt[e])
                o["inc"] = (eng_sem[e], 1)
            else:
                o["sig"] = None
                o["inc"] = None
        per_eng = {e: [] for e in self.ENGS}
        for i, o in enumerate(ops):
            per_eng[o["eng"]].append(i)
        self.stats = {e: len(v) for e, v in per_eng.items()}
        self.stats["sems"] = len(key_sem) + len(eng_sem)

        def emit_engine(eng_name, engine):
            waited = {}
            for i in per_eng[eng_name]:
                o = ops[i]
                w = {}
                for d in o["deps"]:
                    sem, val = ops[d]["sig"]
                    key = id(sem)
                    if key not in w or w[key][1] < val:
                        w[key] = (sem, val)
                for key, (sem, val) in w.items():
                    if waited.get(key, 0) >= val:
                        continue
                    engine.wait_ge(sem, val)
                    waited[key] = val
                ins = o["fn"](engine)
                if o["inc"] is not None:
                    ins.then_inc(o["inc"][0], o["inc"][1])
            last = {}
            for i in per_eng[eng_name]:
                o = ops[i]
                if o["dma_key"] is not None:
                    sem, val = o["sig"]
                    last[id(sem)] = (sem, max(val, last.get(id(sem), (None, 0))[1]))
            for key, (sem, val) in last.items():
                if waited.get(key, 0) < val:
                    engine.wait_ge(sem, val)

        with nc.Block() as block:
            if per_eng["sp"]:
                @block.sync
                def _(e):
                    emit_engine("sp", e)
            if per_eng["pe"]:
                @block.tensor
                def _(e):
                    emit_engine("pe", e)
            if per_eng["act"]:
                @block.scalar
                def _(e):
                    emit_engine("act", e)
            if per_eng["dve"]:
                @block.vector
                def _(e):
                    emit_engine("dve", e)
            if per_eng["pool"]:
                @block.gpsimd
                def _(e):
                    emit_engine("pool", e)
        self.es.close()


def make_consts():
    c = {}
    c["ident32"] = np.eye(128, dtype=np.float32)
    c["ones32"] = np.ones((128, 128), np.float32)
    inv = (10000.0 ** (-np.arange(0, 64, 2, dtype=np.float32) / np.float32(64))).astype(np.float32)
    pos = np.arange(T, dtype=np.float32)
    ang = (pos[:, None] * inv[None, :]).astype(np.float32)
    cos = np.cos(ang.astype(np.float64)).astype(np.float32).T
    sin = np.sin(ang.astype(np.float64)).astype(np.float32).T
    C = np.zeros((128, T), np.float32)
    S = np.zeros((128, T), np.float32)
    for p in range(128):
        C[p] = cos[p % 32]
        S[p] = sin[p % 32] * (-1.0 if (p % 64) < 32 else 1.0)
    c["ropeC"] = C
    c["ropeS"] = S
    gam = 1.0 - 2.0 ** (-5.0 - np.arange(4, dtype=np.float64))
    idx = np.arange(128, dtype=np.float64)
    maskT = np.zeros((128, 4, 128), np.float32)
    for h in range(4):
        dlt = idx[None, :] - idx[:, None]
        m = np.where(dlt >= 0, gam[h] ** np.maximum(dlt, 0), 0.0) * 0.125
        maskT[:, h, :] = m
    c["r_maskT"] = maskT
    rcol = np.zeros((128, 12), np.float32)
    for h in range(4):
        rcol[:, h] = gam[h] ** (idx + 1.0)
        rcol[:, 4 + h] = gam[h] ** (127.0 - idx) * 0.125
    for j in range(2):
        rcol[:64, 8 + j] = gam[2 * j] ** 128.0
        rcol[64:, 8 + j] = gam[2 * j + 1] ** 128.0
    c["r_col"] = rcol
    t = np.arange(128)
    same = (t[:, None] // 64) == (t[None, :] // 64)
    tri = (same & (t[:, None] <= t[None, :])).astype(np.float32)
    su = (same & (t[:, None] > t[None, :])).astype(np.float32)
    c["g_tri"] = tri
    c["g_su"] = su
    c["g_blk"] = same.astype(np.float32)
    NEG = -30000.0
    c["g_negS"] = np.where(same & (t[None, :] < t[:, None]), 0.0, NEG).astype(np.float32)
    c["g_negI"] = np.where(same & (t[None, :] >= t[:, None]), 0.0, NEG).astype(np.float32)
    selA = np.zeros((128, 128), np.float32); selA[:64, :] = 1.0
    selB = np.zeros((128, 128), np.float32); selB[64:, :] = 1.0
    c["g_selA"] = selA
    c["g_selB"] = selB
    return c


CONST_NAMES = ["ident32", "ones32", "ropeC", "ropeS", "r_maskT", "r_col", "g_tri", "g_su",
               "g_blk", "g_negS", "g_negI", "g_selA", "g_selB"]


class Builder:
    def __init__(self, cfg):
        self.cfg = cfg
        nc = bass.Bass("TRN2", target_bir_lowering=False)
        self.nc = nc
        self.P = Prog(nc, same_engine_sync=cfg.get("ses", True))
        self.dram = {}

    def din(self, name, shape, dt=F32):
        t = self.nc.dram_tensor(name, list(shape), dt, kind="ExternalInput").ap()
        self.dram[name] = t
        return t

    def dout(self, name, shape, dt=F32):
        t = self.nc.dram_tensor(name, list(shape), dt, kind="ExternalOutput").ap()
        self.dram[name] = t
        return t

    def build(self):
        cfg = self.cfg
        P = self.P
        nc = self.nc
        layers = cfg.get("layers", [0, 1])
        x_d = self.din("x", [T, D])
        win_d = self.din("w_in", [DEPTH, D, WEXT])
        cw_d = self.din("conv_w", [DEPTH, 128, 12, 4])
        alog_d = self.din("a_log", [DEPTH, 128, 4])
        dtb_d = self.din("dt_bias", [DEPTH, 128, 4])
        gnw_d = self.din("gdn_norm_w", [DEPTH, 128, 128])
        wo_d = self.din("w_o", [DEPTH, D, D])
        lnp_d = {k: self.din(k, [DEPTH, 128, D]) for k in ("ln1_g", "ln1_b", "ln2_g", "ln2_b")}
        lnc_d = {k: self.din(k + "_c", [DEPTH, 128, KC]) for k in ("ln1_g", "ln1_b", "ln2_g", "ln2_b")}
        fwg_d = self.din("ffn_w_gate", [D, D_FF])
        fwu_d = self.din("ffn_w_up", [D, D_FF])
        fwd_d = self.din("ffn_w_down", [D_FF, D])
        rw_d = self.din("router_w", [D, NE])
        mwg_d = self.din("moe_w_gate", [NE, D, D_FFE])
        mwu_d = self.din("moe_w_up", [NE, D, D_FFE])
        mwd_d = self.din("moe_w_down", [NE, D_FFE, D])
        cst_d = {}
        cshapes = {"ident32": [128, 128], "ones32": [128, 128], "ropeC": [128, T], "ropeS": [128, T],
                   "r_maskT": [128, 4, 128], "r_col": [128, 12], "g_tri": [128, 128], "g_su": [128, 128],
                   "g_blk": [128, 128], "g_negS": [128, 128], "g_negI": [128, 128],
                   "g_selA": [128, 128], "g_selB": [128, 128]}
        for k in CONST_NAMES:
            cst_d[k] = self.din("c_" + k, cshapes[k])
        out_d = self.dout("out", [T, D])
        self.dbg_d = None
        if cfg.get("dbg"):
            self.dbg_d = self.dout("dbg", cfg["dbg_shape"])

        self.X = P.sb("X", [128, NT, D], F32)
        self.XT = P.sb("XT", [128, KC, T], BF16)
        self.ident32 = P.sb("ident32", [128, 128], F32)
        self.identb = P.sb("identb", [128, 128], BF16)
        self.ones32 = P.sb("ones32", [128, 128], F32)
        self.small = P.sb("small", [128, NT, 16], F32)
        self.CMB = P.sb("CMB", [128, NT, NE], F32)
        self.PS = [P.ps("ps%d" % k, [128, 512], F32) for k in range(7)]
        self.PSB = P.ps("psb", [128, 1024], BF16)

        P.dma(self.ident32[:], cst_d["ident32"], writes=["ident32"], key="c0")
        P.dma(self.ones32[:], cst_d["ones32"], writes=["ones32"], key="c1")
        P.dma(self.identb[:], cst_d["ident32"], writes=["identb"], key="c2", eng="pool")
        self.onesb = P.sb("onesb", [128, 128], BF16)
        P.dma(self.onesb[:], cst_d["ones32"], writes=["onesb"], key="c3", eng="pool")
        self.cst_d = cst_d
        xv = x_d.rearrange("(tt p) d -> p tt d", p=128)
        for q in range(4):
            P.dma(self.X[:, q * 4:(q + 1) * 4, :], xv[:, q * 4:(q + 1) * 4, :],
                  writes=[("X", tt) for tt in range(q * 4, q * 4 + 4)], key="xin%d" % q)
        for tt in range(NT):
            self.make_xt(tt)

        for l in layers:
            if cfg.get("mixer", True):
                self.mixer(l, win_d, cw_d, alog_d, dtb_d, gnw_d, wo_d)
            if cfg.get("mixer", True) or cfg.get("ln1"):
                self.layer_norm(l, "ln1", lnp_d, lnc_d, route=(l == 1 and cfg.get("ffn", True)), rw_d=rw_d)
            if cfg.get("ln_only"):
                self.layer_norm(l, "ln2", lnp_d, lnc_d)
            if cfg.get("ffn", True):
                if l == 0:
                    self.ffn(1, D_FF, lambda e: fwg_d, lambda e: fwu_d, lambda e: fwd_d, None, "f0")
                else:
                    self.ffn(NE, D_FFE, lambda e: mwg_d[e], lambda e: mwu_d[e], lambda e: mwd_d[e], self.CMB, "f1")
                if not cfg.get("no_ln2"):
                    self.layer_norm(l, "ln2", lnp_d, lnc_d, need_xt=(l != layers[-1]))

        ov = out_d.rearrange("(tt p) d -> p tt d", p=128)
        for q in range(4):
            P.dma(ov[:, q * 4:(q + 1) * 4, :], self.X[:, q * 4:(q + 1) * 4, :],
                  reads=[("X", tt) for tt in range(q * 4, q * 4 + 4)], key="xout%d" % q)
        P.emit()
        return nc

    def make_xt(self, tt, xt32=None):
        P = self.P
        X, XT = self.X, self.XT
        for half in range(2):
            bi = 5 + ((tt * 2 + half) % 2)
            bank = self.PS[bi]
            btok = ("ps", bi)
            for q in range(4):
                kc = half * 4 + q
                P.pe(lambda e, o=bank[:, q * 128:(q + 1) * 128], i=X[:, tt, kc * 128:(kc + 1) * 128]:
                     e.transpose(o, i, self.ident32[:]),
                     reads=[("X", tt), "ident32"], writes=[btok])
            outap = XT[:, half * 4:(half + 1) * 4, tt * 128:(tt + 1) * 128]
            inap = bank[:].rearrange("p (q c) -> p q c", q=4)
            xtok = ("XT", tt, half)
            if half == 0:
                P.act(lambda e, o=outap, i=inap: e.copy(o, i), reads=[btok], writes=[xtok])
            else:
                P.dve(lambda e, o=outap, i=inap: e.tensor_copy(o, i), reads=[btok], writes=[xtok])
            if xt32 is not None:
                o32 = xt32[:, half * 4:(half + 1) * 4, :]
                if half == 0:
                    P.act(lambda e, o=o32, i=inap: e.copy(o, i), reads=[btok], writes=[("xt32", id(xt32), half)])
                else:
                    P.dve(lambda e, o=o32, i=inap: e.tensor_copy(o, i), reads=[btok], writes=[("xt32", id(xt32), half)])

    def layer_norm(self, l, which, lnp_d, lnc_d, route=False, rw_d=None, need_xt=True):
        P = self.P
        X = self.X
        P.push_scope()
        LNG = P.sb("LNG", [128, D], F32)
        LNB = P.sb("LNB", [128, D], F32)
        P.dma(LNG[:], lnp_d[which + "_g"][l], writes=["LNG"], key="lng")
        P.dma(LNB[:], lnp_d[which + "_b"][l], writes=["LNB"], key="lnb")
        sm = self.small
        if route:
            RW = P.sb("RW", [128, KC, NE], F32)
            XT32 = [P.sb("XT32_%d" % i, [128, KC, 128], F32) for i in range(2)]
            LG = P.sb("LG", [128, NT, NE], F32)
            P.dma(RW[:], rw_d.rearrange("(kc p) e -> p kc e", p=128), writes=["RW"], key="rw")
        for tt in range(NT):
            st = ("sm", tt)
            xt = ("X", tt)
            for hf in range(2):
                P.dve(lambda e, o=sm[:, tt, hf * 6:(hf + 1) * 6], i=X[:, tt, hf * 512:(hf + 1) * 512]: e.bn_stats(o, i),
                      reads=[xt, st], writes=[st])
            P.dve(lambda e, o=sm[:, tt, 12:14], i=sm[:, tt, 0:12].rearrange("p (a b) -> p a b", a=2): e.bn_aggr(o, i),
                  reads=[st], writes=[st])
        allst = [("sm", tt) for tt in range(NT)]
        P.act(lambda e: e.activation(sm[:, :, 14], sm[:, :, 13], AF.Sqrt, bias=LN_EPS_S), reads=allst, writes=["sm_r"])
        P.dve(lambda e: e.reciprocal(sm[:, :, 14], sm[:, :, 14]), reads=["sm_r"], writes=["sm_r"])
        P.dve(lambda e: e.scalar_tensor_tensor(sm[:, :, 15], sm[:, :, 12], -1.0, sm[:, :, 14], ALU.mult, ALU.mult), reads=allst + ["sm_r"], writes=["sm_r"])
        for tt in range(NT):
            xt = ("X", tt)
            P.act(lambda e, o=X[:, tt, :], s=sm[:, tt, 14:15], b=sm[:, tt, 15:16]:
                  e.activation(o, o, AF.Identity, bias=b, scale=s), reads=[xt, "sm_r"], writes=[xt])
        for tt in range(NT):
            xt = ("X", tt)
            P.pool(lambda e, o=X[:, tt, :]: e.tensor_tensor(o, o, LNG[:], ALU.mult), reads=[xt, "LNG"], writes=[xt])
            P.pool(lambda e, o=X[:, tt, :]: e.tensor_tensor(o, o, LNB[:], ALU.add), reads=[xt, "LNB"], writes=[xt])
        for tt in range(NT if need_xt else 0):
            xt32 = XT32[tt % 2] if route else None
            self.make_xt(tt, xt32=xt32)
            if route and self.cfg.get("route_mm", True):
                bank = self.PS[4]
                for kc in range(KC):
                    P.pe(lambda e, o=bank[:, 0:NE], a=xt32[:, kc, :], b=RW[:, kc, :], s=(kc == 0), t=(kc == KC - 1):
                         e.matmul(o, a, b, start=s, stop=t),
                         reads=[("xt32", id(xt32), kc // 4), "RW"], writes=[("ps", 4)])
                P.dve(lambda e, o=LG[:, tt, :], i=bank[:, 0:NE]: e.tensor_copy(o, i), reads=[("ps", 4)], writes=["LG"])
        if route:
            if self.cfg.get("route_stop", 9) >= 2:
                self.route(LG)
            else:
                P.dve(lambda e: e.memset(self.CMB[:], INVA), reads=["LG"], writes=["CMB"])
        P.pop_scope()

    def route(self, LG):
        P = self.P
        m1 = P.sb("rt_m1", [128, NT], F32)
        m2 = P.sb("rt_m2", [128, NT], F32)
        eq1 = P.sb("rt_eq1", [128, NT, NE], F32)
        eq2 = P.sb("rt_eq2", [128, NT, NE], F32)
        L2 = P.sb("rt_L2", [128, NT, NE], F32)
        g1 = P.sb("rt_g1", [128, NT], F32)
        g2 = P.sb("rt_g2", [128, NT], F32)
        R = ["LG", "rt"]
        bc = lambda a: a[:].unsqueeze(2).to_broadcast([128, NT, NE])
        P.dve(lambda e: e.tensor_reduce(m1[:], LG[:], AX.X, ALU.max), reads=R, writes=["rt"])
        P.dve(lambda e: e.tensor_tensor(eq1[:], LG[:], bc(m1), ALU.is_equal), reads=R, writes=["rt"])
        P.dve(lambda e: e.scalar_tensor_tensor(L2[:], eq1[:], -1.0e30, LG[:], ALU.mult, ALU.add), reads=R, writes=["rt"])
        P.dve(lambda e: e.tensor_reduce(m2[:], L2[:], AX.X, ALU.max), reads=R, writes=["rt"])
        P.dve(lambda e: e.tensor_tensor(eq2[:], L2[:], bc(m2), ALU.is_equal), reads=R, writes=["rt"])
        P.dve(lambda e: e.tensor_tensor(g2[:], m2[:], m1[:], ALU.subtract), reads=R, writes=["rt"])
        P.act(lambda e: e.activation(g2[:], g2[:], AF.Exp), reads=R, writes=["rt"])
        P.dve(lambda e: e.tensor_scalar(g1[:], g2[:], 1.0, None, ALU.add), reads=R, writes=["rt"])
        P.dve(lambda e: e.reciprocal(g1[:], g1[:]), reads=R, writes=["rt"])
        P.dve(lambda e: e.tensor_tensor(g2[:], g2[:], g1[:], ALU.mult), reads=R, writes=["rt"])
        P.dve(lambda e: e.tensor_tensor(eq1[:], eq1[:], bc(g1), ALU.mult), reads=R, writes=["rt"])
        P.dve(lambda e: e.tensor_tensor(eq2[:], eq2[:], bc(g2), ALU.mult), reads=R, writes=["rt"])
        P.dve(lambda e: e.tensor_tensor(eq1[:], eq1[:], eq2[:], ALU.add), reads=R, writes=["rt"])
        P.dve(lambda e: e.tensor_scalar(self.CMB[:], eq1[:], INVA, None, ALU.mult), reads=R, writes=["CMB"])

    def ffn(self, n_exp, F, wg_of, wu_of, wd_of, cmb, tag):
        P = self.P
        X, XT = self.X, self.XT
        P.push_scope()
        G = 4
        nft = F // 128
        groups = []
        for e in range(n_exp):
            f0 = 0
            while f0 < nft:
                g = min(G, nft - f0)
                groups.append((e, f0, g))
                f0 += g
        WG = [P.sb("%s_WG%d" % (tag, i), [128, KC, G * 128], BF16) for i in range(2)]
        WU = [P.sb("%s_WU%d" % (tag, i), [128, KC, G * 128], BF16) for i in range(2)]
        WD = [P.sb("%s_WD%d" % (tag, i), [128, G, D], BF16) for i in range(2)]
        HT = [P.sb("%s_HT%d" % (tag, i), [128, G, T], BF16) for i in range(2)]
        SG = [P.sb("%s_SG%d" % (tag, i), [128, 512], F32) for i in range(2)]
        PS = self.PS

        def load(gi):
            load_gu(gi)
            load_d(gi)

        def load_gu(gi):
            e, f0, g = groups[gi]
            s = gi % 2
            c0, c1 = f0 * 128, (f0 + g) * 128
            P.dma(WG[s][:, :, 0:g * 128], wg_of(e)[:, c0:c1].rearrange("(kc p) f -> p kc f", p=128),
                  writes=[("WG", s)], key="%swg%d" % (tag, s), eng="pool")
            P.dma(WU[s][:, :, 0:g * 128], wu_of(e)[:, c0:c1].rearrange("(kc p) f -> p kc f", p=128),
                  writes=[("WU", s)], key="%swu%d" % (tag, s), eng="pool")

        def load_d(gi):
            e, f0, g = groups[gi]
            s = gi % 2
            c0, c1 = f0 * 128, (f0 + g) * 128
            P.dma(WD[s][:, 0:g, :], wd_of(e)[c0:c1, :].rearrange("(j p) m -> p j m", p=128),
                  writes=[("WD", s)], key="%swd%d" % (tag, s), eng="pool")

        def gu(gi):
            e, f0, g = groups[gi]
            s = gi % 2
            cnt = 0
            for j in range(g):
                for tb in range(4):
                    b = cnt % 2
                    cnt += 1
                    pg, pu = PS[b], PS[2 + b]
                    for kc in range(KC):
                        P.pe(lambda en, o=pg[:], a=WG[s][:, kc, j * 128:(j + 1) * 128], r=XT[:, kc, tb * 512:(tb + 1) * 512],
                             st=(kc == 0), sp=(kc == KC - 1): en.matmul(o, a, r, start=st, stop=sp),
                             reads=[("WG", s)] + [("XT", tt, hh) for tt in range(tb * 4, tb * 4 + 4) for hh in range(2)], writes=[("ps", b)])
                    for kc in range(KC):
                        P.pe(lambda en, o=pu[:], a=WU[s][:, kc, j * 128:(j + 1) * 128], r=XT[:, kc, tb * 512:(tb + 1) * 512],
                             st=(kc == 0), sp=(kc == KC - 1): en.matmul(o, a, r, start=st, stop=sp),
                             reads=[("WU", s)] + [("XT", tt, hh) for tt in range(tb * 4, tb * 4 + 4) for hh in range(2)], writes=[("ps", 2 + b)])
                    P.act(lambda en, o=SG[b][:], i=pg[:]: en.activation(o, i, AF.Silu),
                          reads=[("ps", b)], writes=[("SG", b)])
                    P.dve(lambda en, o=HT[s][:, j, tb * 512:(tb + 1) * 512], a=SG[b][:], c=pu[:]:
                          en.tensor_tensor(o, a, c, ALU.mult),
                          reads=[("SG", b), ("ps", 2 + b)], writes=[("HT", s, j, tb)])

        def down(gi):
            e, f0, g = groups[gi]
            s = gi % 2
            for tt in range(NT):
                for hf in range(2):
                    b = 4 + ((tt * 2 + hf) % 2)
                    pd = PS[b]
                    for j in range(g):
                        P.pe(lambda en, o=pd[:], a=HT[s][:, j, tt * 128:(tt + 1) * 128], r=WD[s][:, j, hf * 512:(hf + 1) * 512],
                             st=(j == 0), sp=(j == g - 1): en.matmul(o, a, r, start=st, stop=sp),
                             reads=[("HT", s, j, tt // 4), ("WD", s)], writes=[("ps", b)])
                    sc = INVA if cmb is None else cmb[:, tt, e:e + 1]
                    P.dve(lambda en, o=X[:, tt, hf * 512:(hf + 1) * 512], i=pd[:], sc=sc:
                          en.scalar_tensor_tensor(o, i, sc, o, ALU.mult, ALU.add),
                          reads=[("ps", b), ("X", tt), "CMB"], writes=[("X", tt)])

        ng = len(groups)
        ng = min(ng, self.cfg.get("ffn_ng", ng))
        mode = self.cfg.get("ffn_mode", 3)
        load(0)
        for gi in range(ng):
            if gi + 1 < ng:
                load_gu(gi + 1)
            if mode >= 1:
                gu(gi)
            if gi >= 1 and mode >= 3:
                down(gi - 1)
            if gi + 1 < ng:
                load_d(gi + 1)
        if mode >= 3:
            down(ng - 1)
        if self.cfg.get("dbg") == "ht":
            tmp = P.sb("dbg_tmp", [128, T], F32)
            sl = (ng - 1) % 2
            P.dve(lambda en: en.tensor_copy(tmp[:], HT[sl][:, self.cfg.get("dbg_j", 0), :]), reads=[("HT", sl, j, tb) for j in range(G) for tb in range(4)], writes=["dbgtmp"])
            P.dma(self.dbg_d, tmp[:], reads=["dbgtmp"], key="dbg")
        P.pop_scope()

    def mixer(self, l, win_d, cw_d, alog_d, dtb_d, gnw_d, wo_d):
        P = self.P
        cfg = self.cfg
        X = self.X
        P.push_scope()
        OT = P.sb("OT", [128, KC, T], BF16)
        self.OT = OT
        if cfg.get("retnet", True):
            self.retnet(l, win_d, OT)
        if cfg.get("gdn", True):
            self.gdn(l, win_d, cw_d, alog_d, dtb_d, gnw_d, OT)
        if cfg.get("dbg") == "ot":
            tmp = P.sb("dbg_tmp", [128, T], F32)
            for kc in cfg.get("dbg_kcs", range(KC)):
                P.dve(lambda en, kc=kc: en.tensor_copy(tmp[:], OT[:, kc, :]), reads=[("OT", n) for n in range(NT)] + ["dbgtmp"], writes=["dbgtmp"])
                P.dma(self.dbg_d[:, kc, :], tmp[:], reads=["dbgtmp"], key="dbg")
        if cfg.get("wo", True):
            P.push_scope()
            WO = P.sb("WO", [128, KC, D], BF16)
            P.dma(WO[:], wo_d[l].rearrange("(kc p) m -> p kc m", p=128), writes=["WO"], key="wo", eng="pool")
            for tt in range(NT):
                for hf in range(2):
                    b = (tt * 2 + hf) % 2
                    for kc in range(KC):
                        P.pe(lambda en, o=self.PS[b][:], a=OT[:, kc, tt * 128:(tt + 1) * 128], r=WO[:, kc, hf * 512:(hf + 1) * 512],
                             st=(kc == 0), sp=(kc == KC - 1): en.matmul(o, a, r, start=st, stop=sp),
                             reads=[("OT", tt), "WO"], writes=[("ps", b)])
                    P.dve(lambda en, o=X[:, tt, hf * 512:(hf + 1) * 512], i=self.PS[b][:]:
                          en.scalar_tensor_tensor(o, i, INVA, o, ALU.mult, ALU.add),
                          reads=[("ps", b), ("X", tt)], writes=[("X", tt)])
            P.pop_scope()
        P.pop_scope()

    def retnet(self, l, win_d, OT):
        P = self.P
        PS, PSB, XT = self.PS, self.PSB, self.XT
        cst = self.cst_d
        P.push_scope()
        QTz = [P.sb("r_QTz%d" % h, [128, T], BF16) for h in range(4)]
        for h in range(4):
            ob = 64 * (1 - h % 2)
            P.pool(lambda en, o=QTz[h][ob:ob + 64, :]: en.memset(o, 0.0), writes=[("rqz", h)])
        KT = P.sb("r_KT", [128, 2, T], BF16)
        KZ = P.sb("r_KZ", [128, NT, 256], BF16)
        MASKT = P.sb("r_maskT", [128, 4, 128], F32)
        RCOL = P.sb("r_col", [128, 12], F32)
        P.dma(MASKT[:], cst["r_maskT"], writes=["r_maskT"], key="rc_m")
        P.dma(RCOL[:], cst["r_col"], writes=["r_col"], key="rc_c")
        P.push_scope()
        WQK = P.sb("r_WQK", [128, KC, 1024], BF16)
        RC = [P.sb("r_RC%d" % i, [128, 512], F32) for i in range(2)]
        RS = [P.sb("r_RS%d" % i, [128, 512], F32) for i in range(2)]
        T1 = [P.sb("r_T1%d" % i, [128, 512], F32) for i in range(2)]
        T2 = [P.sb("r_T2%d" % i, [128, 512], F32) for i in range(2)]
        P.dma(WQK[:], win_d[l][:, 0:1024].rearrange("(kc p) c -> p kc c", p=128), writes=["WQK"], key="rwqk", eng="pool")
        cnt = 0
        for tb in range(4):
            s = tb % 2
            P.dma(RC[s][:], cst["ropeC"][:, tb * 512:(tb + 1) * 512], writes=[("RC", s)], key="rc%d" % s)
            P.dma(RS[s][:], cst["ropeS"][:, tb * 512:(tb + 1) * 512], writes=[("RS", s)], key="rs%d" % s)
            xr = [("XT", tt, hh) for tt in range(tb * 4, tb * 4 + 4) for hh in range(2)]
            for which in range(2):
                for j in range(2):
                    c = cnt % 2
                    cnt += 1
                    colA = which * 512 + j * 128
                    colB = colA + 256
                    for (col, bi) in ((colA, c), (colB, 2 + c)):
                        for kc in range(KC):
                            P.pe(lambda en, o=PS[bi][:], a=WQK[:, kc, col:col + 128], r=XT[:, kc, tb * 512:(tb + 1) * 512],
                                 st=(kc == 0), sp=(kc == KC - 1): en.matmul(o, a, r, start=st, stop=sp),
                                 reads=["WQK"] + xr, writes=[("ps", bi)])
                    P.dve(lambda en, o=T1[c][:], a=PS[c][:], b=RC[s][:]: en.tensor_tensor(o, a, b, ALU.mult),
                          reads=[("ps", c), ("RC", s)], writes=[("T1", c)])
                    P.dve(lambda en, o=T2[c][:], a=PS[2 + c][:], b=RS[s][:]: en.tensor_tensor(o, a, b, ALU.mult),
                          reads=[("ps", 2 + c), ("RS", s)], writes=[("T2", c)])
                    if which == 1:
                        P.pool(lambda en, o=KT[:, j, tb * 512:(tb + 1) * 512], a=T1[c][:], b=T2[c][:]: en.tensor_tensor(o, a, b, ALU.add),
                               reads=[("T1", c), ("T2", c)], writes=[("rqk", which, j, tb)])
                    else:
                        for hh in range(2):
                            pr = slice(64 * hh, 64 * hh + 64)
                            P.pool(lambda en, o=QTz[2 * j + hh][pr, tb * 512:(tb + 1) * 512], a=T1[c][pr, :], b=T2[c][pr, :]: en.tensor_tensor(o, a, b, ALU.add),
                                   reads=[("T1", c), ("T2", c)], writes=[("rqk", 0, 2 * j + hh, tb)])
        rstop = self.cfg.get("ret_stop", 99)
        for tt in range(NT if rstop >= 2 else 0):
            for j in range(2):
                P.pe(lambda en, o=PSB[:, j * 128:(j + 1) * 128], i=KT[:, j, tt * 128:(tt + 1) * 128]: en.transpose(o, i, self.identb[:]),
                     reads=[("rqk", 1, j, tt // 4), "identb"], writes=["psb"])
            for j in range(2):
                for hh in range(2):
                    h = 2 * j + hh
                    P.act(lambda en, o=KZ[:, tt, j * 128 + hh * 64: j * 128 + hh * 64 + 64], i=PSB[:, j * 128 + hh * 64: j * 128 + hh * 64 + 64],
                          sc=RCOL[:, 4 + h:5 + h]: en.activation(o, i, AF.Copy, scale=sc),
                          reads=["psb", "r_col"], writes=[("KZ", tt, j, hh)])
        P.pop_scope()
        P.push_scope()
        WV = P.sb("r_WV", [128, KC, 1024], BF16)
        P.dma(WV[:], win_d[l][:, 1024:2048].rearrange("(kc p) c -> p kc c", p=128), writes=["WV"], key="rwv", eng="pool")
        Vt = [P.sb("r_Vt%d" % i, [128, 512], BF16) for i in range(2)]
        RG = [P.sb("r_RG%d" % i, [128, 512], F32) for i in range(2)]
        S32 = P.sb("r_S32", [128, 2, 128], F32)
        Sb = P.sb("r_Sb", [128, 2, 128], BF16)
        PT = [P.sb("r_PT%d" % i, [128, 128], BF16) for i in range(4)]
        TMP = [P.sb("r_TMP%d" % i, [128, 128], F32) for i in range(4)]
        OR = [P.sb("r_OR%d" % i, [128, 4, 128], F32) for i in range(2)]
        OF = [P.sb("r_OF0", [128, 512], F32)] * 2
        ST = P.sb("r_ST", [128, 2, 4, 8], F32)
        R2 = P.sb("r_R2", [128, 2, 4, 2], F32)
        P.dve(lambda en: en.memset(S32[:], 0.0), writes=["rS32"])
        P.dve(lambda en: en.memset(Sb[:], 0.0), writes=[("rSb", 0), ("rSb", 1)])
        for n in range(NT if rstop >= 3 else 0):
            s = n % 2
            tk = slice(n * 128, (n + 1) * 128)
            xr = [("XT", n, hh) for hh in range(2)]
            for part in range(2):
                for kc in range(KC):
                    P.pe(lambda en, o=PS[part][:], a=XT[:, kc, tk], r=WV[:, kc, part * 512:(part + 1) * 512],
                         st=(kc == 0), sp=(kc == KC - 1): en.matmul(o, a, r, start=st, stop=sp),
                         reads=["WV"] + xr, writes=[("ps", part)])
            P.act(lambda en, o=Vt[s][:], i=PS[0][:]: en.copy(o, i), reads=[("ps", 0)], writes=[("rVt", s)])
            P.act(lambda en, o=RG[s][:], i=PS[1][:]: en.activation(o, i, AF.Silu), reads=[("ps", 1)], writes=[("rRG", s)])
            if rstop < 4:
                continue
            for j in range(0 if self.cfg.get("ret_skip_kv") else 2):
                P.pe(lambda en, o=PS[2][:, j * 256:(j + 1) * 256], a=KZ[:, n, j * 128:(j + 1) * 128], r=Vt[s][:, j * 256:(j + 1) * 256]:
                     en.matmul(o, a, r, start=True, stop=True),
                     reads=[("KZ", n, j, 0), ("KZ", n, j, 1), ("rVt", s)], writes=[("ps", 2)])
            for h in range(4):
                j, pb = h // 2, 64 * (h % 2)
                hs = slice(h * 128, (h + 1) * 128)
                P.pe(lambda en, o=PS[3][:, hs], a=KT[:, j, tk], r=QTz[h][:, tk]: en.matmul(o, a, r, start=True, stop=True),
                     reads=[("rqk", 0, h, n // 4), ("rqk", 1, j, n // 4), ("rqz", h)], writes=[("ps", 3)])
            for h in range(4):
                hs = slice(h * 128, (h + 1) * 128)
                P.dve(lambda en, o=PT[h][:], a=PS[3][:, hs], b=MASKT[:, h, :]: en.tensor_tensor(o, a, b, ALU.mult),
                      reads=[("ps", 3), "r_maskT"], writes=[("rPT", h)])
            if rstop < 5:
                continue
            for h in range(4):
                j, pb = h // 2, 64 * (h % 2)
                hs = slice(h * 128, (h + 1) * 128)
                P.pe(lambda en, o=PS[4][:, hs], a=PT[h][:], r=Vt[s][:, hs]: en.matmul(o, a, r, start=True, stop=True),
                     reads=[("rPT", h), ("rVt", s)], writes=[("ps", 4)])
                P.pe(lambda en, o=PS[5][:, hs], a=QTz[h][:, tk], r=Sb[:, j, :]: en.matmul(o, a, r, start=True, stop=True),
                     reads=[("rqk", 0, h, n // 4), ("rqz", h), ("rSb", j)], writes=[("ps", 5)])
            if rstop < 6:
                continue
            for h in range(4):
                hs = slice(h * 128, (h + 1) * 128)
                P.act(lambda en, o=TMP[h][:], i=PS[5][:, hs], sc=RCOL[:, h:h + 1]: en.activation(o, i, AF.Copy, scale=sc),
                      reads=[("ps", 5), "r_col"], writes=[("rTMP", h)])
                P.dve(lambda en, o=OR[s][:, h, :], a=PS[4][:, hs], b=TMP[h][:]: en.tensor_tensor(o, a, b, ALU.add),
                      reads=[("ps", 4), ("rTMP", h)], writes=[("rOR", s, h)])
            for h in range(4):
                j, pb = h // 2, 64 * (h % 2)
                P.dve(lambda en, o=S32[pb:pb + 64, j, :], i=PS[2][pb:pb + 64, j * 256 + (h % 2) * 128: j * 256 + (h % 2) * 128 + 128],
                      sc=RCOL[pb:pb + 64, 8 + j:9 + j]: en.scalar_tensor_tensor(o, o, sc, i, ALU.mult, ALU.add),
                      reads=[("ps", 2), "rS32", "r_col"], writes=["rS32"])
            for j in range(2):
                P.act(lambda en, o=Sb[:, j, :], i=S32[:, j, :]: en.copy(o, i), reads=["rS32"], writes=[("rSb", j)])
            if rstop < 7:
                continue
            for h in range(4):
                P.dve(lambda en, o=ST[:, n % 2, h, 0:6], i=OR[s][:, h, :]: en.bn_stats(o, i), reads=[("rOR", s, h)], writes=[("rST", n % 2, h)])
                P.dve(lambda en, o=ST[:, n % 2, h, 6:8], i=ST[:, n % 2, h, 0:6]: en.bn_aggr(o, i), reads=[("rST", n % 2, h)], writes=[("rST", n % 2, h)])
            st_all = [("rST", n % 2, h) for h in range(4)]
            P.act(lambda en, o=R2[:, n % 2, :, 0], i=ST[:, n % 2, :, 7]: en.activation(o, i, AF.Sqrt, bias=LN_EPS), reads=st_all, writes=[("rR2", n % 2)])
            P.dve(lambda en, o=R2[:, n % 2, :, 0]: en.reciprocal(o, o), reads=[("rR2", n % 2)], writes=[("rR2", n % 2)])
            P.dve(lambda en, o=R2[:, n % 2, :, 1], a=ST[:, n % 2, :, 6], b=R2[:, n % 2, :, 0]: en.scalar_tensor_tensor(o, a, -1.0, b, ALU.mult, ALU.mult),
                  reads=st_all + [("rR2", n % 2)], writes=[("rR2", n % 2)])
            for h in range(4):
                P.act(lambda en, o=OR[s][:, h, :], sc=R2[:, n % 2, h, 0:1], bi=R2[:, n % 2, h, 1:2]: en.activation(o, o, AF.Identity, bias=bi, scale=sc),
                      reads=[("rOR", s, h), ("rR2", n % 2)], writes=[("rOR", s, h)])
            P.dve(lambda en, o=OF[s][:], a=OR[s][:].rearrange("p h e -> p (h e)"), b=RG[s][:]: en.tensor_tensor(o, a, b, ALU.mult),
                  reads=[("rOR", s, h) for h in range(4)] + [("rRG", s)], writes=["rOF"])
            for h in range(4):
                hs = slice(h * 128, (h + 1) * 128)
                P.pe(lambda en, o=PS[6][:, hs], i=OF[s][:, hs]: en.transpose(o, i, self.ident32[:]),
                     reads=["rOF", "ident32"], writes=[("ps", 6)])
            P.act(lambda en, o=OT[:, 0:4, tk], i=PS[6][:].rearrange("p (q c) -> p q c", q=4): en.copy(o, i),
                  reads=[("ps", 6)], writes=[("OT", n)])
        P.pop_scope()
        P.pop_scope()

    def gdn(self, l, win_d, cw_d, alog_d, dtb_d, gnw_d, OT):
        P = self.P
        cfg = self.cfg
        PS, PSB, XT = self.PS, self.PSB, self.XT
        cst = self.cst_d
        I32 = self.ident32
        P.push_scope()
        CN = {}
        for k in ("g_tri", "g_su", "g_blk", "g_negS", "g_negI", "g_selA", "g_selB"):
            CN[k] = P.sb(k, [128, 128], F32)
            P.dma(CN[k][:], cst[k], writes=[k], key="gc_" + k)
        TRI, SU, BLK, NEGS, NEGI, SELA, SELB = (CN[k] for k in ("g_tri", "g_su", "g_blk", "g_negS", "g_negI", "g_selA", "g_selB"))
        CW = P.sb("g_CW", [128, 12, 4], F32)
        ALOG = P.sb("g_ALOG", [128, 4], F32)
        DTB = P.sb("g_DTB", [128, 4], F32)
        GNW = P.sb("g_GNW", [128, 128], F32)
        P.dma(CW[:], cw_d[l], writes=["g_CW"], key="gc_cw")
        P.dma(ALOG[:], alog_d[l], writes=["g_sc"], key="gc_al")
        P.dma(DTB[:], dtb_d[l], writes=["g_sc2"], key="gc_dt")
        P.dma(GNW[:], gnw_d[l], writes=["g_GNW"], key="gc_gn")
        WAB = P.sb("g_WAB", [128, KC, 8], BF16)
        P.dma(WAB[:], win_d[l][:, 4096:4104].rearrange("(kc p) c -> p kc c", p=128), writes=["g_WAB"], key="gwab", eng="pool")
        names = ["AB8", "Z", "Gt", "BETA", "NBETA", "GC", "GL", "EG", "EKD", "BEG"]
        SC = {}
        SC["AB8"] = P.sb("g_AB8", [128, NT, 8], F32)
        for k in names[1:]:
            SC[k] = P.sb("g_" + k, [128, NT, 4], F32)
        CD = P.sb("g_CD", [128, 2, NT, 4], F32)
        NEGA = P.sb("g_NEGA", [128, 4], F32)
        S_ = ["g_scal"]
        for tt in range(NT):
            for kc in range(KC):
                P.pe(lambda en, o=PS[0][:, tt * 8:(tt + 1) * 8], a=XT[:, kc, tt * 128:(tt + 1) * 128], r=WAB[:, kc, :], st=(kc == 0), sp=(kc == KC - 1):
                     en.matmul(o, a, r, start=st, stop=sp), reads=["g_WAB", ("XT", tt, 0), ("XT", tt, 1)], writes=[("ps", 0)])
        P.dve(lambda en: en.tensor_copy(SC["AB8"][:], PS[0][:, 0:128].rearrange("p (t c) -> p t c", c=8)), reads=[("ps", 0)], writes=S_)
        bc4 = lambda a: a[:].unsqueeze(1).to_broadcast([128, NT, 4])
        P.dve(lambda en: en.tensor_tensor(SC["Z"][:], SC["AB8"][:, :, 0:4], bc4(DTB), ALU.add), reads=S_ + ["g_sc2"], writes=S_)
        P.act(lambda en: en.activation(SC["Z"][:], SC["Z"][:], AF.Exp), reads=S_, writes=S_)
        P.act(lambda en: en.activation(SC["Z"][:], SC["Z"][:], AF.Ln, bias=1.0), reads=S_, writes=S_)
        P.act(lambda en: en.activation(NEGA[:], ALOG[:], AF.Exp), reads=["g_sc"], writes=S_)
        P.dve(lambda en: en.scalar_tensor_tensor(SC["Gt"][:], SC["Z"][:], -1.0, bc4(NEGA), ALU.mult, ALU.mult), reads=S_, writes=S_)
        P.act(lambda en: en.activation(SC["BETA"][:], SC["AB8"][:, :, 4:8], AF.Sigmoid), reads=S_, writes=S_)
        P.dve(lambda en: en.tensor_scalar(SC["NBETA"][:], SC["BETA"][:], -1.0, None, ALU.mult), reads=S_, writes=S_)
        gflat = SC["Gt"][:].rearrange("p t c -> p (t c)")
        for (lhs, name, off) in ((TRI, "g_tri", 0), (BLK, "g_blk", 64), (SELA, "g_selA", 128), (SELB, "g_selB", 192)):
            P.pe(lambda en, o=PS[1][:, off:off + 64], a=lhs[:]: en.matmul(o, a, gflat, start=True, stop=True), reads=S_ + [name], writes=[("ps", 1)])
        v64 = lambda off: PS[1][:, off:off + 64].rearrange("p (t c) -> p t c", c=4)
        P.dve(lambda en: en.tensor_copy(SC["GC"][:], v64(0)), reads=[("ps", 1)], writes=S_)
        P.dve(lambda en: en.tensor_copy(SC["GL"][:], v64(64)), reads=[("ps", 1)], writes=S_)
        P.dve(lambda en: en.tensor_copy(CD[:, 0, :, :], v64(128)), reads=[("ps", 1)], writes=S_)
        P.dve(lambda en: en.tensor_copy(CD[:, 1, :, :], v64(192)), reads=[("ps", 1)], writes=S_)
        P.act(lambda en: en.activation(CD[:], CD[:], AF.Exp), reads=S_, writes=S_)
        P.act(lambda en: en.activation(SC["EG"][:], SC["GC"][:], AF.Exp), reads=S_, writes=S_)
        P.dve(lambda en: en.tensor_tensor(SC["EKD"][:], SC["GL"][:], SC["GC"][:], ALU.subtract), reads=S_, writes=S_)
        P.act(lambda en: en.activation(SC["EKD"][:], SC["EKD"][:], AF.Exp), reads=S_, writes=S_)
        P.dve(lambda en: en.tensor_tensor(SC["BEG"][:], SC["BETA"][:], SC["EG"][:], ALU.mult), reads=S_, writes=S_)
        P.barrier()
        gstop = cfg.get("gdn_stop", 99)

        def one_pass(hp):
            P.push_scope()
            QN = P.sb("g_QN", [128, 2, T], BF16)
            KN = P.sb("g_KN", [128, 2, T], BF16)
            KTOK = P.sb("g_KTOK", [128, NT, 256], BF16)
            VTOK = P.sb("g_VTOK", [128, NT, 256], BF16)
            P.push_scope()
            PRE = P.sb("g_PRE", [128, T + 3], F32)
            CV = P.sb("g_CV", [128, T], F32)
            VT = P.sb("g_VT", [128, T], BF16)
            RN = [P.sb("g_RN%d" % i, [128, 512], F32) for i in range(2)]
            WC = [P.sb("g_WC%d" % i, [128, KC, 128], BF16) for i in range(2)]
            P.dve(lambda en: en.memset(PRE[:, 0:3], 0.0), writes=["g_PRE0"])
            SSQB = (4, 5, 6, 0)
            items = []
            cnt = 0
            for kind in range(3):
                for hh in range(2):
                    items.append((kind, hh, kind * 4 + 2 * hp + hh, cnt % 2))
                    cnt += 1

            def stageA(it):
                kind, hh, f, s = it
                c0 = 2048 + f * 128
                P.dma(WC[s][:], win_d[l][:, c0:c0 + 128].rearrange("(kc p) c -> p kc c", p=128), writes=[("g_WC", s)], key="gwc%d" % s, eng="pool")
                for tb in range(4):
                    b = 2 + tb % 2
                    blk = slice(tb * 512, (tb + 1) * 512)
                    for kc in range(KC):
                        P.pe(lambda en, o=PS[b][:], a=WC[s][:, kc, :], r=XT[:, kc, blk], st=(kc == 0), sp=(kc == KC - 1):
                             en.matmul(o, a, r, start=st, stop=sp),
                             reads=[("g_WC", s)] + [("XT", tt, q) for tt in range(tb * 4, tb * 4 + 4) for q in range(2)], writes=[("ps", b)])
                    P.act(lambda en, o=PRE[:, 3 + tb * 512: 3 + (tb + 1) * 512], i=PS[b][:]: en.copy(o, i), reads=[("ps", b)], writes=[("g_PRE", tb)])

            def stageB(it):
                kind, hh, f, s = it
                for tb in range(4):
                    blk = slice(tb * 512, (tb + 1) * 512)
                    pre_r = [("g_PRE", tb), "g_PRE0", "g_CW"] + ([("g_PRE", tb - 1)] if tb > 0 else [])
                    P.dve(lambda en, sc=CW[:, f, 3:4], o=CV[:, blk], i=PRE[:, 3 + tb * 512: 3 + (tb + 1) * 512]: en.tensor_scalar(o, i, sc, None, ALU.mult),
                          reads=pre_r, writes=[("g_CV", tb)])
                    for k in (2, 1, 0):
                        P.dve(lambda en, sc=CW[:, f, k:k + 1], o=CV[:, blk], i=PRE[:, k + tb * 512: k + (tb + 1) * 512]: en.scalar_tensor_tensor(o, i, sc, o, ALU.mult, ALU.add),
                              reads=pre_r + [("g_CV", tb)], writes=[("g_CV", tb)])
                    if kind == 2:
                        P.act(lambda en, o=VT[:, blk], i=CV[:, blk]: en.activation(o, i, AF.Silu), reads=[("g_CV", tb)], writes=[("g_VT", tb)])
                    else:
                        P.act(lambda en, o=CV[:, blk]: en.activation(o, o, AF.Silu), reads=[("g_CV", tb)], writes=[("g_CV", tb)])
                        P.act(lambda en, o=VT[:, blk], i=CV[:, blk]: en.activation(o, i, AF.Square), reads=[("g_CV", tb)], writes=[("g_VT", tb)])
                        b2_ = SSQB[tb]
                        P.pe(lambda en, o=PS[b2_][:], r=VT[:, blk]: en.matmul(o, self.onesb[:], r, start=True, stop=True),
                             reads=[("g_VT", tb), "onesb"], writes=[("ps", b2_)])
                if kind == 2:
                    tposes(VTOK, lambda tt: VT[:, tt * 128:(tt + 1) * 128], lambda tt: [("g_VT", tt // 4)], kind, hh)

            def stageC(it):
                kind, hh, f, s = it
                if kind == 2:
                    return
                dst = QN if kind == 0 else KN
                scale = (128.0 ** -0.5) if kind == 0 else 1.0
                for tb in range(4):
                    blk = slice(tb * 512, (tb + 1) * 512)
                    b2_ = SSQB[tb]
                    P.act(lambda en, o=RN[tb % 2][:], i=PS[b2_][:]: en.activation(o, i, AF.Ln, bias=NORM_EPS), reads=[("ps", b2_)], writes=[("g_RN", tb % 2)])
                    P.act(lambda en, o=RN[tb % 2][:]: en.activation(o, o, AF.Exp, scale=-0.5), reads=[("g_RN", tb % 2)], writes=[("g_RN", tb % 2)])
                    P.dve(lambda en, o=dst[:, hh, blk], a=CV[:, blk], r=RN[tb % 2][:], sc=scale:
                          en.scalar_tensor_tensor(o, a, sc, r, ALU.mult, ALU.mult),
                          reads=[("g_CV", tb), ("g_RN", tb % 2)], writes=[("g_N", kind, hh, tb)])
                if kind == 1:
                    tposes(KTOK, lambda tt: KN[:, hh, tt * 128:(tt + 1) * 128], lambda tt: [("g_N", 1, hh, tt // 4)], kind, hh)

            def tposes(dstT, srcview, rtok_of, kind, hh):
                for rnd in range(2):
                    for q in range(8):
                        tt = rnd * 8 + q
                        P.pe(lambda en, o=PSB[:, q * 128:(q + 1) * 128], i=srcview(tt): en.transpose(o, i, self.identb[:]),
                             reads=rtok_of(tt) + ["identb"], writes=["psb"])
                    P.act(lambda en, o=dstT[:, rnd * 8:(rnd + 1) * 8, hh * 128:(hh + 1) * 128], i=PSB[:].rearrange("p (q c) -> p q c", q=8): en.copy(o, i),
                          reads=["psb"], writes=[("g_TOK", kind, hh, rnd)])

            stageA(items[0])
            for i, it in enumerate(items):
                stageB(it)
                if i + 1 < len(items):
                    stageA(items[i + 1])
                stageC(it)
            P.pop_scope()
            P.push_scope()
            WGG = P.sb("g_WGG", [128, KC, 256], BF16)
            cg = 3584 + 2 * hp * 128
            P.dma(WGG[:], win_d[l][:, cg:cg + 256].rearrange("(kc p) c -> p kc c", p=128), writes=["g_WGG"], key="gwgg", eng="pool")
            f32t = lambda nm: P.sb(nm, [128, 128], F32)
            b16t = lambda nm: P.sb(nm, [128, 128], BF16)
            TT = []
            for hh in range(2):
                d = {}
                for nm in ("R", "R2", "Dm", "DTm", "Na", "Nb", "Mm", "Ma", "Mb", "Pa", "Pb", "VNf", "TMPo"):
                    d[nm] = f32t("g_%s%d" % (nm, hh))
                d["Nm"] = [f32t("g_Nm%d_%d" % (hh, i)) for i in range(2)]
                for nm in ("Pu", "Pw"):
                    d[nm] = b16t("g_%s%d" % (nm, hh))
                for nm in ("VN", "VN2"):
                    d[nm] = [b16t("g_%s%d_%d" % (nm, hh, i)) for i in range(2)]
                d["QKm"] = [b16t("g_QKm%d_%d" % (hh, i)) for i in range(3)]
                d["WT"] = [b16t("g_WT%d_%d" % (hh, i)) for i in range(2)]
                d["U"] = [f32t("g_U%d_%d" % (hh, i)) for i in range(2)]
                d["S32"] = f32t("g_S32_%d" % hh)
                d["SB"] = b16t("g_SB_%d" % hh)
                TT.append(d)
            GO = [P.sb("g_GO%d" % i, [128, 256], F32) for i in range(2)]
            GG = P.sb("g_GG", [128, 256], F32)
            OFg = P.sb("g_OFg", [128, 256], F32)
            SS = P.sb("g_SS", [128, NT, 2], F32)
            RS = P.sb("g_RS", [128, NT, 2], F32)
            P.dve(lambda en: en.memset(SS[:], 0.0), writes=["g_SS"])
            for hh in range(2):
                P.dve(lambda en, t=TT[hh]["S32"]: en.memset(t[:], 0.0), writes=[("gS32", hh)])
                P.dve(lambda en, t=TT[hh]["SB"]: en.memset(t[:], 0.0), writes=[("gSB", hh)])
                for nm in ("VN", "VN2"):
                    for i in range(2):
                        P.pool(lambda en, t=TT[hh][nm][i]: en.memset(t[:], 0.0), writes=[("g2", nm, hh, i)])

            def early(n, hh):
                h = 2 * hp + hh
                d = TT[hh]
                tk = slice(n * 128, (n + 1) * 128)
                bk = PS[hh]
                bt = ("ps", hh)
                bB = PS[2 + hh]
                btB = ("ps", 2 + hh)
                tg = lambda nm: ("g2", nm, hh)
                rq = [("g_N", 0, hh, n // 4)]
                rk = [("g_N", 1, hh, n // 4)]
                gcol = SC["Gt"][:, n, h:h + 1]
                P.dve(lambda en: en.tensor_scalar(d["R"][:], SU[:], gcol, None, ALU.mult), reads=S_ + ["g_su"], writes=[tg("R")])
                P.dve(lambda en: en.tensor_scalar(d["R2"][:], TRI[:], gcol, None, ALU.mult), reads=S_ + ["g_tri"], writes=[tg("R2")])
                P.pe(lambda en: en.matmul(bB[:, 256:384], KN[:, hh, tk], KN[:, hh, tk], start=True, stop=True), reads=rk, writes=[btB])
                P.pe(lambda en: en.matmul(bB[:, 384:512], KN[:, hh, tk], QN[:, hh, tk], start=True, stop=True), reads=rk + rq, writes=[btB])
                yield
                P.pe(lambda en: en.matmul(bk[:, 256:384], TRI[:], d["R"][:], start=True, stop=False), reads=[tg("R"), "g_tri"], writes=[bt])
                P.pe(lambda en: en.matmul(bk[:, 256:384], I32[:], NEGS[:], start=False, stop=True), reads=["ident32", "g_negS"], writes=[bt])
                P.pe(lambda en: en.matmul(bk[:, 384:512], SU[:], d["R2"][:], start=True, stop=False), reads=[tg("R2"), "g_su"], writes=[bt])
                P.pe(lambda en: en.matmul(bk[:, 384:512], I32[:], NEGI[:], start=False, stop=True), reads=["ident32", "g_negI"], writes=[bt])
                yield
                P.act(lambda en: en.activation(d["Dm"][:], bk[:, 256:384], AF.Exp), reads=[bt], writes=[tg("Dm")])
                P.act(lambda en: en.activation(d["DTm"][:], bk[:, 384:512], AF.Exp), reads=[bt], writes=[tg("DTm")])
                yield
                nm_ = d["Nm"][n % 2]
                P.dve(lambda en: en.scalar_tensor_tensor(nm_[:], bB[:, 256:384], SC["NBETA"][:, n, h:h + 1], d["Dm"][:], ALU.mult, ALU.mult),
                      reads=[btB, tg("Dm")] + S_, writes=[("g2", "Nm", hh, n % 2)])
                qkm = d["QKm"][n % 3]
                P.dve(lambda en: en.tensor_tensor(qkm[:], bB[:, 384:512], d["DTm"][:], ALU.mult), reads=[btB, tg("DTm")], writes=[("g2", "QKm", hh, n % 3)])
                yield

            def late(n, hh):
                h = 2 * hp + hh
                d = TT[hh]
                bk = PS[hh]
                bt = ("ps", hh)
                bB = PS[2 + hh]
                btB = ("ps", 2 + hh)
                tg0 = lambda nm: ("g2", nm, hh)
                nmtok = ("g2", "Nm", hh, n % 2)
                tg = lambda nm: nmtok if nm == "Nm" else tg0(nm)
                nm_ = d["Nm"][n % 2]
                P.pe(lambda en: en.transpose(bk[:, 0:128], nm_[:], I32[:]), reads=[nmtok, "ident32"], writes=[bt])
                yield
                P.act(lambda en: en.copy(d["Mm"][:], bk[:, 0:128]), reads=[bt], writes=[tg("Mm")])
                yield
                P.pool(lambda en: en.tensor_tensor(d["Pa"][:], d["Mm"][:], I32[:], ALU.add), reads=[tg("Mm"), "ident32"], writes=[tg("Pa")])
                Nk, Mk, Pk = nm_, d["Mm"], d["Pa"]
                nN, nM, nP = ("Nm", "Mm", "Pa")
                for k in range(5):
                    Nn, nNn = (d["Na"], "Na") if k % 2 == 0 else (d["Nb"], "Nb")
                    Mn, nMn = (d["Ma"], "Ma") if k % 2 == 0 else (d["Mb"], "Mb")
                    Pn, nPn = (d["Pb"], "Pb") if k % 2 == 0 else (d["Pa"], "Pa")
                    P.pe(lambda en, a=Mk, r=Nk: en.matmul(bk[:, 0:128], a[:], r[:], start=True, stop=True), reads=[tg(nN), tg(nM)], writes=[bt])
                    if k < 4:
                        P.pe(lambda en, a=Nk, r=Mk: en.matmul(bB[:, 0:128], a[:], r[:], start=True, stop=True), reads=[tg(nN), tg(nM)], writes=[btB])
                    yield
                    P.act(lambda en, o=Nn: en.copy(o[:], bk[:, 0:128]), reads=[bt], writes=[tg(nNn)])
                    if k < 4:
                        P.dve(lambda en, o=Mn: en.tensor_copy(o[:], bB[:, 0:128]), reads=[btB], writes=[tg(nMn)])
                    yield
                    P.pe(lambda en, a=Nn, r=Pk: en.matmul(bB[:, 128:256], a[:], r[:], start=True, stop=True), reads=[tg(nP), tg(nNn)], writes=[btB])
                    yield
                    P.dve(lambda en, o=Pn, r=Pk: en.tensor_tensor(o[:], bB[:, 128:256], r[:], ALU.add), reads=[btB, tg(nP)], writes=[tg(nPn)])
                    yield
                    if k == 4:
                        P.act(lambda en, r=Pn: en.activation(d["Pu"][:], r[:], AF.Copy, scale=SC["BETA"][:, n, h:h + 1]), reads=[tg(nPn)] + S_, writes=[tg("Pu")])
                        P.act(lambda en, r=Pn: en.activation(d["Pw"][:], r[:], AF.Copy, scale=SC["BEG"][:, n, h:h + 1]), reads=[tg(nPn)] + S_, writes=[tg("Pw")])
                        yield
                    Nk, Mk, Pk = Nn, Mn, Pn
                    nN, nM, nP = nNn, nMn, nPn
                P.pe(lambda en: en.matmul(bB[:, 0:128], d["Pu"][:], VTOK[:, n, hh * 128:(hh + 1) * 128], start=True, stop=True),
                     reads=[tg("Pu"), ("g_TOK", 2, hh, n // 8)], writes=[btB])
                P.pe(lambda en: en.matmul(bk[:, 384:512], KTOK[:, n, hh * 128:(hh + 1) * 128], d["Pw"][:], start=True, stop=True),
                     reads=[tg("Pw"), ("g_TOK", 1, hh, n // 8)], writes=[bt])
                yield
                P.dve(lambda en: en.tensor_copy(d["U"][n % 2][:], bB[:, 0:128]), reads=[btB], writes=[("g2", "U", hh, n % 2)])
                P.act(lambda en: en.copy(d["WT"][n % 2][:], bk[:, 384:512]), reads=[bt], writes=[("g2", "WT", hh, n % 2)])
                yield

            def scan(n, hh):
                tk = slice(n * 128, (n + 1) * 128)
                h = 2 * hp + hh
                d = TT[hh]
                b4 = PS[4 + hh]
                bt4 = ("ps", 4 + hh)
                tg = lambda nm: ("g2", nm, hh)
                go = GO[n % 2]
                for cpar in range(2):
                    rows = slice(64 * cpar, 64 * cpar + 64)
                    P.pe(lambda en: en.matmul(b4[:, 0:128], d["WT"][n % 2][:], d["SB"][:], start=True, stop=True),
                         reads=[("g2", "WT", hh, n % 2), ("gSB", hh)], writes=[bt4])
                    P.pe(lambda en: en.matmul(b4[:, 128:256], QN[:, hh, tk], d["SB"][:], start=True, stop=True),
                         reads=[("g_N", 0, hh, n // 4), ("gSB", hh)], writes=[bt4])
                    yield
                    P.dve(lambda en, rows=rows: en.scalar_tensor_tensor(d["VNf"][rows, :], b4[rows, 0:128], -1.0, d["U"][n % 2][rows, :], ALU.mult, ALU.add),
                          reads=[bt4, ("g2", "U", hh, n % 2)], writes=[tg("VNf")])
                    P.dve(lambda en, rows=rows: en.tensor_scalar(d["TMPo"][rows, :], b4[rows, 128:256], SC["EG"][rows, n, h:h + 1], None, ALU.mult),
                          reads=[bt4] + S_, writes=[tg("TMPo")])
                    yield
                    P.act(lambda en, rows=rows, cpar=cpar: en.copy(d["VN"][cpar][rows, :], d["VNf"][rows, :]),
                          reads=[tg("VNf"), ("g2", "VN", hh, cpar)], writes=[("g2", "VN", hh, cpar)])
                    P.act(lambda en, rows=rows, cpar=cpar: en.activation(d["VN2"][cpar][rows, :], d["VNf"][rows, :], AF.Copy, scale=SC["EKD"][rows, n, h:h + 1]),
                          reads=[tg("VNf"), ("g2", "VN2", hh, cpar)] + S_, writes=[("g2", "VN2", hh, cpar)])
                    yield
                    P.pe(lambda en, cpar=cpar: en.matmul(b4[:, 256:384], d["QKm"][n % 3][:], d["VN"][cpar][:], start=True, stop=True),
                         reads=[("g2", "QKm", hh, n % 3), ("g2", "VN", hh, cpar)], writes=[bt4])
                    P.pe(lambda en, cpar=cpar: en.matmul(b4[:, 384:512], KTOK[:, n, hh * 128:(hh + 1) * 128], d["VN2"][cpar][:], start=True, stop=True),
                         reads=[("g_TOK", 1, hh, n // 8), ("g2", "VN2", hh, cpar)], writes=[bt4])
                    yield
                    P.dve(lambda en, cpar=cpar: en.scalar_tensor_tensor(d["S32"][:], d["S32"][:], CD[:, cpar, n, h:h + 1], b4[:, 384:512], ALU.mult, ALU.add),
                          reads=[bt4, ("gS32", hh)] + S_, writes=[("gS32", hh)])
                    P.dve(lambda en, rows=rows: en.tensor_tensor(go[rows, hh * 128:(hh + 1) * 128], d["TMPo"][rows, :], b4[rows, 256:384], ALU.add),
                          reads=[bt4, tg("TMPo")], writes=[("gGO", n % 2, hh)])
                    yield
                    P.act(lambda en: en.copy(d["SB"][:], d["S32"][:]), reads=[("gS32", hh)], writes=[("gSB", hh)])
                    yield

            def post(n):
                tk = slice(n * 128, (n + 1) * 128)
                b6 = PS[6]
                bt6 = ("ps", 6)
                go = GO[n % 2]
                for kc in range(KC):
                    P.pe(lambda en, kc=kc: en.matmul(b6[:, 0:256], XT[:, kc, tk], WGG[:, kc, :], start=(kc == 0), stop=(kc == KC - 1)),
                         reads=["g_WGG", ("XT", n, 0), ("XT", n, 1)], writes=[bt6])
                for hh in range(2):
                    P.act(lambda en, hh=hh: en.activation(OFg[:, hh * 128:(hh + 1) * 128], go[:, hh * 128:(hh + 1) * 128], AF.Square, accum_out=SS[:, n, hh:hh + 1]),
                          reads=[("gGO", n % 2, hh), "g_SS", ("gOF", hh)], writes=["g_SS", ("gOF", hh)])
                yield
                P.act(lambda en: en.activation(GG[:], b6[:, 0:256], AF.Silu), reads=[bt6], writes=["gGG"])
                P.act(lambda en: en.activation(RS[:, n, :], SS[:, n, :], AF.Sqrt, bias=NORM_EPS, scale=1.0 / 128.0), reads=["g_SS"], writes=["g_RS"])
                yield
                P.dve(lambda en: en.reciprocal(RS[:, n, :], RS[:, n, :]), reads=["g_RS"], writes=["g_RS"])
                yield
                for hh in range(2):
                    hs = slice(hh * 128, (hh + 1) * 128)
                    P.dve(lambda en, hh=hh, hs=hs: en.scalar_tensor_tensor(OFg[:, hs], go[:, hs], RS[:, n, hh:hh + 1], GNW[:], ALU.mult, ALU.mult),
                          reads=[("gGO", n % 2, hh), "g_RS", "g_GNW", ("gOF", hh)], writes=[("gOF", hh)])
                yield
                for hh in range(2):
                    hs = slice(hh * 128, (hh + 1) * 128)
                    P.pool(lambda en, hs=hs: en.tensor_tensor(OFg[:, hs], OFg[:, hs], GG[:, hs], ALU.mult), reads=[("gOF", hh), "gGG"], writes=[("gOF", hh)])
                yield
                for hh in range(2):
                    hs = slice(hh * 128, (hh + 1) * 128)
                    P.pe(lambda en, hh=hh, hs=hs: en.transpose(b6[:, 256 + hh * 128: 256 + (hh + 1) * 128], OFg[:, hs], I32[:]),
                         reads=[("gOF", hh), "ident32"], writes=[bt6])
                yield
                P.act(lambda en: en.copy(OT[:, 4 + 2 * hp: 6 + 2 * hp, tk], b6[:, 256:512].rearrange("p (q c) -> p q c", q=2)),
                      reads=[bt6], writes=[("OT", n)])
                yield

            nt_ = cfg.get("gdn_nt", NT) if gstop >= 3 else 0

            def rr(gens):
                while gens:
                    alive = []
                    for g in gens:
                        try:
                            next(g)
                            alive.append(g)
                        except StopIteration:
                            pass
                    gens = alive

            if nt_:
                rr([early(0, 0), early(0, 1)])
            for it in range(nt_ + 2 if nt_ else 0):
                gens = []
                if it < nt_:
                    gens += [late(it, 0), late(it, 1)]
                if it + 1 < nt_:
                    gens += [early(it + 1, 0), early(it + 1, 1)]
                if 1 <= it <= nt_ and gstop >= 4:
                    gens += [scan(it - 1, 0), scan(it - 1, 1)]
                if 2 <= it <= nt_ + 1 and gstop >= 5:
                    gens += [post(it - 2)]
                rr(gens)
            P.pop_scope()
            P.pop_scope()

        for hp in (cfg.get("gdn_passes", range(2)) if gstop >= 2 else []):
            one_pass(hp)
        P.pop_scope()


def ext_w_in(w_in):
    L = w_in.shape[0]
    q = w_in[:, :, 0:256]
    k = w_in[:, :, 256:512]

    def sw(a):
        a4 = a.reshape(L, D, 4, 2, 32)
        return a4[:, :, :, ::-1, :].reshape(L, D, 256)
    parts = [q, sw(q), k, sw(k), w_in[:, :, 512:]]
    return np.ascontiguousarray(np.concatenate(parts, axis=2))


def prep_inputs(inp):
    f = lambda a: np.ascontiguousarray(np.asarray(a, dtype=np.float32))
    shared = {}
    shared["w_in"] = ext_w_in(f(inp["w_in"]))
    cw = f(inp["conv_w"])
    shared["conv_w"] = np.ascontiguousarray(cw.reshape(DEPTH, 4, 12, 128).transpose(0, 3, 2, 1))
    shared["a_log"] = np.ascontiguousarray(np.broadcast_to(f(inp["a_log"])[:, None, :], (DEPTH, 128, 4)))
    shared["dt_bias"] = np.ascontiguousarray(np.broadcast_to(f(inp["dt_bias"])[:, None, :], (DEPTH, 128, 4)))
    shared["gdn_norm_w"] = np.ascontiguousarray(np.broadcast_to(f(inp["gdn_norm_w"])[:, None, :], (DEPTH, 128, 128)))
    shared["w_o"] = f(inp["w_o"])
    for k in ("ln1_g", "ln1_b", "ln2_g", "ln2_b"):
        a = f(inp[k])
        shared[k] = np.ascontiguousarray(np.broadcast_to(a[:, None, :], (DEPTH, 128, D)))
        shared[k + "_c"] = np.ascontiguousarray(a.reshape(DEPTH, KC, 128).transpose(0, 2, 1))
    shared["ffn_w_gate"] = f(inp["ffn_w_gate"])[0]
    shared["ffn_w_up"] = f(inp["ffn_w_up"])[0]
    shared["ffn_w_down"] = f(inp["ffn_w_down"])[0]
    shared["router_w"] = f(inp["router_w"])[0]
    shared["moe_w_gate"] = f(inp["moe_w_gate"])[0]
    shared["moe_w_up"] = f(inp["moe_w_up"])[0]
    shared["moe_w_down"] = f(inp["moe_w_down"])[0]
    for k, v in make_consts().items():
        shared["c_" + k] = v
    return shared


_CACHE = {}


def run(inp, cfg, cores=8):
    shared = prep_inputs(inp)
    x = np.ascontiguousarray(np.asarray(inp["x"], dtype=np.float32))
    b = Builder(cfg)
    nc = b.build()
    in_maps = []
    for c in range(cores):
        m = dict(shared)
        m["x"] = x[c]
        in_maps.append(m)
    res = run_bass_kernel_spmd(nc, in_maps, core_ids=list(range(cores)))
    return res, b


def kernel(**inputs):
    res, _ = run(inputs, {}, cores=8)
    out = np.stack([r["out"] for r in res.results], axis=0)
    return out.astype(np.float32)
```
